# Optimizing a Trainium2 kernel written in Bass

```python
import jax, jax.numpy as jnp
from jax import lax
import numpy as np

D_MODEL = 1024
BATCH = 16
SEQ = 2048
DEPTH = 1

CTX_LEN = 256
GRID_W = 64
EPS = 1e-6

M_HEADS = 8
M_HEAD_DIM = 128
M_WIDTH = M_HEADS * M_HEAD_DIM
M_CHUNK = 64
M_INIT = -1e30

A_HEADS = 8
A_KV_HEADS = 2
A_GROUP = A_HEADS // A_KV_HEADS
A_HEAD_DIM = 128
A_Q_WIDTH = A_HEADS * A_HEAD_DIM
A_KV_WIDTH = A_KV_HEADS * A_HEAD_DIM
Q_BLOCK = 128
ROPE_THETA = 10000.0

N_BRANCH = 2

N_GROUPS = 4
EXPERTS_PER_GROUP = 8
N_EXPERTS = N_GROUPS * EXPERTS_PER_GROUP
TOP_K = 2
D_EXPERT = 256

SPLITS = (M_WIDTH, M_WIDTH, M_WIDTH, M_WIDTH, 4 * M_HEADS, A_Q_WIDTH, A_KV_WIDTH, A_KV_WIDTH, N_BRANCH * D_MODEL)
IN_WIDTH = 4 * M_WIDTH + 4 * M_HEADS + A_Q_WIDTH + 2 * A_KV_WIDTH + N_BRANCH * D_MODEL

kernel_name = "hybrid_mlstm_gqa_hmoe_dit_layer"


def rms(x, w=None):
    xf = x.astype(jnp.float32)
    y = (xf * lax.rsqrt(jnp.mean(xf * xf, axis=-1, keepdims=True) + EPS)).astype(x.dtype)
    return y if w is None else y * w


def ada_mod(cvec, w, b):
    mod = jax.nn.silu(cvec) @ w + b
    return jnp.split(mod[..., None, :], 6, axis=-1)


def modulate(xn, shift, scale):
    return xn * (1.0 + scale) + shift


def split_proj(p):
    points = [int(s) for s in np.cumsum(SPLITS)[:-1]]
    return jnp.split(p, points, axis=-1)


def axial_rope_tables(n_tokens):
    rows = n_tokens // GRID_W
    row = jnp.repeat(jnp.arange(rows), GRID_W).astype(jnp.float32)
    col = jnp.tile(jnp.arange(GRID_W), rows).astype(jnp.float32)
    half = A_HEAD_DIM // 2
    inv = ROPE_THETA ** (-jnp.arange(0, half, 2, dtype=jnp.float32) / half)
    ang_r = row[:, None] * inv[None, :]
    ang_c = col[:, None] * inv[None, :]
    return (jnp.cos(ang_r), jnp.sin(ang_r), jnp.cos(ang_c), jnp.sin(ang_c))


def rope_axis(x, cos, sin):
    d = x.shape[-1] // 2
    x1, x2 = x[..., :d], x[..., d:]
    return jnp.concatenate([x1 * cos - x2 * sin, x2 * cos + x1 * sin], axis=-1)


def apply_axial_rope(x, tabs):
    cr, sr, cc, sc = tabs
    half = A_HEAD_DIM // 2
    y = jnp.concatenate([rope_axis(x[..., :half], cr, sr), rope_axis(x[..., half:], cc, sc)], axis=-1)
    return y.astype(x.dtype)


def flip(t):
    return jnp.flip(t, axis=2)


def mlstm_inputs(p, b_gate):
    mq, mk, mv, mg = p[0], p[1], p[2], p[4]
    B, L, _ = mq.shape

    def to_heads(t):
        return t.reshape(B, L, M_HEADS, M_HEAD_DIM).transpose(0, 2, 1, 3).astype(jnp.float32)

    g = (mg.astype(jnp.float32) + b_gate.astype(jnp.float32)).reshape(B, L, 4, M_HEADS).transpose(2, 0, 3, 1)
    fwd = (g[0], jax.nn.log_sigmoid(g[1]))
    bwd = (g[2], jax.nn.log_sigmoid(g[3]))
    return to_heads(mq), to_heads(mk), to_heads(mv), fwd, bwd


def mlstm_init(B):
    return (jnp.zeros((B, M_HEADS, M_HEAD_DIM, M_HEAD_DIM), jnp.float32),
            jnp.zeros((B, M_HEADS, M_HEAD_DIM), jnp.float32),
            jnp.full((B, M_HEADS), M_INIT, jnp.float32))


def mlstm_scan(q, k, v, i_pre, logf, state):
    B, H, L, dh = q.shape
    nc = L // M_CHUNK

    def to_chunks(a):
        a = a.reshape(B, H, nc, M_CHUNK, *a.shape[3:])
        return jnp.moveaxis(a, 2, 0)

    xs = (to_chunks(q * (dh ** -0.5)), to_chunks(k), to_chunks(v), to_chunks(i_pre), to_chunks(logf))
    lower = jnp.tril(jnp.ones((M_CHUNK, M_CHUNK), dtype=bool))

    def step(carry, chunk):
        C, n, m = carry
        qc, kc, vc, ic, fc = chunk
        b = jnp.cumsum(fc, axis=-1)
        dmat = jnp.where(lower, b[..., :, None] - b[..., None, :] + ic[..., None, :], -jnp.inf)
        inter = b + m[..., None]
        m_q = jnp.maximum(inter, jnp.max(dmat, axis=-1))
        w_intra = jnp.exp(dmat - m_q[..., None])
        w_inter = jnp.exp(inter - m_q)
        s = jnp.einsum('bhsd,bhrd->bhsr', qc, kc) * w_intra
        num = w_inter[..., None] * jnp.einsum('bhsd,bhde->bhse', qc, C) + jnp.einsum('bhsr,bhre->bhse', s, vc)
        den = w_inter * jnp.einsum('bhsd,bhd->bhs', qc, n) + jnp.sum(s, axis=-1)
        h = num / jnp.maximum(jnp.abs(den), jnp.exp(-m_q))[..., None]
        b_last = b[..., -1]
        dec_in = b_last[..., None] - b + ic
        m_new = jnp.maximum(b_last + m, jnp.max(dec_in, axis=-1))
        w_old = jnp.exp(b_last + m - m_new)
        w_in = jnp.exp(dec_in - m_new[..., None])
        C_new = w_old[..., None, None] * C + jnp.einsum('bhr,bhrd,bhre->bhde', w_in, kc, vc)
        n_new = w_old[..., None] * n + jnp.einsum('bhr,bhrd->bhd', w_in, kc)
        return (C_new, n_new, m_new), h

    final, hs = lax.scan(step, state, xs)
    h = jnp.moveaxis(hs, 0, 2).reshape(B, H, L, dh)
    return h, final


def attn_inputs(p, q_norm_w, k_norm_w):
    aq, ak, av = p[5], p[6], p[7]
    B, L, _ = aq.shape
    q = rms(aq.reshape(B, L, A_KV_HEADS, A_GROUP, A_HEAD_DIM), q_norm_w).transpose(0, 2, 3, 1, 4)
    k = rms(ak.reshape(B, L, A_KV_HEADS, A_HEAD_DIM), k_norm_w).transpose(0, 2, 1, 3)
    v = av.reshape(B, L, A_KV_HEADS, A_HEAD_DIM).transpose(0, 2, 1, 3)
    return q, k, v


def block_attention(q, k, v):
    B, Hk, G, Lq, dh = q.shape
    nb = Lq // Q_BLOCK
    qb = jnp.moveaxis(q.reshape(B, Hk, G, nb, Q_BLOCK, dh), 3, 0)
    scale = dh ** -0.5

    def attend(qblk):
        s = jnp.einsum('bkgqd,bksd->bkgqs', qblk, k).astype(jnp.float32) * scale
        pr = jax.nn.softmax(s, axis=-1).astype(v.dtype)
        return jnp.einsum('bkgqs,bksd->bkgqd', pr, v)

    o = lax.map(attend, qb)
    o = jnp.moveaxis(o, 0, 3).reshape(B, Hk, G, Lq, dh)
    return o.transpose(0, 3, 1, 2, 4).reshape(B, Lq, A_Q_WIDTH)


def merge_branches(p, h_m, o_attn, mh_norm_w, w_bm, w_ba, w_o):
    dtype = o_attn.dtype
    B, L, _ = o_attn.shape
    hm = rms(h_m.transpose(0, 2, 1, 3)).reshape(B, L, M_WIDTH).astype(dtype) * mh_norm_w
    y_m = (jax.nn.sigmoid(p[3]) * hm) @ w_bm
    y_a = o_attn @ w_ba
    g = jax.nn.sigmoid(p[8]).reshape(B, L, N_BRANCH, D_MODEL)
    return (g[:, :, 0] * y_m + g[:, :, 1] * y_a) @ w_o


def hier_moe(h, w_rg, b_rg, w_re, b_re, w_g, w_u, w_d):
    B, L, D = h.shape
    hf = h.reshape(B * L, D)
    g_logits = (hf @ w_rg).astype(jnp.float32) + b_rg.astype(jnp.float32)
    g_sel = jnp.argmax(g_logits, axis=-1)
    p_grp = jnp.max(jax.nn.softmax(g_logits, axis=-1), axis=-1, keepdims=True)
    e_logits = ((hf @ w_re).astype(jnp.float32) + b_re.astype(jnp.float32)).reshape(-1, N_GROUPS, EXPERTS_PER_GROUP)
    e_grp = jnp.einsum('nge,ng->ne', e_logits, jax.nn.one_hot(g_sel, N_GROUPS, dtype=jnp.float32))
    top_v, top_i = lax.top_k(e_grp, TOP_K)
    w_top = jax.nn.softmax(top_v, axis=-1) * p_grp
    eid = g_sel[:, None] * EXPERTS_PER_GROUP + top_i
    combine = jnp.einsum('nk,nke->ne', w_top, jax.nn.one_hot(eid, N_EXPERTS, dtype=jnp.float32)).astype(h.dtype)

    def expert(acc, xs):
        wg, wu, wd, col = xs
        y = (jax.nn.silu(hf @ wg) * (hf @ wu)) @ wd
        return acc + col[:, None] * y, None

    out, _ = lax.scan(expert, jnp.zeros_like(hf), (w_g, w_u, w_d, combine.T))
    return out.reshape(B, L, D)


def setup_inputs(seed: int = 0) -> dict:
    key = jax.random.key(seed)
    ks = jax.random.split(key, 23)
    D = D_MODEL

    def nrm(k, shape, scale):
        return jax.random.normal(k, shape, jnp.float32) * scale

    f_base = jnp.linspace(3.0, 6.0, M_HEADS, dtype=jnp.float32)
    gate_base = jnp.stack([jnp.zeros_like(f_base), f_base, jnp.zeros_like(f_base), f_base]).reshape(-1)
    return {
        "x": nrm(ks[0], (BATCH, SEQ, D), 1.0),
        "c": nrm(ks[1], (BATCH, D), 1.0),
        "ctx": nrm(ks[2], (BATCH, CTX_LEN, D), 1.0),
        "c_ctx": nrm(ks[3], (D,), 1.0),
        "w_ada": nrm(ks[4], (DEPTH, D, 6 * D), 0.5 * D ** -0.5),
        "b_ada": nrm(ks[5], (DEPTH, 6 * D), 0.02),
        "norm1_w": 1.0 + nrm(ks[6], (DEPTH, D), 0.02),
        "w_in": nrm(ks[7], (DEPTH, D, IN_WIDTH), D ** -0.5),
        "b_mgate": gate_base[None, :] + nrm(ks[8], (DEPTH, 4 * M_HEADS), 0.1),
        "q_norm_w": 1.0 + nrm(ks[9], (DEPTH, A_HEAD_DIM), 0.02),
        "k_norm_w": 1.0 + nrm(ks[10], (DEPTH, A_HEAD_DIM), 0.02),
        "mh_norm_w": 1.0 + nrm(ks[11], (DEPTH, M_WIDTH), 0.02),
        "w_branch_m": nrm(ks[12], (DEPTH, M_WIDTH, D), M_WIDTH ** -0.5),
        "w_branch_a": nrm(ks[13], (DEPTH, A_Q_WIDTH, D), A_Q_WIDTH ** -0.5),
        "w_out": nrm(ks[14], (DEPTH, D, D), D ** -0.5),
        "norm2_w": 1.0 + nrm(ks[15], (DEPTH, D), 0.02),
        "w_rg": nrm(ks[16], (DEPTH, D, N_GROUPS), D ** -0.5),
        "b_rg": nrm(ks[17], (DEPTH, N_GROUPS), 0.01),
        "w_re": nrm(ks[18], (DEPTH, D, N_EXPERTS), D ** -0.5),
        "b_re": nrm(ks[19], (DEPTH, N_EXPERTS), 0.01),
        "w_e_gate": nrm(ks[20], (DEPTH, N_EXPERTS, D, D_EXPERT), D ** -0.5),
        "w_e_up": nrm(ks[21], (DEPTH, N_EXPERTS, D, D_EXPERT), D ** -0.5),
        "w_e_down": nrm(ks[22], (DEPTH, N_EXPERTS, D_EXPERT, D), D_EXPERT ** -0.5),
    }


def reference(x, c, ctx, c_ctx, w_ada, b_ada, norm1_w, w_in, b_mgate, q_norm_w, k_norm_w, mh_norm_w,
              w_branch_m, w_branch_a, w_out, norm2_w, w_rg, b_rg, w_re, b_re, w_e_gate, w_e_up, w_e_down):
    B, L, _ = x.shape
    rope = axial_rope_tables(L)
    h_lat, h_ctx = x, ctx
    for l in range(DEPTH):
        update_ctx = l < DEPTH - 1
        sh1, sc1, g1, sh2, sc2, g2 = ada_mod(c, w_ada[l], b_ada[l])
        csh1, csc1, cg1, csh2, csc2, cg2 = ada_mod(c_ctx, w_ada[l], b_ada[l])

        p_lat = split_proj(modulate(rms(h_lat, norm1_w[l]), sh1, sc1) @ w_in[l])
        p_ctx = split_proj(modulate(rms(h_ctx, norm1_w[l]), csh1, csc1) @ w_in[l])

        init = mlstm_init(B)
        cq, ck, cv, (ci_f, cf_f), (ci_b, cf_b) = mlstm_inputs(p_ctx, b_mgate[l])
        hc_f, st_f = mlstm_scan(cq, ck, cv, ci_f, cf_f, init)
        hc_b, st_b = mlstm_scan(flip(cq), flip(ck), flip(cv), flip(ci_b), flip(cf_b), init)
        lq, lk, lv, (li_f, lf_f), (li_b, lf_b) = mlstm_inputs(p_lat, b_mgate[l])
        hl_f, _ = mlstm_scan(lq, lk, lv, li_f, lf_f, st_f)
        hl_b, _ = mlstm_scan(flip(lq), flip(lk), flip(lv), flip(li_b), flip(lf_b), st_b)
        hm_lat = hl_f + flip(hl_b)

        cq_a, ck_a, cv_a = attn_inputs(p_ctx, q_norm_w[l], k_norm_w[l])
        lq_a, lk_a, lv_a = attn_inputs(p_lat, q_norm_w[l], k_norm_w[l])
        lq_a = apply_axial_rope(lq_a, rope)
        lk_a = apply_axial_rope(lk_a, rope)
        ha_lat = block_attention(lq_a, jnp.concatenate([lk_a, ck_a], axis=2), jnp.concatenate([lv_a, cv_a], axis=2))

        mix_lat = merge_branches(p_lat, hm_lat, ha_lat, mh_norm_w[l], w_branch_m[l], w_branch_a[l], w_out[l])
        h_lat_mid = h_lat + g1 * mix_lat

        if update_ctx:
            hm_ctx = hc_f + flip(hc_b)
            ha_ctx = block_attention(cq_a, ck_a, cv_a)
            mix_ctx = merge_branches(p_ctx, hm_ctx, ha_ctx, mh_norm_w[l], w_branch_m[l], w_branch_a[l], w_out[l])
            h_ctx = h_ctx + cg1 * mix_ctx
            f_ctx = modulate(rms(h_ctx, norm2_w[l]), csh2, csc2)
            h_ctx = h_ctx + cg2 * hier_moe(f_ctx, w_rg[l], b_rg[l], w_re[l], b_re[l], w_e_gate[l], w_e_up[l], w_e_down[l])

        f_lat = modulate(rms(h_lat_mid, norm2_w[l]), sh2, sc2)
        h_lat = h_lat_mid + g2 * hier_moe(f_lat, w_rg[l], b_rg[l], w_re[l], b_re[l], w_e_gate[l], w_e_up[l], w_e_down[l])
    return h_lat
```

```python
import os
import numpy as np
import concourse.bass as bass
import concourse.mybir as mybir
from concourse.bass_utils import run_bass_kernel_spmd

F32 = mybir.dt.float32
BF16 = mybir.dt.bfloat16
AF = mybir.ActivationFunctionType
ALU = mybir.AluOpType
AX = mybir.AxisListType

D = 1024
SEQ = 2048
CTX = 256
NT_LAT = 16
NT = 18
NTOK = NT * 128
INW = 7712
EPS = 1e-6
NEXP = 32
DEXP = 256
N_CORES = 8

ENGS = ("pe", "act", "dve", "pool", "sp")


class _Ins:
    __slots__ = ("eng", "idx", "fn", "deps", "dma", "flag")

    def __init__(self, eng, idx, fn, deps, dma):
        self.eng, self.idx, self.fn, self.deps, self.dma, self.flag = eng, idx, fn, deps, dma, False


class Sched:
    def __init__(self, nc):
        self.nc = nc
        self.ins = {e: [] for e in ENGS}
        self.res = {}
        self.dma_cnt = {}
        self.pending = {e: set() for e in ENGS}
        self.order = []
        self.swq = []
        self.SW_LIMIT = 2600

    def add(self, eng, fn, reads=(), writes=(), dma=None, ndesc=0):
        deps = set(self.pending[eng])
        self.pending[eng] = set()
        if dma is not None and eng == "pool":
            tot = sum(n for _, n in self.swq) + ndesc
            while self.swq and tot > self.SW_LIMIT:
                tk, n = self.swq.pop(0)
                deps.add(tk)
                tot -= n
        raw = set()
        for r in reads:
            st = self.res.get(r)
            if st is not None and st[0] is not None:
                deps.add(st[0])
                raw.add(st[0])
            if st is not None and isinstance(r, str) and r.startswith("pb"):
                deps.update(st[1].values())
        for w in writes:
            st = self.res.get(w)
            if st is not None:
                if st[0] is not None:
                    deps.add(st[0])
                deps.update(st[1].values())
        deps = {(d if d[0] == "E" else ("D", d[1], self.dma_cnt[d[1]])) for d in deps}
        idx = len(self.ins[eng])
        self.order.append((eng, idx))
        if dma is None:
            deps = {d for d in deps if not (d[0] == "E" and d[1] == eng and (eng == "pe" or d not in raw))}
            tok = ("E", eng, idx)
        else:
            c = self.dma_cnt.get(dma, 0) + 1
            self.dma_cnt[dma] = c
            tok = ("D", dma, c)
        if dma is not None and eng == "pool":
            self.swq.append((tok, ndesc))
        ins = _Ins(eng, idx, fn, deps, dma)
        self.ins[eng].append(ins)
        for w in writes:
            self.res[w] = [tok, {}]
        for r in reads:
            st = self.res.setdefault(r, [None, {}])
            st[1][(tok[0], tok[1])] = tok
        return tok

    def barrier(self):
        toks = set()
        for e in ENGS:
            if self.ins[e]:
                last = self.ins[e][-1]
                if last.dma is None:
                    toks.add(("E", e, last.idx))
                else:
                    for j in range(len(self.ins[e]) - 1, -1, -1):
                        if self.ins[e][j].dma is None:
                            toks.add(("E", e, j))
                            break
        for k, c in self.dma_cnt.items():
            toks.add(("D", k, c))
        for e in ENGS:
            self.pending[e] |= toks

    def emit(self, block):
        nc = self.nc
        for e in ENGS:
            for ins in self.ins[e]:
                for d in ins.deps:
                    if d[0] == "E":
                        self.ins[d[1]][d[2]].flag = True
        cnt = {}
        for e in ENGS:
            c = 0
            arr = []
            for ins in self.ins[e]:
                if ins.dma is None and ins.flag:
                    c += 1
                arr.append(c)
            cnt[e] = arr
        esem = {e: nc.alloc_semaphore("s_" + e) for e in ENGS}
        dsem = {k: nc.alloc_semaphore("d_%d" % i) for i, k in enumerate(self.dma_cnt)}
        self.n_waits = 0

        if block is None:
            engobjs = {"pe": nc.tensor, "act": nc.scalar, "dve": nc.vector, "pool": nc.gpsimd, "sp": nc.sync}
            seen_all = {e: {} for e in ENGS}
            for (e, idx) in self.order:
                ins = self.ins[e][idx]
                engobj = engobjs[e]
                seen = seen_all[e]
                need = {}
                for d in ins.deps:
                    if d[0] == "E":
                        key, val = ("E", d[1]), cnt[d[1]][d[2]]
                    else:
                        key, val = ("D", d[1]), 16 * d[2]
                    if val > need.get(key, 0):
                        need[key] = val
                for key, val in need.items():
                    if seen.get(key, 0) < val:
                        sem = esem[key[1]] if key[0] == "E" else dsem[key[1]]
                        engobj.wait_ge(sem, val)
                        seen[key] = val
                        self.n_waits += 1
                inst = ins.fn(engobj)
                if ins.dma is not None:
                    inst.then_inc(dsem[ins.dma], 16)
                elif ins.flag:
                    inst.then_inc(esem[e], 1)
            return

        def run(e, engobj):
            seen = {}
            for ins in self.ins[e]:
                need = {}
                for d in ins.deps:
                    if d[0] == "E":
                        key, val = ("E", d[1]), cnt[d[1]][d[2]]
                    else:
                        key, val = ("D", d[1]), 16 * d[2]
                    if val > need.get(key, 0):
                        need[key] = val
                for key, val in need.items():
                    if seen.get(key, 0) < val:
                        sem = esem[key[1]] if key[0] == "E" else dsem[key[1]]
                        engobj.wait_ge(sem, val)
                        seen[key] = val
                        self.n_waits += 1
                inst = ins.fn(engobj)
                if ins.dma is not None:
                    inst.then_inc(dsem[ins.dma], 16)
                elif ins.flag:
                    inst.then_inc(esem[e], 1)

        block.tensor(lambda t: run("pe", t))
        block.scalar(lambda a: run("act", a))
        block.vector(lambda v: run("dve", v))
        block.gpsimd(lambda g: run("pool", g))
        block.sync(lambda s: run("sp", s))


class Pump:
    def __init__(self, gens, width):
        self.queue = list(gens)
        self.width = width
        self.active = []
        self.rr = 0
        self.completed = 0

    def done(self):
        return not self.queue and not self.active

    def step(self, n=1):
        for _ in range(n):
            while len(self.active) < self.width and self.queue:
                self.active.append(self.queue.pop(0))
            if not self.active:
                return
            self.rr %= len(self.active)
            g = self.active[self.rr]
            try:
                next(g)
                self.rr += 1
            except StopIteration:
                self.active.pop(self.rr)
                self.completed += 1

    def drain(self):
        while not self.done():
            self.step(1)


class Arena:
    def __init__(self, nc, words):
        self.t = nc.alloc_sbuf_tensor("arena", [128, words], F32)
        self.words = words
        self.top = 0
        self.peak = 0

    def mark(self):
        return self.top

    def release(self, m):
        self.top = m

    def alloc(self, nbytes):
        w = (nbytes + 3) // 4
        w = (w + 7) // 8 * 8
        off = self.top
        self.top += w
        self.peak = max(self.peak, self.top)
        assert self.top <= self.words, "SBUF arena overflow %d > %d" % (self.top, self.words)
        return off, w

    def f32(self, n):
        off, w = self.alloc(4 * n)
        return self.t[:, off:off + n]

    def bf16(self, n):
        off, w = self.alloc(2 * n)
        self.last = self.t[:, off:off + w]
        return self.t[:, off:off + w].bitcast(BF16)[:, 0:n]


def build_program(NB=2, stop_after=None, dbg=()):
    nc = bass.Bass("TRN2", target_bir_lowering=False)
    NV = NB + 1

    def din(name, shape, dt=F32):
        return nc.dram_tensor(name, list(shape), dt, kind="ExternalInput").ap()

    x_d = din("x", [NB, SEQ, D])
    ctx_d = din("ctxx", [NB, CTX, D])
    cT_d = din("cT", [128, 8 * NV])
    wada_d = din("w_ada", [D, 6 * D])
    bada_d = din("b_adaT", [128, 48])
    n1_d = din("n1T", [128, 8])
    n2_d = din("n2T", [128, 8])
    win_d = din("w_in", [D, INW])
    bmg_d = din("b_mgate", [1, 32])
    qw_d = din("q_norm_w", [1, 128])
    kw_d = din("k_norm_w", [1, 128])
    mhw_d = din("mh_norm_w", [1, D])
    wbm_d = din("w_bm", [D, D])
    wba_d = din("w_ba", [D, D])
    wo_d = din("w_o", [D, D])
    wr_d = din("w_r", [D, 36])
    br_d = din("b_r", [1, 36])
    wgu_d = din("w_egu", [NEXP, D, 2 * DEXP])
    wed_d = din("w_ed", [NEXP, DEXP, D])
    ident_d = din("c_ident", [128, 128])
    trif_d = din("c_trif", [128, 128])
    trib_d = din("c_trib", [128, 128])
    cos_d = din("c_cos", [128, NT_LAT * 128])
    sin_d = din("c_sin", [128, NT_LAT * 128])
    out_d = nc.dram_tensor("out", [NB, SEQ, D], F32, kind="ExternalOutput").ap()
    hmid_d = nc.dram_tensor("hmid_scr", [NB, SEQ, D], F32, kind="Internal").ap()
    otscr_d = nc.dram_tensor("ot_scr", [NB, 128, 4 * SEQ], F32, kind="Internal").ap()
    dbg_d = {}
    for name, shape in dbg:
        dbg_d[name] = nc.dram_tensor("dbg_" + name, list(shape), F32, kind="ExternalOutput").ap()

    S = Sched(nc)
    A = Arena(nc, 53180)
    psum = nc.alloc_psum_tensor("psum", [128, 4096], F32)

    def bank(i, n=512, off=0):
        return psum[:, i * 512 + off: i * 512 + off + n]

    def bank_bf(i, n=1024, off=0):
        return psum[:, i * 512:(i + 1) * 512].bitcast(BF16)[:, off:off + n]

    PB = ["pb%d" % i for i in range(8)]

    win_v = win_d.rearrange("(k p) c -> p k c", p=128)

    def dma(eng, out, in_, reads, writes, sem):
        def nd(ap):
            sh = list(ap.shape)
            n = 1
            for v_ in sh[:-1]:
                n *= v_
            return n
        ndesc = max(nd(out), nd(in_))
        S.add(eng, lambda e: e.dma_start(out=out, in_=in_), reads=reads, writes=writes, dma=sem, ndesc=ndesc)

    def mm(out, lhsT, rhs, start, stop, reads, writes):
        S.add("pe", lambda e: e.matmul(out, lhsT=lhsT, rhs=rhs, start=start, stop=stop), reads=reads, writes=writes)

    def tr(out, in_, ident, reads, writes):
        S.add("pe", lambda e: e.transpose(out, in_, ident), reads=reads, writes=writes)

    def act(out, in_, func, reads, writes, bias=None, scale=None, accum_out=None):
        kw = {}
        if bias is not None:
            kw["bias"] = bias
        if scale is not None:
            kw["scale"] = scale
        if accum_out is not None:
            kw["accum_out"] = accum_out
        S.add("act", lambda e: e.activation(out=out, in_=in_, func=func, **kw), reads=reads, writes=writes)

    def ts(eng, out, in0, s1, s2, op0, op1, reads, writes):
        if op1 is None:
            S.add(eng, lambda e: e.tensor_scalar(out=out, in0=in0, scalar1=s1, scalar2=None, op0=op0), reads=reads, writes=writes)
        else:
            S.add(eng, lambda e: e.tensor_scalar(out=out, in0=in0, scalar1=s1, scalar2=s2, op0=op0, op1=op1), reads=reads, writes=writes)

    def tt(eng, out, in0, in1, op, reads, writes):
        S.add(eng, lambda e: e.tensor_tensor(out=out, in0=in0, in1=in1, op=op), reads=reads, writes=writes)

    def stt(out, in0, scalar, in1, op0, op1, reads, writes):
        S.add("dve", lambda e: e.scalar_tensor_tensor(out=out, in0=in0, scalar=scalar, in1=in1, op0=op0, op1=op1), reads=reads, writes=writes)

    def recip(out, in_, reads, writes):
        S.add("dve", lambda e: e.reciprocal(out=out, in_=in_), reads=reads, writes=writes)

    def copy(eng, out, in_, reads, writes):
        if eng == "act":
            S.add("act", lambda e: e.activation(out=out, in_=in_, func=AF.Copy), reads=reads, writes=writes)
        else:
            S.add(eng, lambda e: e.tensor_copy(out=out, in_=in_), reads=reads, writes=writes)

    def memset(eng, ap, val, writes):
        S.add(eng, lambda e: e.memset(ap, val), writes=writes)

    def reduce(out, in_, axis, op, reads, writes):
        S.add("dve", lambda e: e.tensor_reduce(out=out, in_=in_, axis=axis, op=op), reads=reads, writes=writes)

    def dump(name, src_ap, reads, dst=None):
        if name in dbg_d:
            d = dbg_d[name] if dst is None else dst
            dma("pool", d, src_ap, reads, [("dbg", name)], "dbg_" + name)

    ident_f = A.f32(128)
    trif = A.f32(128)
    trib = A.f32(128)
    ones_f = A.f32(128)
    ident_b = A.bf16(128)
    ones_b = A.bf16(128)
    bmg_row = A.f32(32)
    qw_row = A.f32(128)
    kw_row = A.f32(128)
    mhw_row = A.f32(D)
    br_row = A.f32(36)
    n1T = A.f32(8)
    n2T = A.f32(8)
    badaT = A.f32(48)
    modT = A.f32(48 * NV)
    modT3 = modT.rearrange("p (j v) -> p j v", v=NV)
    A1 = A.f32(8 * NV)
    A2 = A.f32(8 * NV)
    grow1 = [A.f32(D) for _ in range(2)]
    grow = [grow1 for _ in range(NB)]

    dma("sp", ident_f, ident_d, [], ["ident_f"], "c0")
    dma("sp", trif, trif_d, [], ["trif"], "c0")
    dma("sp", trib, trib_d, [], ["trib"], "c0")
    dma("pool", ident_b, ident_d, [], ["ident_b"], "c1")
    dma("sp", bmg_row, bmg_d.partition_broadcast(128), [], ["bmg"], "c0")
    dma("sp", qw_row, qw_d.partition_broadcast(128), [], ["qw"], "c0")
    dma("sp", kw_row, kw_d.partition_broadcast(128), [], ["kw"], "c0")
    dma("sp", mhw_row, mhw_d.partition_broadcast(128), [], ["mhw"], "c0")
    dma("sp", br_row, br_d.partition_broadcast(128), [], ["br"], "c0")
    dma("sp", n1T, n1_d, [], ["n1T"], "c0")
    dma("sp", n2T, n2_d, [], ["n2T"], "c0")
    dma("sp", badaT, bada_d, [], ["badaT"], "c0")
    memset("dve", ones_f, 1.0, ["ones_f"])
    memset("dve", ones_b, 1.0, ["ones_b"])

    mA = A.mark()
    cT = A.f32(8 * NV)
    scT = A.f32(8 * NV)
    wa = [A.f32(8 * 1024).rearrange("p (k c) -> p k c", k=8) for _ in range(2)]
    diag = A.f32(D)
    dma("sp", cT, cT_d, [], ["cT"], "c0")
    act(scT, cT, AF.Silu, ["cT"], ["scT"])
    wada_v = wada_d.rearrange("(k p) c -> p k c", p=128)
    for blk in range(6):
        sl = blk % 2
        for k in range(8):
            dma(("sp", "act")[k % 2] if os.environ.get("KADA2", "1") == "1" else "sp", wa[sl][:, k, :],
                wada_v[:, k, blk * 1024:(blk + 1) * 1024], [], [("wa", sl)], "wa%d" % sl)
        for j in range(8):
            col = (blk * 8 + j) * NV
            for k in range(8):
                mm(bank(0, NV, col), wa[sl][:, k, j * 128:(j + 1) * 128], scT[:, k * NV:(k + 1) * NV],
                   k == 0, k == 7, [("wa", sl), "scT"], [PB[0]])
    tt("dve", modT3, bank(0, 48 * NV).rearrange("p (j v) -> p j v", v=NV),
       badaT.unsqueeze(2).to_broadcast([128, 48, NV]), ALU.add, [PB[0], "badaT"], ["modT"])
    for v in range(NV):
        stt(A1[:, v * 8:(v + 1) * 8], modT3[:, 8:16, v], 1.0, n1T, ALU.add, ALU.mult, ["modT", "n1T"], ["A1"])
        stt(A2[:, v * 8:(v + 1) * 8], modT3[:, 32:40, v], 1.0, n2T, ALU.add, ALU.mult, ["modT", "n2T"], ["A2"])
    if "modT" in dbg_d:
        dump("modT", modT, ["modT"])
    S.barrier()
    A.release(mA)
    if stop_after == "A":
        return finish(nc, S, A)

    for b in range(NB):
        mB = A.mark()
        xmodT = A.bf16(8 * NTOK).rearrange("p (k t) -> p k t", k=8)

        m1 = A.mark()
        diag = A.f32(D)
        for gi, base in enumerate((16, 40)):
            for k in range(8):
                ts("dve", diag[:, k * 128:(k + 1) * 128], ident_f, modT3[:, base + k, b:b + 1], None, ALU.mult, None,
                   ["ident_f", "modT"], ["diag"])
            for hf in range(2):
                mm(bank(hf), ones_f, diag[:, hf * 512:(hf + 1) * 512], True, True, ["ones_f", "diag"], [PB[hf]])
                copy("act", grow[b][gi][:, hf * 512:(hf + 1) * 512], bank(hf), [PB[hf]], [("grow", b, gi)])
        xt = [A.f32(D) for _ in range(3)]
        junk = A.bf16(D)
        xn = [A.bf16(D) for _ in range(2)]
        ss = [A.f32(1) for _ in range(2)]
        sv = [A.f32(1) for _ in range(2)]
        for t in range(NT):
            s3, s2 = t % 3, t % 2
            src = ctx_d[b, t * 128:(t + 1) * 128, :] if t < 2 else x_d[b, (t - 2) * 128:(t - 1) * 128, :]
            vec = NB if t < 2 else b
            dma("sp", xt[s3], src, [], [("xt", s3)], "xt%d" % s3)
            act(junk, xt[s3], AF.Square, [("xt", s3)], ["junk", ("ss", s2)], accum_out=ss[s2])
            ts("dve", sv[s2], ss[s2], 1.0 / D, EPS, ALU.mult, ALU.add, [("ss", s2)], [("sv", s2)])
            act(sv[s2], sv[s2], AF.Sqrt, [("sv", s2)], [("sv", s2)])
            recip(sv[s2], sv[s2], [("sv", s2)], [("sv", s2)])
            ts("pool", xn[s2], xt[s3], sv[s2], 1.0, ALU.mult, ALU.mult, [("xt", s3), ("sv", s2)], [("xn", s2)])
            pbk = 2 + s2
            for k in range(8):
                tr(bank_bf(pbk, 128, k * 128), xn[s2][:, k * 128:(k + 1) * 128], ident_b, [("xn", s2), "ident_b"], [PB[pbk]])
            for k in range(8):
                o = xmodT[:, k, t * 128:(t + 1) * 128]
                i_ = bank_bf(pbk, 128, k * 128)
                if t % 2 == 0:
                    act(o, i_, AF.Identity, [PB[pbk], "A1", "modT"], [("xmodT", t)],
                        bias=modT3[:, k, vec:vec + 1], scale=A1[:, vec * 8 + k:vec * 8 + k + 1])
                else:
                    ts("dve", o, i_, A1[:, vec * 8 + k:vec * 8 + k + 1], modT3[:, k, vec:vec + 1], ALU.mult, ALU.add,
                       [PB[pbk], "A1", "modT"], [("xmodT", t)])
        XM_ALL = [("xmodT", t) for t in range(NT)]
        if b == 0 and "xmodT" in dbg_d:
            for k in range(8):
                dump("xmodT", xmodT[:, k, :], XM_ALL, dst=dbg_d["xmodT"][k * 128:(k + 1) * 128, :])
        S.barrier()
        A.release(m1)
        if stop_after == "1":
            return finish(nc, S, A)


        m3 = A.mark()
        oT = A.bf16(8 * SEQ).rearrange("p (k t) -> p k t", k=8)
        oT_w = A.last
        COSt = A.f32(NT_LAT * 128).rearrange("p (j c) -> p j c", c=128)
        SINt = A.f32(NT_LAT * 128).rearrange("p (j c) -> p j c", c=128)
        waq = [A.bf16(8 * 512).rearrange("p (k c) -> p k c", k=8) for _ in range(2)]
        wakv = [A.bf16(8 * 256).rearrange("p (k c) -> p k c", k=8) for _ in range(2)]
        kTa = [A.bf16(NTOK) for _ in range(2)]
        Va = [A.bf16(NT * 128).rearrange("p (t c) -> p t c", c=128) for _ in range(2)]
        qTa = [A.bf16(4 * SEQ).rearrange("p (g t) -> p g t", g=4) for _ in range(2)]
        qs = [A.f32(512) for _ in range(2)]
        qn = [A.f32(512) for _ in range(2)]
        qt1 = [A.f32(512) for _ in range(2)]
        qr = [A.bf16(512) for _ in range(2)]
        ssq4 = [A.f32(4) for _ in range(2)]
        kq = [A.f32(128) for _ in range(2)]
        kt1 = [A.f32(128) for _ in range(2)]
        kt2 = [A.f32(128) for _ in range(2)]
        kr = [A.bf16(128) for _ in range(2)]
        kjunk = A.f32(128)
        ssk = [A.f32(1) for _ in range(2)]
        Pt = [A.bf16(512) for _ in range(3)]
        rD = [A.f32(512) for _ in range(2)]
        DACC = os.environ.get("KDACC", "0") == "1"
        Pacc = [[A.f32(512) for _ in range(2)] for _ in range(2)] if DACC else None
        SC = float(128 ** -0.5)
        dma("sp", COSt.rearrange("p j c -> p (j c)"), cos_d, [], ["COS"], "cos")
        dma("sp", SINt.rearrange("p j c -> p (j c)"), sin_d, [], ["SIN"], "sin")

        def halves(ap2d, g=None):
            if g is None:
                return ap2d.rearrange("p (a f c) -> p a f c", a=2, f=2)
            return ap2d.rearrange("p (g a f c) -> p g a f c", g=g, a=2, f=2)

        def load_aw(j):
            dma("pool", waq[j], win_v[:, :, 4128 + j * 512: 4128 + (j + 1) * 512], [], [("waq", j)], "waq%d" % j)
            dma("pool", wakv[j], win_v[:, :, 5152 + j * 256: 5152 + (j + 1) * 256], [], [("wakv", j)], "wakv%d" % j)

        KYLD = int(os.environ.get("KYLD", "1"))
        KGAP = int(os.environ.get("KGAP", "4"))
        def gen_k(j, t):
            bk, s2 = 5, t % 2
            ko = s2 * 256
            for k in range(8):
                mm(bank(bk, 256, ko), xmodT[:, k, t * 128:(t + 1) * 128], wakv[j][:, k, :], k == 0, k == 7,
                   [("xmodT", t), ("wakv", j)], [PB[bk]])
            for _y in range(KYLD):
                yield
            act(kjunk, bank(bk, 128, ko), AF.Square, [PB[bk]], ["kjunk", ("ssk", s2)], accum_out=ssk[s2])
            for _y in range(KYLD):
                yield
            copy("act", Va[j][:, t, :], bank(bk, 128, ko + 128), [PB[bk]], [("Va", j, t)])
            for _y in range(KYLD):
                yield
            ts("dve", ssk[s2], ssk[s2], 1.0 / 128, EPS, ALU.mult, ALU.add, [("ssk", s2)], [("ssk", s2)])
            for _y in range(KYLD):
                yield
            act(ssk[s2], ssk[s2], AF.Sqrt, [("ssk", s2)], [("ssk", s2)])
            for _y in range(KYLD):
                yield
            recip(ssk[s2], ssk[s2], [("ssk", s2)], [("ssk", s2)])
            for _y in range(KYLD):
                yield
            if t < 2:
                stt(kr[s2], bank(bk, 128, ko), ssk[s2], kw_row, ALU.mult, ALU.mult, [PB[bk], ("ssk", s2), "kw"], [("kr", s2)])
                yield
            else:
                jt = t - 2
                stt(kq[s2], bank(bk, 128, ko), ssk[s2], kw_row, ALU.mult, ALU.mult, [PB[bk], ("ssk", s2), "kw"], [("kq", s2)])
                yield
                tt("pool", kt1[s2], kq[s2], COSt[:, jt, :], ALU.mult, [("kq", s2), "COS"], [("kt1", s2)])
                yield
                kq4, kt24, sn4 = halves(kq[s2]), halves(kt2[s2]), halves(SINt[:, jt, :])
                tt("pool", kt24[:, :, 0, :], kq4[:, :, 1, :], sn4[:, :, 0, :], ALU.mult, [("kq", s2), "SIN"], [("kt2", s2)])
                yield
                tt("dve", kt24[:, :, 1, :], kq4[:, :, 0, :], sn4[:, :, 1, :], ALU.mult, [("kq", s2), "SIN"], [("kt2", s2)])
                yield
                tt("dve", kr[s2], kt1[s2], kt2[s2], ALU.add, [("kt1", s2), ("kt2", s2)], [("kr", s2)])
                yield
            for _ in range(KGAP):
                yield
            tr(bank_bf(7, 128, 512 + s2 * 128), kr[s2], ident_b, [("kr", s2), "ident_b"], [PB[7]])
            copy("dve", kTa[j][:, t * 128:(t + 1) * 128], bank_bf(7, 128, 512 + s2 * 128), [PB[7]], [("kTa", j, t)])
            for _y in range(KYLD):
                yield

        def gen_q(j, jt):
            t = jt + 2
            bk, s2 = 6, jt % 2
            for k in range(8):
                mm(bank(bk, 512), xmodT[:, k, t * 128:(t + 1) * 128], waq[j][:, k, :], k == 0, k == 7,
                   [("xmodT", t), ("waq", j)], [PB[bk]])
            copy("act", qs[s2], bank(bk, 512), [PB[bk]], [("qs", s2)])
            for _y in range(KYLD):
                yield
            tt("dve", qt1[s2], qs[s2], qs[s2], ALU.mult, [("qs", s2)], [("qt1", s2)])
            for _y in range(KYLD):
                yield
            reduce(ssq4[s2], qt1[s2].rearrange("p (g c) -> p g c", g=4), AX.X, ALU.add, [("qt1", s2)], [("ssq4", s2)])
            for _y in range(KYLD):
                yield
            ts("dve", ssq4[s2], ssq4[s2], 1.0 / 128, EPS, ALU.mult, ALU.add, [("ssq4", s2)], [("ssq4", s2)])
            for _y in range(KYLD):
                yield
            act(ssq4[s2], ssq4[s2], AF.Sqrt, [("ssq4", s2)], [("ssq4", s2)])
            for _y in range(KYLD):
                yield
            recip(ssq4[s2], ssq4[s2], [("ssq4", s2)], [("ssq4", s2)])
            for _y in range(KYLD):
                yield
            qs3 = qs[s2].rearrange("p (g c) -> p g c", g=4)
            qn3 = qn[s2].rearrange("p (g c) -> p g c", g=4)
            tt("dve", qn3, qs3, ssq4[s2].unsqueeze(2).to_broadcast([128, 4, 128]), ALU.mult, [("qs", s2), ("ssq4", s2)], [("qn", s2)])
            for _y in range(KYLD):
                yield
            tt("pool", qn3, qn3, qw_row.unsqueeze(1).to_broadcast([128, 4, 128]), ALU.mult, [("qn", s2), "qw"], [("qn", s2)])
            for _y in range(KYLD):
                yield
            tt("pool", qt1[s2].rearrange("p (g c) -> p g c", g=4), qn3,
               COSt[:, jt, :].unsqueeze(1).to_broadcast([128, 4, 128]), ALU.mult, [("qn", s2), "COS"], [("qt1", s2)])
            for _y in range(KYLD):
                yield
            qn5, qt25, sn4 = halves(qn[s2], 4), halves(qs[s2], 4), halves(SINt[:, jt, :])
            tt("dve", qt25[:, :, :, 0, :], qn5[:, :, :, 1, :], sn4[:, :, 0, :].unsqueeze(1).to_broadcast([128, 4, 2, 32]),
               ALU.mult, [("qn", s2), "SIN"], [("qs", s2)])
            for _y in range(KYLD):
                yield
            tt("pool", qt25[:, :, :, 1, :], qn5[:, :, :, 0, :], sn4[:, :, 1, :].unsqueeze(1).to_broadcast([128, 4, 2, 32]),
               ALU.mult, [("qn", s2), "SIN"], [("qs", s2)])
            for _y in range(KYLD):
                yield
            tt("dve", qr[s2], qt1[s2], qs[s2], ALU.add, [("qt1", s2), ("qs", s2)], [("qr", s2)])
            for _y in range(KYLD):
                yield
            for _ in range(KGAP):
                yield
            for g in range(4):
                tr(bank_bf(7, 128, g * 128), qr[s2][:, g * 128:(g + 1) * 128], ident_b, [("qr", s2), "ident_b"], [PB[7]])
            copy("act", qTa[j][:, :, jt * 128:(jt + 1) * 128], bank_bf(7, 512, 0).rearrange("p (g c) -> p g c", g=4),
                 [PB[7]], [("qTa", j, jt // 4)])
            for _y in range(KYLD):
                yield

        def prep_pumps(j):
            return [Pump([gen_k(j, t) for t in range(NT)], 2), Pump([gen_q(j, jt) for jt in range(NT_LAT)], 2)]

        def core_block(j, g, qb, itn, pumps):
            bO = 2
            bD = 4
            qsl = qTa[j][:, g, qb * 512:(qb + 1) * 512]

            def s_mm(kt):
                bS = (0, 1, 3)[kt % 3]
                mm(bank(bS, 512), kTa[j][:, kt * 128:(kt + 1) * 128], qsl, True, True, [("kTa", j, kt), ("qTa", j, qb)], [PB[bS]])
                act(Pt[kt % 3], bank(bS, 512), AF.Exp, [PB[bS]], [("Pt", kt % 3)], scale=SC)

            s_mm(0)
            s_mm(1)
            for kt in range(NT):
                if kt + 2 < NT:
                    s_mm(kt + 2)
                mm(bank(bO, 512), Va[j][:, kt, :], Pt[kt % 3], kt == 0, kt == NT - 1, [("Va", j, kt), ("Pt", kt % 3)], [PB[bO]])
                if not DACC:
                    mm(bank(bD, 512), ones_b, Pt[kt % 3], kt == 0, kt == NT - 1, ["ones_b", ("Pt", kt % 3)], [PB[bD]])
                else:
                    ae, ai = ("pool", 1) if kt % 3 == 2 else ("dve", 0)
                    pa = Pacc[itn % 2][ai]
                    ka = ("Pacc", itn % 2, ai)
                    if kt == 0 or kt == 2:
                        copy(ae, pa, Pt[kt % 3], [("Pt", kt % 3)], [ka])
                    else:
                        tt(ae, pa, pa, Pt[kt % 3], ALU.add, [ka, ("Pt", kt % 3)], [ka])
                for p_ in pumps:
                    p_.step(int(os.environ.get("KPS", "2")))
            if DACC:
                mm(bank(bD, 512), ones_f, Pacc[itn % 2][0], True, False, ["ones_f", ("Pacc", itn % 2, 0)], [PB[bD]])
                mm(bank(bD, 512), ones_f, Pacc[itn % 2][1], False, True, ["ones_f", ("Pacc", itn % 2, 1)], [PB[bD]])
            r2 = itn % 2
            recip(rD[r2], bank(bD, 512), [PB[bD]], [("rD", r2)])
            tt("dve", oT[:, j * 4 + g, qb * 512:(qb + 1) * 512], bank(bO, 512), rD[r2], ALU.mult,
               [PB[bO], ("rD", r2)], [("oT", j * 4 + g)])

        load_aw(0)
        load_aw(1)
        kpump = Pump([gen_k(j_, t) for j_ in range(2) for t in range(NT)], 2)
        qpump = Pump([gen_q(j_, jt) for j_ in range(2) for jt in range(NT_LAT)], 2)
        itn = 0
        pumps = [kpump, qpump]
        for j in range(2):
            for qb in range(4):
                while kpump.completed < NT * (j + 1) or qpump.completed < NT_LAT * j + 4 * (qb + 1):
                    if kpump.completed < NT * (j + 1):
                        kpump.step(1)
                    if qpump.completed < NT_LAT * j + 4 * (qb + 1):
                        qpump.step(1)
                for g in range(4):
                    if os.environ.get("KSKIPCORE") is None:
                        core_block(j, g, qb, itn, pumps)
                    itn += 1
        for p_ in pumps:
            p_.drain()
        OT_ALL = [("oT", h) for h in range(8)]
        if b == 0 and "oT" in dbg_d:
            for h in range(8):
                dump("oT", oT[:, h, :], OT_ALL, dst=dbg_d["oT"][h * 128:(h + 1) * 128, :])
        for h in range(8):
            dma("sp", otscr_d[b, :, h * (SEQ // 2):(h + 1) * (SEQ // 2)], oT_w[:, h * (SEQ // 2):(h + 1) * (SEQ // 2)],
                [("oT", h)], [("otscr", h)], "otst")
        S.barrier()
        A.release(m3)
        if stop_after == "3":
            return finish(nc, S, A)

        hmgT = A.bf16(8 * SEQ).rearrange("p (k t) -> p k t", k=8)
        m2 = A.mark()
        wgt = A.bf16(8 * 32).rearrange("p (k c) -> p k c", k=8)
        AA = [A.f32(NT * 8).rearrange("p (t h) -> p t h", h=8) for _ in range(2)]
        FL = [A.f32(NT * 8).rearrange("p (t h) -> p t h", h=8) for _ in range(2)]
        EB = [A.f32(NT * 8).rearrange("p (t h) -> p t h", h=8) for _ in range(2)]
        wsl_flat = [A.bf16(8 * 512).rearrange("p (k c) -> p k c", k=8) for _ in range(2)]
        wsl = [w_.rearrange("p k (f c) -> p k f c", f=4) for w_ in wsl_flat]
        qT = [A.bf16(NTOK) for _ in range(2)]
        kT = [A.bf16(NTOK) for _ in range(2)]
        Kt = [A.bf16(NT * 128).rearrange("p (t c) -> p t c", c=128) for _ in range(2)]
        Vv = [A.bf16(NT * 130).rearrange("p (t c) -> p t c", c=130) for _ in range(2)]
        vT = A.bf16(NTOK)
        ogT = [A.bf16(SEQ) for _ in range(2)]
        Tst = [A.f32(136) for _ in range(2)]
        VP = [A.bf16(NT * 130).rearrange("p (t c) -> p t c", c=130) for _ in range(2)]
        CBs = [A.bf16(NT * 130).rearrange("p (t c) -> p t c", c=130) for _ in range(2)]
        W_OUT = 6
        SpS = [A.bf16(128) for _ in range(W_OUT)]
        denS = [A.f32(1) for _ in range(W_OUT)]
        ssq = A.f32(NT_LAT)
        rs16 = A.f32(NT_LAT)
        hg4 = [A.bf16(128) for _ in range(4)]
        sqj = A.f32(128)
        m2g = A.mark()
        G = A.f32(NT * 32).rearrange("p (t c) -> p t c", c=32)
        SP_ = [A.f32(NT * 8).rearrange("p (t h) -> p t h", h=8) for _ in range(2)]
        tmpA = A.f32(NT * 8).rearrange("p (t h) -> p t h", h=8)
        tri = [trif, trib]
        trik = ["trif", "trib"]

        dma("pool", wgt, win_v[:, :, 4096:4128], [], ["wgt"], "wgt")
        for hb in range(2):
            memset("pool", Vv[hb][:, :, 128:129], 1.0, [("Vv1", hb)])
        for t in range(NT):
            bk, col = (0, t * 32) if t < 16 else (1, (t - 16) * 32)
            for k in range(8):
                mm(bank(bk, 32, col), xmodT[:, k, t * 128:(t + 1) * 128], wgt[:, k, :], k == 0, k == 7,
                   [("xmodT", t), "wgt"], [PB[bk]])
        if stop_after == "2a1":
            return finish(nc, S, A)
        tt("dve", G[:, 0:16, :], bank(0).rearrange("p (t c) -> p t c", c=32),
           bmg_row.unsqueeze(1).to_broadcast([128, 16, 32]), ALU.add, [PB[0], "bmg"], ["G"])
        tt("dve", G[:, 16:18, :], bank(1, 64).rearrange("p (t c) -> p t c", c=32),
           bmg_row.unsqueeze(1).to_broadcast([128, 2, 32]), ALU.add, [PB[1], "bmg"], ["G"])
        if stop_after == "2a2":
            return finish(nc, S, A)
        for d_ in range(2):
            fo = 8 + 16 * d_
            io = 16 * d_
            act(tmpA, G[:, :, fo:fo + 8], AF.Exp, ["G"], ["tmpA"], scale=-1.0)
            act(SP_[d_], tmpA, AF.Ln, ["tmpA"], [("SP", d_)], bias=1.0)
            spf = SP_[d_].rearrange("p t h -> p (t h)")
            mm(bank(2, 144, 0), tri[d_], spf, True, True, [trik[d_], ("SP", d_)], [PB[2]])
            mm(bank(3, 144, 0), ones_f, spf, True, True, ["ones_f", ("SP", d_)], [PB[3]])
            cum3 = bank(2, 144, 0).rearrange("p (t h) -> p t h", h=8)
            tot3 = bank(3, 144, 0).rearrange("p (t h) -> p t h", h=8)
            tt("dve", tmpA, G[:, :, io:io + 8], cum3, ALU.add, ["G", PB[2]], ["tmpA"])
            if stop_after == "2a3":
                return finish(nc, S, A)
            act(AA[d_], tmpA, AF.Exp, ["tmpA"], [("AA", d_)])
            act(FL[d_], cum3, AF.Exp, [PB[2]], [("FL", d_)])
            act(EB[d_], tot3, AF.Exp, [PB[3]], [("EB", d_)], scale=-1.0)
        if b == 0:
            dump("G", G.rearrange("p t c -> p (t c)"), ["G"])
            for d_ in range(2):
                dump("AA%d" % d_, AA[d_].rearrange("p t h -> p (t h)"), [("AA", d_)])
                dump("FL%d" % d_, FL[d_].rearrange("p t h -> p (t h)"), [("FL", d_)])
                dump("EB%d" % d_, EB[d_].rearrange("p t h -> p (t h)"), [("EB", d_)])
        if stop_after == "2a":
            return finish(nc, S, A)
        S.barrier()
        A.release(m2g)
        Hs = [A.f32(NT_LAT * 128).rearrange("p (t c) -> p t c", c=128) for _ in range(2)]
        order = [list(range(NT)), [1, 0] + list(range(NT - 1, 1, -1))]
        KDIR = os.environ.get("KDIR")
        KDIR = int(KDIR) if KDIR is not None else None
        QS = float(128 ** -0.5)

        def load_head_w(h):
            sl = h % 2
            dma("pool", wsl_flat[sl], win_v[:, :, h * 512:(h + 1) * 512], [], [("wsl", sl)], "wsl%d" % sl)

        def gen_proj(h):
            sl = h % 2
            hb = h % 2
            W = wsl[sl]
            wk_ = ("wsl", sl)
            cbs = [(0, 512), (512, 512), (1024, 512), (1536, 512), (2048, 256)]
            n = 0
            for ci, (c0, cn) in enumerate(cbs):
                tl = [("xmodT", t) for t in range(c0 // 128, (c0 + cn) // 128)]
                for f_, dst, nm in ((0, qT[hb], ("qT", hb, ci)), (1, kT[hb], ("kT", hb, ci)), (2, vT, ("vT", ci))):
                    bk = 6 + n % 2
                    n += 1
                    for k in range(8):
                        mm(bank(bk, cn), W[:, k, f_, :], xmodT[:, k, c0:c0 + cn], k == 0, k == 7, tl + [wk_], [PB[bk]])
                    if f_ == 0:
                        act(dst[:, c0:c0 + cn], bank(bk, cn), AF.Copy, [PB[bk]], [nm], scale=QS)
                    elif f_ == 1:
                        copy("dve", dst[:, c0:c0 + cn], bank(bk, cn), [PB[bk]], [nm])
                    else:
                        copy("act", dst[:, c0:c0 + cn], bank(bk, cn), [PB[bk]], [nm])
                    yield
                nt_ = cn // 128
                t0 = c0 // 128
                for src, dst3, nm in ((kT[hb], Kt[hb], "Kt"), (vT, Vv[hb], "Vv")):
                    bk = 6 + n % 2
                    n += 1
                    rk = ("kT", hb, ci) if nm == "Kt" else ("vT", ci)
                    for q4 in range(nt_):
                        tr(bank_bf(bk, 128, q4 * 128), src[:, c0 + q4 * 128:c0 + (q4 + 1) * 128], ident_b, [rk, "ident_b"], [PB[bk]])
                    copy("dve" if nm == "Kt" else "act", dst3[:, t0:t0 + nt_, 0:128],
                         bank_bf(bk, nt_ * 128, 0).rearrange("p (t c) -> p t c", c=128), [PB[bk]],
                         [(nm, hb, t0 + q4) for q4 in range(nt_)])
                    yield
            for g4 in range(4):
                bk = 6 + n % 2
                n += 1
                xsl = slice(256 + g4 * 512, 256 + (g4 + 1) * 512)
                tl = [("xmodT", 2 + g4 * 4 + q4) for q4 in range(4)]
                for k in range(8):
                    mm(bank(bk, 512), W[:, k, 3, :], xmodT[:, k, xsl], k == 0, k == 7, tl + [wk_], [PB[bk]])
                act(ogT[hb][:, g4 * 512:(g4 + 1) * 512], bank(bk, 512), AF.Sigmoid, [PB[bk]], [("ogT", hb, g4)])
                yield
            if h + 2 < 8:
                load_head_w(h + 2)

        DC_REG = [[(0, 0), (1, 0)], [(2, 0), (3, 0)]]
        S_REG = [(0, 0), (0, 128), (0, 256), (0, 384), (1, 0), (1, 128)]
        X_REG = [(2, 0), (2, 136), (2, 272), (3, 0), (3, 136), (3, 272)]

        def gen_state(h, d_):
            hb = h % 2
            for i in range(NT):
                t = order[d_][i]
                ts("pool", VP[d_][:, t, 0:129], Vv[hb][:, t, 0:129], AA[d_][:, t, h:h + 1], 1.0, ALU.mult, ALU.mult,
                   [("Vv", hb, t), ("Vv1", hb), ("AA", d_)], [("VP", d_, t)])
                yield
                if i < NT - 1:
                    bC, cC = DC_REG[d_][i % 2]
                    kC = PB[bC]
                    mm(bank(bC, 129, cC), Kt[hb][:, t, :], VP[d_][:, t, 0:129], True, True, [("Kt", hb, t), ("VP", d_, t)], [kC])
                    yield
                    if i == 0:
                        copy("dve", Tst[d_][:, 0:129], bank(bC, 129, cC), [kC], [("T", d_)])
                    else:
                        tp = order[d_][i - 1]
                        stt(Tst[d_][:, 0:129], Tst[d_][:, 0:129], EB[d_][:, tp, h:h + 1], bank(bC, 129, cC), ALU.mult, ALU.add,
                            [("T", d_), ("EB", d_), kC], [("T", d_)])
                    yield
                    tn = order[d_][i + 1]
                    act(CBs[d_][:, tn, 0:129], Tst[d_][:, 0:129], AF.Identity, [("T", d_), ("EB", d_)], [("CB", d_, tn)],
                        scale=EB[d_][:, t, h:h + 1])
                    yield

        def gen_out(h, d_, t, slot):
            hb = h % 2
            ci = min(t // 4, 4)
            tsl = slice(t * 128, (t + 1) * 128)
            bS, cS = S_REG[slot]
            kS = PB[bS]
            mm(bank(bS, 128, cS), kT[hb][:, tsl], qT[hb][:, tsl], True, True, [("kT", hb, ci), ("qT", hb, ci)], [kS])
            yield
            tt("dve", SpS[slot], bank(bS, 128, cS), tri[d_], ALU.mult, [kS, trik[d_]], [("SpS", slot)])
            yield
            bX, cX = X_REG[slot]
            mm(bank(bX, 129, cX), SpS[slot], VP[d_][:, t, 0:129], True, False, [("SpS", slot), ("VP", d_, t)], [PB[bX]])
            mm(bank(bX, 129, cX), qT[hb][:, tsl], CBs[d_][:, t, 0:129], False, True, [("qT", hb, ci), ("CB", d_, t)], [PB[bX]])
            yield
            act(denS[slot], bank(bX, 1, cX + 128), AF.Abs, [PB[bX]], [("den", slot)])
            yield
            ts("dve", denS[slot], denS[slot], FL[d_][:, t, h:h + 1], None, ALU.max, None,
               [("den", slot), ("FL", d_)], [("den", slot)])
            yield
            recip(denS[slot], denS[slot], [("den", slot)], [("den", slot)])
            yield
            if d_ == 0:
                act(Hs[hb][:, t - 2, :], bank(bX, 128, cX), AF.Identity, [PB[bX], ("den", slot)], [("Hs", hb, t - 2)],
                    scale=denS[slot])
            else:
                stt(Hs[hb][:, t - 2, :], bank(bX, 128, cX), denS[slot], Hs[hb][:, t - 2, :], ALU.mult, ALU.add,
                    [PB[bX], ("den", slot), ("Hs", hb, t - 2)], [("Hs", hb, t - 2)])
            yield

        def gen_norm(h):
            hb = h % 2
            for j in range(NT_LAT):
                act(sqj, Hs[hb][:, j, :], AF.Square, [("Hs", hb, j)], ["sqj", "ssq"], accum_out=ssq[:, j:j + 1])
                yield
            ts("dve", rs16, ssq, 1.0 / 128, EPS, ALU.mult, ALU.add, ["ssq"], ["rs16"])
            act(rs16, rs16, AF.Sqrt, ["rs16"], ["rs16"])
            recip(rs16, rs16, ["rs16"], ["rs16"])
            yield
            for g4 in range(4):
                for j4 in range(4):
                    j = g4 * 4 + j4
                    stt(hg4[j4], Hs[hb][:, j, :], rs16[:, j:j + 1], mhw_row[:, h * 128:(h + 1) * 128], ALU.mult, ALU.mult,
                        [("Hs", hb, j), "rs16", "mhw"], [("hg4", j4)])
                    yield
                for j4 in range(4):
                    tr(bank_bf(5, 128, 512 + j4 * 128), hg4[j4], ident_b, [("hg4", j4), "ident_b"], [PB[5]])
                tt("dve", hmgT[:, h, g4 * 512:(g4 + 1) * 512], bank_bf(5, 512, 512), ogT[hb][:, g4 * 512:(g4 + 1) * 512], ALU.mult,
                   [PB[5], ("ogT", hb, g4)], [("hmgT", h)])
                yield

        load_head_w(0)
        load_head_w(1)
        Pump([gen_proj(0)], 1).drain()
        prev_norm = None
        for h in range(8):
            stp = Pump([gen_state(h, 0), gen_state(h, 1)], 2)
            projp = Pump([gen_proj(h + 1)], 1) if h + 1 < 8 else None
            cnt_ = [0]

            def side():
                cnt_[0] += 1
                if prev_norm is not None and cnt_[0] % 3 == 0:
                    prev_norm.step(1)
                if projp is not None and cnt_[0] % 8 == 0:
                    projp.step(1)

            for d_ in range(2):
                memset("pool", Tst[d_], 0.0, [("T", d_)])
            while not stp.done():
                stp.step(1)
                side()
            items = [(0, t) for t in range(2, NT)] + [(1, t) for t in range(2, NT)]
            if os.environ.get("KNOOUT"):
                items = []
            if os.environ.get("KNITEMS"):
                items = items[:int(os.environ["KNITEMS"])]
            slots = [None] * W_OUT
            while items or any(g_ is not None for g_ in slots):
                for k_ in range(W_OUT):
                    if slots[k_] is None and items:
                        d_, t_ = items.pop(0)
                        slots[k_] = gen_out(h, d_, t_, k_)
                    if slots[k_] is not None:
                        try:
                            next(slots[k_])
                        except StopIteration:
                            slots[k_] = None
                        side()
            if prev_norm is not None:
                prev_norm.drain()
            if projp is not None:
                projp.drain()
            prev_norm = Pump([gen_norm(h)], 1)
            if b == 0 and "Hs%d" % h in dbg_d:
                dump("Hs%d" % h, Hs[h % 2].rearrange("p t c -> p (t c)"), [("Hs", h % 2, j) for j in range(NT_LAT)])
        prev_norm.drain()
        HMG_ALL = [("hmgT", h) for h in range(8)]
        if b == 0 and "hmgT" in dbg_d:
            for h in range(8):
                dump("hmgT", hmgT[:, h, :], HMG_ALL, dst=dbg_d["hmgT"][h * 128:(h + 1) * 128, :])
        S.barrier()
        A.release(m2)
        if stop_after == "2":
            return finish(nc, S, A)


        m4 = A.mark()
        oT = A.bf16(8 * SEQ).rearrange("p (k t) -> p k t", k=8)
        oT_w = A.last
        for h in range(8):
            dma("sp", oT_w[:, h * (SEQ // 2):(h + 1) * (SEQ // 2)], otscr_d[b, :, h * (SEQ // 2):(h + 1) * (SEQ // 2)],
                [("otscr", h)], [("oT", h)], "otld")
        zT_off = A.mark()
        zT = A.bf16(8 * SEQ).rearrange("p (k t) -> p k t", k=8)
        wbm = A.bf16(8 * D).rearrange("p (k c) -> p k c", k=8)
        wba = A.bf16(8 * D).rearrange("p (k c) -> p k c", k=8)
        wbg = [A.bf16(8 * 256).rearrange("p (k c) -> p k c", k=8) for _ in range(2)]
        sg0 = A.f32(512)
        sg1 = A.f32(512)
        zt0 = A.f32(512)
        zt1 = A.f32(512)
        for k in range(8):
            dma("pool", wbm[:, k, :], wbm_d[k * 128:(k + 1) * 128, :], [], ["wbm"], "wbm")
            dma("pool", wba[:, k, :], wba_d[k * 128:(k + 1) * 128, :], [], ["wba"], "wba")
        it = 0
        for c in range(8):
            sl = c % 2
            dma("pool", wbg[sl], win_v[:, :, 5664 + c * 256: 5664 + (c + 1) * 256], [], [("wbg", sl)], "wbg%d" % sl)
            for tb in range(4):
                pb0 = 4 * (it % 2)
                it += 1
                tsl = slice(tb * 512, (tb + 1) * 512)
                xsl = slice(256 + tb * 512, 256 + (tb + 1) * 512)
                xk = [("xmodT", 2 + tb * 4 + q4) for q4 in range(4)]
                for k in range(8):
                    mm(bank(pb0, 512), wbm[:, k, c * 128:(c + 1) * 128], hmgT[:, k, tsl], k == 0, k == 7, ["wbm", ("hmgT", k)], [PB[pb0]])
                for k in range(8):
                    mm(bank(pb0 + 1, 512), wba[:, k, c * 128:(c + 1) * 128], oT[:, k, tsl], k == 0, k == 7, ["wba", ("oT", k)], [PB[pb0 + 1]])
                for k in range(8):
                    mm(bank(pb0 + 2, 512), wbg[sl][:, k, 0:128], xmodT[:, k, xsl], k == 0, k == 7, [("wbg", sl)] + xk, [PB[pb0 + 2]])
                for k in range(8):
                    mm(bank(pb0 + 3, 512), wbg[sl][:, k, 128:256], xmodT[:, k, xsl], k == 0, k == 7, [("wbg", sl)] + xk, [PB[pb0 + 3]])
                act(sg0, bank(pb0 + 2, 512), AF.Sigmoid, [PB[pb0 + 2]], ["sg0"])
                act(sg1, bank(pb0 + 3, 512), AF.Sigmoid, [PB[pb0 + 3]], ["sg1"])
                tt("dve", zt0, bank(pb0, 512), sg0, ALU.mult, [PB[pb0], "sg0"], ["zt0"])
                tt("dve", zt1, bank(pb0 + 1, 512), sg1, ALU.mult, [PB[pb0 + 1], "sg1"], ["zt1"])
                tt("pool", zT[:, c, tsl], zt0, zt1, ALU.add, ["zt0", "zt1"], [("zT", tb)])
        S.barrier()
        if stop_after == "4a":
            return finish(nc, S, A)

        A.release(mB)
        fT = A.bf16(8 * SEQ).rearrange("p (k t) -> p k t", k=8)
        m5 = A.mark()
        LG = A.f32(NT_LAT * 36).rearrange("p (t c) -> p t c", c=36)
        m4b = A.mark()
        wo = A.bf16(8 * D).rearrange("p (k c) -> p k c", k=8)
        wr = A.f32(8 * 36).rearrange("p (k c) -> p k c", k=8)
        xt2 = [A.f32(D) for _ in range(2)]
        hm = [A.f32(D) for _ in range(2)]
        hn = [A.f32(D) for _ in range(2)]
        fr = [A.f32(D).rearrange("p (k c) -> p k c", k=8) for _ in range(2)]
        junk2 = A.bf16(D)
        ss2 = [A.f32(1) for _ in range(2)]
        assert A.top <= zT_off, (A.top, zT_off)
        for k in range(8):
            dma("pool", wo[:, k, :], wo_d[k * 128:(k + 1) * 128, :], [], ["wo"], "wo")
        dma("sp", wr, wr_d.rearrange("(k p) c -> p k c", p=128), [], ["wr"], "wr")
        def gen_4b(jt):
            s2 = jt % 2
            dma("sp", xt2[s2], x_d[b, jt * 128:(jt + 1) * 128, :], [], [("xt2", s2)], "xt2%d" % s2)
            yield
            for hf in range(2):
                for k in range(8):
                    mm(bank(hf, 512), zT[:, k, jt * 128:(jt + 1) * 128], wo[:, k, hf * 512:(hf + 1) * 512], k == 0, k == 7,
                       [("zT", jt // 4), "wo"], [PB[hf]])
                hsl = slice(hf * 512, (hf + 1) * 512)
                tt("dve", hm[s2][:, hsl], bank(hf, 512), grow[b][0][:, hsl], ALU.mult, [PB[hf], ("grow", b, 0)], [("hm", s2)])
                yield
                tt("dve", hm[s2][:, hsl], hm[s2][:, hsl], xt2[s2][:, hsl], ALU.add, [("hm", s2), ("xt2", s2)], [("hm", s2)])
                yield
            dma("sp", hmid_d[b, jt * 128:(jt + 1) * 128, :], hm[s2], [("hm", s2)], [("hmid", jt)], "hmst%d" % s2)
            act(junk2, hm[s2], AF.Square, [("hm", s2)], ["junk2", ("ss2", s2)], accum_out=ss2[s2])
            yield
            ts("dve", ss2[s2], ss2[s2], 1.0 / D, EPS, ALU.mult, ALU.add, [("ss2", s2)], [("ss2", s2)])
            yield
            act(ss2[s2], ss2[s2], AF.Sqrt, [("ss2", s2)], [("ss2", s2)])
            yield
            recip(ss2[s2], ss2[s2], [("ss2", s2)], [("ss2", s2)])
            yield
            act(hn[s2], hm[s2], AF.Identity, [("hm", s2), ("ss2", s2)], [("hn", s2)], scale=ss2[s2])
            yield
            for hf in range(2):
                bt = 2 + 2 * s2 + hf
                for k4 in range(4):
                    k = hf * 4 + k4
                    tr(bank(bt, 128, k4 * 128), hn[s2][:, k * 128:(k + 1) * 128], ident_f, [("hn", s2), "ident_f"], [PB[bt]])
                yield
                for k4 in range(4):
                    k = hf * 4 + k4
                    ts("dve", fr[s2][:, k, :], bank(bt, 128, k4 * 128), A2[:, b * 8 + k:b * 8 + k + 1], modT3[:, 24 + k, b:b + 1],
                       ALU.mult, ALU.add, [PB[bt], "A2", "modT"], [("fr", s2)])
                yield
            copy("act", fT[:, :, jt * 128:(jt + 1) * 128], fr[s2], [("fr", s2)], [("fT", jt // 4)])
            yield
            bl = 6 + s2
            for k in range(8):
                mm(bank(bl, 36), fr[s2][:, k, :], wr[:, k, :], k == 0, k == 7, [("fr", s2), "wr"], [PB[bl]])
            yield
            tt("dve", LG[:, jt, :], bank(bl, 36), br_row, ALU.add, [PB[bl], "br"], ["LG"])
            yield

        Pump([gen_4b(jt) for jt in range(NT_LAT)], 2).drain()
        if b == 0 and "fT" in dbg_d:
            for k in range(8):
                dump("fT", fT[:, k, :], [("fT", q4) for q4 in range(4)], dst=dbg_d["fT"][k * 128:(k + 1) * 128, :])
        if b == 0:
            dump("LG", LG.rearrange("p t c -> p (t c)"), ["LG"])
        S.barrier()
        A.release(m4b)
        if stop_after == "4b":
            return finish(nc, S, A)

        CW = A.f32(NT_LAT * 32).rearrange("p (t e) -> p t e", e=32)
        m5b = A.mark()
        BIG = 1.0e30
        gmax = A.f32(16)
        goh = A.f32(64).rearrange("p (t g) -> p t g", g=4)
        gsh = A.f32(64).rearrange("p (t g) -> p t g", g=4)
        gsum = A.f32(16)
        pgrp = A.f32(16)
        negm = A.f32(64).rearrange("p (t g) -> p t g", g=4)
        em = A.f32(512).rearrange("p (t e) -> p t e", e=32)
        em2 = A.f32(512).rearrange("p (t e) -> p t e", e=32)
        oh1 = A.f32(512).rearrange("p (t e) -> p t e", e=32)
        oh2 = A.f32(512).rearrange("p (t e) -> p t e", e=32)
        v1 = A.f32(16)
        v2 = A.f32(16)
        w1 = A.f32(16)
        w2 = A.f32(16)
        gl = LG[:, :, 0:4]
        el4 = LG[:, :, 4:36].rearrange("p t (g e) -> p t g e", g=4)
        bc4 = lambda a: a.unsqueeze(2).to_broadcast([128, 16, 4])
        bc32 = lambda a: a.unsqueeze(2).to_broadcast([128, 16, 32])
        reduce(gmax, gl, AX.X, ALU.max, ["LG"], ["gmax"])
        tt("dve", goh, gl, bc4(gmax), ALU.is_equal, ["LG", "gmax"], ["goh"])
        tt("dve", gsh, gl, bc4(gmax), ALU.subtract, ["LG", "gmax"], ["gsh"])
        act(gsh, gsh, AF.Exp, ["gsh"], ["gsh"])
        reduce(gsum, gsh, AX.X, ALU.add, ["gsh"], ["gsum"])
        recip(pgrp, gsum, ["gsum"], ["pgrp"])
        ts("dve", negm, goh, BIG, -BIG, ALU.mult, ALU.add, ["goh"], ["negm"])
        tt("dve", em.rearrange("p t (g e) -> p t g e", g=4), el4, negm.unsqueeze(3).to_broadcast([128, 16, 4, 8]), ALU.add,
           ["LG", "negm"], ["em"])
        reduce(v1, em, AX.X, ALU.max, ["em"], ["v1"])
        tt("dve", oh1, em, bc32(v1), ALU.is_equal, ["em", "v1"], ["oh1"])
        stt(em2.rearrange("p t e -> p (t e)"), oh1.rearrange("p t e -> p (t e)"), -BIG, em.rearrange("p t e -> p (t e)"),
            ALU.mult, ALU.add, ["oh1", "em"], ["em2"])
        reduce(v2, em2, AX.X, ALU.max, ["em2"], ["v2"])
        tt("dve", oh2, em2, bc32(v2), ALU.is_equal, ["em2", "v2"], ["oh2"])
        tt("dve", w1, v1, v2, ALU.subtract, ["v1", "v2"], ["w1"])
        act(w1, w1, AF.Sigmoid, ["w1"], ["w1"])
        tt("dve", w1, w1, pgrp, ALU.mult, ["w1", "pgrp"], ["w1"])
        tt("dve", w2, pgrp, w1, ALU.subtract, ["pgrp", "w1"], ["w2"])
        tt("dve", CW, oh1, bc32(w1), ALU.mult, ["oh1", "w1"], ["CW"])
        tt("dve", oh2, oh2, bc32(w2), ALU.mult, ["oh2", "w2"], ["oh2"])
        tt("dve", CW, CW, oh2, ALU.add, ["CW", "oh2"], ["CW"])
        if b == 0:
            dump("CW", CW.rearrange("p t e -> p (t e)"), ["CW"])
        if stop_after == "5b":
            return finish(nc, S, A)

        acc = A.f32(NT_LAT * D).rearrange("p (t c) -> p t c", c=D)
        wgu = [A.bf16(8 * 512).rearrange("p (k c) -> p k c", k=8) for _ in range(2)]
        wdn = [A.bf16(2 * D).rearrange("p (k c) -> p k c", k=2) for _ in range(2)]
        sgl = [A.f32(512) for _ in range(2)]
        hT = [[A.bf16(512) for _ in range(2)] for _ in range(2)]
        hm2 = [A.f32(D) for _ in range(2)]
        ot = [A.f32(D) for _ in range(2)]
        ysc = [A.f32(512) for _ in range(2)]
        NE = NEXP if stop_after != "5c1" else 2

        def load_expert(e):
            sl = e % 2
            dma("pool", wgu[sl], wgu_d[e].rearrange("(k p) c -> p k c", p=128), [], [("wgu", sl)], "wgu%d" % sl)
            dma("pool", wdn[sl], wed_d[e].rearrange("(k p) c -> p k c", p=128), [], [("wdn", sl)], "wdn%d" % sl)

        load_expert(0)
        ity = [0]

        def moe_gu(e, tb, fc):
            sl = e % 2
            tsl = slice(tb * 512, (tb + 1) * 512)
            for k in range(8):
                mm(bank(fc, 512), wgu[sl][:, k, fc * 128:(fc + 1) * 128], fT[:, k, tsl], k == 0, k == 7,
                   [("wgu", sl), ("fT", tb)], [PB[fc]])
            for k in range(8):
                mm(bank(2 + fc, 512), wgu[sl][:, k, 256 + fc * 128:256 + (fc + 1) * 128], fT[:, k, tsl], k == 0, k == 7,
                   [("wgu", sl), ("fT", tb)], [PB[2 + fc]])
            act(sgl[fc], bank(fc, 512), AF.Silu, [PB[fc]], [("sgl", fc)])
            tt("dve", hT[tb % 2][fc], bank(2 + fc, 512), sgl[fc], ALU.mult, [PB[2 + fc], ("sgl", fc)], [("hT", tb % 2, fc)])

        def moe_down(e, tb):
            sl = e % 2
            for q4 in range(4):
                jt = tb * 4 + q4
                by = 4 + 2 * (ity[0] % 2)
                ity[0] += 1
                for hf in range(2):
                    for fc in range(2):
                        mm(bank(by + hf, 512), hT[tb % 2][fc][:, q4 * 128:(q4 + 1) * 128], wdn[sl][:, fc, hf * 512:(hf + 1) * 512],
                           fc == 0, fc == 1, [("hT", tb % 2, fc), ("wdn", sl)], [PB[by + hf]])
                    hsl = slice(hf * 512, (hf + 1) * 512)
                    if e == 0:
                        ts("dve", acc[:, jt, hsl], bank(by + hf, 512), CW[:, jt, e:e + 1], None, ALU.mult, None,
                           [PB[by + hf], "CW"], [("acc", jt, hf)])
                    elif hf == 1:
                        sy = ysc[ity[0] % 2]
                        act(sy, bank(by + hf, 512), AF.Identity, [PB[by + hf], "CW"], [("ysc", ity[0] % 2)], scale=CW[:, jt, e:e + 1])
                        tt("pool", acc[:, jt, hsl], acc[:, jt, hsl], sy, ALU.add, [("ysc", ity[0] % 2), ("acc", jt, hf)], [("acc", jt, hf)])
                    else:
                        stt(acc[:, jt, hsl], bank(by + hf, 512), CW[:, jt, e:e + 1], acc[:, jt, hsl], ALU.mult, ALU.add,
                            [PB[by + hf], "CW", ("acc", jt, hf)], [("acc", jt, hf)])

        pend = None
        for e in range(NE):
            for tb in range(4):
                moe_gu(e, tb, 0)
                if pend is not None:
                    moe_down(*pend)
                if tb == 0 and e + 1 < NE:
                    load_expert(e + 1)
                moe_gu(e, tb, 1)
                pend = (e, tb)
        moe_down(*pend)
        for jt in range(NT_LAT):
            s2 = jt % 2
            dma("sp", hm2[s2], hmid_d[b, jt * 128:(jt + 1) * 128, :], [("hmid", jt)], [("hm2", s2)], "hm2%d" % s2)
            tt("pool", ot[s2], acc[:, jt, :], grow[b][1], ALU.mult, [("acc", jt, 0), ("acc", jt, 1), ("grow", b, 1)], [("ot", s2)])
            tt("dve", ot[s2], ot[s2], hm2[s2], ALU.add, [("ot", s2), ("hm2", s2)], [("ot", s2)])
            dma("sp", out_d[b, jt * 128:(jt + 1) * 128, :], ot[s2], [("ot", s2)], [("out", jt)], "ost%d" % s2)
        S.barrier()
        A.release(mB)

    return finish(nc, S, A)


def finish(nc, S, A):
    print('SBUF arena peak words', A.peak, 'of', A.words)
    S.barrier()
    S.add("sp", lambda e: e.nop(), reads=(), writes=())
    S.emit(None)
    return nc


def _consts():
    ident = np.eye(128, dtype=np.float32)
    r = np.arange(128)
    trif = (r[:, None] <= r[None, :]).astype(np.float32)
    trib = (r[:, None] >= r[None, :]).astype(np.float32)
    half = 64
    inv = (10000.0 ** (-np.arange(0, half, 2, dtype=np.float32) / half)).astype(np.float32)
    tok = np.arange(SEQ)
    row = (tok // 64).astype(np.float32)
    col = (tok % 64).astype(np.float32)
    ang_r = row[:, None] * inv[None, :]
    ang_c = col[:, None] * inv[None, :]
    cr, sr, cc, sc = np.cos(ang_r), np.sin(ang_r), np.cos(ang_c), np.sin(ang_c)
    cos = np.concatenate([cr, cr, cc, cc], axis=1).astype(np.float32)
    sin = np.concatenate([-sr, sr, -sc, sc], axis=1).astype(np.float32)
    cos_t = cos.reshape(NT_LAT, 128, 128).transpose(1, 0, 2).reshape(128, NT_LAT * 128)
    sin_t = sin.reshape(NT_LAT, 128, 128).transpose(1, 0, 2).reshape(128, NT_LAT * 128)
    return ident, trif, trib, np.ascontiguousarray(cos_t), np.ascontiguousarray(sin_t)


def _win_perm():
    idx = []
    for h in range(8):
        for f_ in range(4):
            idx += list(range(f_ * 1024 + h * 128, f_ * 1024 + (h + 1) * 128))
    idx += list(range(4096, 4128))
    idx += list(range(4128, 5152))
    for j in range(2):
        idx += list(range(5152 + j * 128, 5152 + (j + 1) * 128))
        idx += list(range(5408 + j * 128, 5408 + (j + 1) * 128))
    for c in range(8):
        idx += list(range(5664 + c * 128, 5664 + (c + 1) * 128))
        idx += list(range(6688 + c * 128, 6688 + (c + 1) * 128))
    assert len(idx) == INW and len(set(idx)) == INW
    return np.asarray(idx)


def make_in_maps(inputs, NB, cores):
    f = lambda a: np.ascontiguousarray(np.asarray(a, dtype=np.float32))
    ident, trif, trib, cos_t, sin_t = _consts()

    def pk(v):
        return np.ascontiguousarray(np.asarray(v, np.float32).reshape(8, 128).T)

    shared = {
        "w_ada": f(inputs["w_ada"][0]),
        "b_adaT": np.ascontiguousarray(np.asarray(inputs["b_ada"][0], np.float32).reshape(48, 128).T),
        "n1T": pk(inputs["norm1_w"][0]), "n2T": pk(inputs["norm2_w"][0]),
        "w_in": np.ascontiguousarray(np.asarray(inputs["w_in"][0], np.float32)[:, _win_perm()]),
        "b_mgate": f(inputs["b_mgate"][0]).reshape(1, 32),
        "q_norm_w": f(inputs["q_norm_w"][0]).reshape(1, 128),
        "k_norm_w": f(inputs["k_norm_w"][0]).reshape(1, 128),
        "mh_norm_w": f(inputs["mh_norm_w"][0]).reshape(1, D),
        "w_bm": f(inputs["w_branch_m"][0]), "w_ba": f(inputs["w_branch_a"][0]), "w_o": f(inputs["w_out"][0]),
        "w_r": np.ascontiguousarray(np.concatenate([inputs["w_rg"][0], inputs["w_re"][0]], axis=1).astype(np.float32)),
        "b_r": np.ascontiguousarray(np.concatenate([inputs["b_rg"][0], inputs["b_re"][0]]).astype(np.float32).reshape(1, 36)),
        "w_egu": np.ascontiguousarray(np.concatenate([np.asarray(inputs["w_e_gate"][0], np.float32),
                                                      np.asarray(inputs["w_e_up"][0], np.float32)], axis=2)),
        "w_ed": f(inputs["w_e_down"][0]),
        "c_ident": ident, "c_trif": trif, "c_trib": trib, "c_cos": cos_t, "c_sin": sin_t,
    }
    maps = []
    for c in cores:
        bs = slice(c * NB, (c + 1) * NB)
        cv = np.concatenate([np.asarray(inputs["c"], np.float32)[bs], np.asarray(inputs["c_ctx"], np.float32)[None, :]], axis=0)
        NV = NB + 1
        cT = cv.reshape(NV, 8, 128).transpose(2, 1, 0).reshape(128, 8 * NV)
        m = dict(shared)
        m["x"] = f(inputs["x"][bs])
        m["ctxx"] = f(inputs["ctx"][bs])
        m["cT"] = np.ascontiguousarray(cT)
        maps.append(m)
    return maps


_NC_CACHE = {}


def kernel(**inputs):
    NB = 2
    if "full" not in _NC_CACHE:
        _NC_CACHE["full"] = build_program(NB=NB)
    nc = _NC_CACHE["full"]
    maps = make_in_maps(inputs, NB, list(range(N_CORES)))
    res = run_bass_kernel_spmd(nc, maps, core_ids=list(range(N_CORES)))
    out = np.concatenate([np.asarray(r["out"], dtype=np.float32) for r in res.results], axis=0)
    return out
```

```python
import os
import numpy as np
import concourse.bass as bass
import concourse.mybir as mybir
from concourse.bass_utils import run_bass_kernel_spmd

F32 = mybir.dt.float32
BF16 = mybir.dt.bfloat16
AF = mybir.ActivationFunctionType
ALU = mybir.AluOpType
AX = mybir.AxisListType

D = 1024
SEQ = 2048
CTX = 256
NT_LAT = 16
NT = 18
NTOK = NT * 128
INW = 7712
EPS = 1e-6
NEXP = 32
DEXP = 256
N_CORES = 8

ENGS = ("pe", "act", "dve", "pool", "sp")


class _Ins:
    __slots__ = ("eng", "idx", "fn", "deps", "dma", "flag")

    def __init__(self, eng, idx, fn, deps, dma):
        self.eng, self.idx, self.fn, self.deps, self.dma, self.flag = eng, idx, fn, deps, dma, False


class Sched:
    def __init__(self, nc):
        self.nc = nc
        self.ins = {e: [] for e in ENGS}
        self.res = {}
        self.dma_cnt = {}
        self.pending = {e: set() for e in ENGS}
        self.order = []
        self.swq = []
        self.SW_LIMIT = 2600

    def add(self, eng, fn, reads=(), writes=(), dma=None, ndesc=0):
        deps = set(self.pending[eng])
        self.pending[eng] = set()
        if dma is not None and eng == "pool":
            tot = sum(n for _, n in self.swq) + ndesc
            while self.swq and tot > self.SW_LIMIT:
                tk, n = self.swq.pop(0)
                deps.add(tk)
                tot -= n
        raw = set()
        for r in reads:
            st = self.res.get(r)
            if st is not None and st[0] is not None:
                deps.add(st[0])
                raw.add(st[0])
            if st is not None and isinstance(r, str) and r.startswith("pb"):
                deps.update(st[1].values())
        for w in writes:
            st = self.res.get(w)
            if st is not None:
                if st[0] is not None:
                    deps.add(st[0])
                deps.update(st[1].values())
        deps = {(d if d[0] == "E" else ("D", d[1], self.dma_cnt[d[1]])) for d in deps}
        idx = len(self.ins[eng])
        self.order.append((eng, idx))
        if dma is None:
            deps = {d for d in deps if not (d[0] == "E" and d[1] == eng and (eng == "pe" or d not in raw))}
            tok = ("E", eng, idx)
        else:
            c = self.dma_cnt.get(dma, 0) + 1
            self.dma_cnt[dma] = c
            tok = ("D", dma, c)
        if dma is not None and eng == "pool":
            self.swq.append((tok, ndesc))
        ins = _Ins(eng, idx, fn, deps, dma)
        self.ins[eng].append(ins)
        for w in writes:
            self.res[w] = [tok, {}]
        for r in reads:
            st = self.res.setdefault(r, [None, {}])
            st[1][(tok[0], tok[1])] = tok
        return tok

    def barrier(self):
        toks = set()
        for e in ENGS:
            if self.ins[e]:
                last = self.ins[e][-1]
                if last.dma is None:
                    toks.add(("E", e, last.idx))
                else:
                    for j in range(len(self.ins[e]) - 1, -1, -1):
                        if self.ins[e][j].dma is None:
                            toks.add(("E", e, j))
                            break
        for k, c in self.dma_cnt.items():
            toks.add(("D", k, c))
        for e in ENGS:
            self.pending[e] |= toks

    def emit(self, block):
        nc = self.nc
        for e in ENGS:
            for ins in self.ins[e]:
                for d in ins.deps:
                    if d[0] == "E":
                        self.ins[d[1]][d[2]].flag = True
        cnt = {}
        for e in ENGS:
            c = 0
            arr = []
            for ins in self.ins[e]:
                if ins.dma is None and ins.flag:
                    c += 1
                arr.append(c)
            cnt[e] = arr
        esem = {e: nc.alloc_semaphore("s_" + e) for e in ENGS}
        dsem = {k: nc.alloc_semaphore("d_%d" % i) for i, k in enumerate(self.dma_cnt)}
        self.n_waits = 0

        if block is None:
            engobjs = {"pe": nc.tensor, "act": nc.scalar, "dve": nc.vector, "pool": nc.gpsimd, "sp": nc.sync}
            seen_all = {e: {} for e in ENGS}
            for (e, idx) in self.order:
                ins = self.ins[e][idx]
                engobj = engobjs[e]
                seen = seen_all[e]
                need = {}
                for d in ins.deps:
                    if d[0] == "E":
                        key, val = ("E", d[1]), cnt[d[1]][d[2]]
                    else:
                        key, val = ("D", d[1]), 16 * d[2]
                    if val > need.get(key, 0):
                        need[key] = val
                for key, val in need.items():
                    if seen.get(key, 0) < val:
                        sem = esem[key[1]] if key[0] == "E" else dsem[key[1]]
                        engobj.wait_ge(sem, val)
                        seen[key] = val
                        self.n_waits += 1
                inst = ins.fn(engobj)
                if ins.dma is not None:
                    inst.then_inc(dsem[ins.dma], 16)
                elif ins.flag:
                    inst.then_inc(esem[e], 1)
            return

        def run(e, engobj):
            seen = {}
            for ins in self.ins[e]:
                need = {}
                for d in ins.deps:
                    if d[0] == "E":
                        key, val = ("E", d[1]), cnt[d[1]][d[2]]
                    else:
                        key, val = ("D", d[1]), 16 * d[2]
                    if val > need.get(key, 0):
                        need[key] = val
                for key, val in need.items():
                    if seen.get(key, 0) < val:
                        sem = esem[key[1]] if key[0] == "E" else dsem[key[1]]
                        engobj.wait_ge(sem, val)
                        seen[key] = val
                        self.n_waits += 1
                inst = ins.fn(engobj)
                if ins.dma is not None:
                    inst.then_inc(dsem[ins.dma], 16)
                elif ins.flag:
                    inst.then_inc(esem[e], 1)

        block.tensor(lambda t: run("pe", t))
        block.scalar(lambda a: run("act", a))
        block.vector(lambda v: run("dve", v))
        block.gpsimd(lambda g: run("pool", g))
        block.sync(lambda s: run("sp", s))


class Pump:
    def __init__(self, gens, width):
        self.queue = list(gens)
        self.width = width
        self.active = []
        self.rr = 0
        self.completed = 0

    def done(self):
        return not self.queue and not self.active

    def step(self, n=1):
        for _ in range(n):
            while len(self.active) < self.width and self.queue:
                self.active.append(self.queue.pop(0))
            if not self.active:
                return
            self.rr %= len(self.active)
            g = self.active[self.rr]
            try:
                next(g)
                self.rr += 1
            except StopIteration:
                self.active.pop(self.rr)
                self.completed += 1

    def drain(self):
        while not self.done():
            self.step(1)


class Arena:
    def __init__(self, nc, words):
        self.t = nc.alloc_sbuf_tensor("arena", [128, words], F32)
        self.words = words
        self.top = 0
        self.peak = 0

    def mark(self):
        return self.top

    def release(self, m):
        self.top = m

    def alloc(self, nbytes):
        w = (nbytes + 3) // 4
        w = (w + 7) // 8 * 8
        off = self.top
        self.top += w
        self.peak = max(self.peak, self.top)
        assert self.top <= self.words, "SBUF arena overflow %d > %d" % (self.top, self.words)
        return off, w

    def f32(self, n):
        off, w = self.alloc(4 * n)
        return self.t[:, off:off + n]

    def bf16(self, n):
        off, w = self.alloc(2 * n)
        self.last = self.t[:, off:off + w]
        return self.t[:, off:off + w].bitcast(BF16)[:, 0:n]


def build_program(NB=2, stop_after=None, dbg=()):
    nc = bass.Bass("TRN2", target_bir_lowering=False)
    NV = NB + 1

    def din(name, shape, dt=F32):
        return nc.dram_tensor(name, list(shape), dt, kind="ExternalInput").ap()

    x_d = din("x", [NB, SEQ, D])
    ctx_d = din("ctxx", [NB, CTX, D])
    cT_d = din("cT", [128, 8 * NV])
    wada_d = din("w_ada", [D, 6 * D])
    bada_d = din("b_adaT", [128, 48])
    n1_d = din("n1T", [128, 8])
    n2_d = din("n2T", [128, 8])
    win_d = din("w_in", [D, INW])
    bmg_d = din("b_mgate", [1, 32])
    qw_d = din("q_norm_w", [1, 128])
    kw_d = din("k_norm_w", [1, 128])
    mhw_d = din("mh_norm_w", [1, D])
    wbm_d = din("w_bm", [D, D])
    wba_d = din("w_ba", [D, D])
    wo_d = din("w_o", [D, D])
    wr_d = din("w_r", [D, 36])
    br_d = din("b_r", [1, 36])
    wgu_d = din("w_egu", [NEXP, D, 2 * DEXP])
    wed_d = din("w_ed", [NEXP, DEXP, D])
    ident_d = din("c_ident", [128, 128])
    trif_d = din("c_trif", [128, 128])
    trib_d = din("c_trib", [128, 128])
    cos_d = din("c_cos", [128, NT_LAT * 128])
    sin_d = din("c_sin", [128, NT_LAT * 128])
    out_d = nc.dram_tensor("out", [NB, SEQ, D], F32, kind="ExternalOutput").ap()
    hmid_d = nc.dram_tensor("hmid_scr", [NB, SEQ, D], F32, kind="Internal").ap()
    otscr_d = nc.dram_tensor("ot_scr", [NB, 128, 4 * SEQ], F32, kind="Internal").ap()
    dbg_d = {}
    for name, shape in dbg:
        dbg_d[name] = nc.dram_tensor("dbg_" + name, list(shape), F32, kind="ExternalOutput").ap()

    S = Sched(nc)
    A = Arena(nc, 53180)
    psum = nc.alloc_psum_tensor("psum", [128, 4096], F32)

    def bank(i, n=512, off=0):
        return psum[:, i * 512 + off: i * 512 + off + n]

    def bank_bf(i, n=1024, off=0):
        return psum[:, i * 512:(i + 1) * 512].bitcast(BF16)[:, off:off + n]

    PB = ["pb%d" % i for i in range(8)]

    win_v = win_d.rearrange("(k p) c -> p k c", p=128)

    def dma(eng, out, in_, reads, writes, sem):
        def nd(ap):
            sh = list(ap.shape)
            n = 1
            for v_ in sh[:-1]:
                n *= v_
            return n
        ndesc = max(nd(out), nd(in_))
        S.add(eng, lambda e: e.dma_start(out=out, in_=in_), reads=reads, writes=writes, dma=sem, ndesc=ndesc)

    def mm(out, lhsT, rhs, start, stop, reads, writes):
        S.add("pe", lambda e: e.matmul(out, lhsT=lhsT, rhs=rhs, start=start, stop=stop), reads=reads, writes=writes)

    def tr(out, in_, ident, reads, writes):
        S.add("pe", lambda e: e.transpose(out, in_, ident), reads=reads, writes=writes)

    def act(out, in_, func, reads, writes, bias=None, scale=None, accum_out=None):
        kw = {}
        if bias is not None:
            kw["bias"] = bias
        if scale is not None:
            kw["scale"] = scale
        if accum_out is not None:
            kw["accum_out"] = accum_out
        S.add("act", lambda e: e.activation(out=out, in_=in_, func=func, **kw), reads=reads, writes=writes)

    def ts(eng, out, in0, s1, s2, op0, op1, reads, writes):
        if op1 is None:
            S.add(eng, lambda e: e.tensor_scalar(out=out, in0=in0, scalar1=s1, scalar2=None, op0=op0), reads=reads, writes=writes)
        else:
            S.add(eng, lambda e: e.tensor_scalar(out=out, in0=in0, scalar1=s1, scalar2=s2, op0=op0, op1=op1), reads=reads, writes=writes)

    def tt(eng, out, in0, in1, op, reads, writes):
        S.add(eng, lambda e: e.tensor_tensor(out=out, in0=in0, in1=in1, op=op), reads=reads, writes=writes)

    def stt(out, in0, scalar, in1, op0, op1, reads, writes):
        S.add("dve", lambda e: e.scalar_tensor_tensor(out=out, in0=in0, scalar=scalar, in1=in1, op0=op0, op1=op1), reads=reads, writes=writes)

    def recip(out, in_, reads, writes):
        S.add("dve", lambda e: e.reciprocal(out=out, in_=in_), reads=reads, writes=writes)

    def copy(eng, out, in_, reads, writes):
        if eng == "act":
            S.add("act", lambda e: e.activation(out=out, in_=in_, func=AF.Copy), reads=reads, writes=writes)
        else:
            S.add(eng, lambda e: e.tensor_copy(out=out, in_=in_), reads=reads, writes=writes)

    def memset(eng, ap, val, writes):
        S.add(eng, lambda e: e.memset(ap, val), writes=writes)

    def reduce(out, in_, axis, op, reads, writes):
        S.add("dve", lambda e: e.tensor_reduce(out=out, in_=in_, axis=axis, op=op), reads=reads, writes=writes)

    def dump(name, src_ap, reads, dst=None):
        if name in dbg_d:
            d = dbg_d[name] if dst is None else dst
            dma("pool", d, src_ap, reads, [("dbg", name)], "dbg_" + name)

    ident_f = A.f32(128)
    trif = A.f32(128)
    trib = A.f32(128)
    ones_f = A.f32(128)
    ident_b = A.bf16(128)
    ones_b = A.bf16(128)
    bmg_row = A.f32(32)
    qw_row = A.f32(128)
    kw_row = A.f32(128)
    mhw_row = A.f32(D)
    br_row = A.f32(36)
    n1T = A.f32(8)
    n2T = A.f32(8)
    badaT = A.f32(48)
    modT = A.f32(48 * NV)
    modT3 = modT.rearrange("p (j v) -> p j v", v=NV)
    A1 = A.f32(8 * NV)
    A2 = A.f32(8 * NV)
    grow1 = [A.f32(D) for _ in range(2)]
    grow = [grow1 for _ in range(NB)]

    dma("sp", ident_f, ident_d, [], ["ident_f"], "c0")
    dma("sp", trif, trif_d, [], ["trif"], "c0")
    dma("sp", trib, trib_d, [], ["trib"], "c0")
    dma("pool", ident_b, ident_d, [], ["ident_b"], "c1")
    dma("sp", bmg_row, bmg_d.partition_broadcast(128), [], ["bmg"], "c0")
    dma("sp", qw_row, qw_d.partition_broadcast(128), [], ["qw"], "c0")
    dma("sp", kw_row, kw_d.partition_broadcast(128), [], ["kw"], "c0")
    dma("sp", mhw_row, mhw_d.partition_broadcast(128), [], ["mhw"], "c0")
    dma("sp", br_row, br_d.partition_broadcast(128), [], ["br"], "c0")
    dma("sp", n1T, n1_d, [], ["n1T"], "c0")
    dma("sp", n2T, n2_d, [], ["n2T"], "c0")
    dma("sp", badaT, bada_d, [], ["badaT"], "c0")
    memset("dve", ones_f, 1.0, ["ones_f"])
    memset("dve", ones_b, 1.0, ["ones_b"])

    mA = A.mark()
    cT = A.f32(8 * NV)
    scT = A.f32(8 * NV)
    wa = [A.f32(8 * 1024).rearrange("p (k c) -> p k c", k=8) for _ in range(2)]
    diag = A.f32(D)
    dma("sp", cT, cT_d, [], ["cT"], "c0")
    act(scT, cT, AF.Silu, ["cT"], ["scT"])
    wada_v = wada_d.rearrange("(k p) c -> p k c", p=128)
    for blk in range(6):
        sl = blk % 2
        for k in range(8):
            dma(("sp", "act")[k % 2] if os.environ.get("KADA2", "1") == "1" else "sp", wa[sl][:, k, :],
                wada_v[:, k, blk * 1024:(blk + 1) * 1024], [], [("wa", sl)], "wa%d" % sl)
        for j in range(8):
            col = (blk * 8 + j) * NV
            for k in range(8):
                if os.environ.get("KSKIPA") and not (k == 0 or k == 7):
                    continue
                mm(bank(0, NV, col), wa[sl][:, k, j * 128:(j + 1) * 128], scT[:, k * NV:(k + 1) * NV],
                   k == 0, k == 7, [("wa", sl), "scT"], [PB[0]])
    tt("dve", modT3, bank(0, 48 * NV).rearrange("p (j v) -> p j v", v=NV),
       badaT.unsqueeze(2).to_broadcast([128, 48, NV]), ALU.add, [PB[0], "badaT"], ["modT"])
    for v in range(NV):
        stt(A1[:, v * 8:(v + 1) * 8], modT3[:, 8:16, v], 1.0, n1T, ALU.add, ALU.mult, ["modT", "n1T"], ["A1"])
        stt(A2[:, v * 8:(v + 1) * 8], modT3[:, 32:40, v], 1.0, n2T, ALU.add, ALU.mult, ["modT", "n2T"], ["A2"])
    if "modT" in dbg_d:
        dump("modT", modT, ["modT"])
    S.barrier()
    A.release(mA)
    if stop_after == "A":
        return finish(nc, S, A)

    for b in range(NB):
        mB = A.mark()
        xmodT = A.bf16(8 * NTOK).rearrange("p (k t) -> p k t", k=8)

        m1 = A.mark()
        diag = A.f32(D)
        for gi, base in enumerate((16, 40)):
            for k in range(8):
                ts("dve", diag[:, k * 128:(k + 1) * 128], ident_f, modT3[:, base + k, b:b + 1], None, ALU.mult, None,
                   ["ident_f", "modT"], ["diag"])
            for hf in range(2):
                mm(bank(hf), ones_f, diag[:, hf * 512:(hf + 1) * 512], True, True, ["ones_f", "diag"], [PB[hf]])
                copy("act", grow[b][gi][:, hf * 512:(hf + 1) * 512], bank(hf), [PB[hf]], [("grow", b, gi)])
        xt = [A.f32(D) for _ in range(3)]
        junk = A.bf16(D)
        xn = [A.bf16(D) for _ in range(2)]
        ss = [A.f32(1) for _ in range(2)]
        sv = [A.f32(1) for _ in range(2)]
        def gen_p1(t):
            s3, s2 = t % 3, t % 2
            src = ctx_d[b, t * 128:(t + 1) * 128, :] if t < 2 else x_d[b, (t - 2) * 128:(t - 1) * 128, :]
            vec = NB if t < 2 else b
            dma("sp", xt[s3], src, [], [("xt", s3)], "xt%d" % s3)
            yield
            act(junk, xt[s3], AF.Square, [("xt", s3)], ["junk", ("ss", s2)], accum_out=ss[s2])
            yield
            ts("dve", sv[s2], ss[s2], 1.0 / D, EPS, ALU.mult, ALU.add, [("ss", s2)], [("sv", s2)])
            yield
            act(sv[s2], sv[s2], AF.Sqrt, [("sv", s2)], [("sv", s2)])
            yield
            recip(sv[s2], sv[s2], [("sv", s2)], [("sv", s2)])
            yield
            ts("pool", xn[s2], xt[s3], sv[s2], 1.0, ALU.mult, ALU.mult, [("xt", s3), ("sv", s2)], [("xn", s2)])
            yield
            pbk = 2 + s2
            for k in range(8):
                tr(bank_bf(pbk, 128, k * 128), xn[s2][:, k * 128:(k + 1) * 128], ident_b, [("xn", s2), "ident_b"], [PB[pbk]])
            yield
            yield
            for k in range(8):
                o = xmodT[:, k, t * 128:(t + 1) * 128]
                i_ = bank_bf(pbk, 128, k * 128)
                if t % 2 == 0:
                    act(o, i_, AF.Identity, [PB[pbk], "A1", "modT"], [("xmodT", t)],
                        bias=modT3[:, k, vec:vec + 1], scale=A1[:, vec * 8 + k:vec * 8 + k + 1])
                else:
                    ts("dve", o, i_, A1[:, vec * 8 + k:vec * 8 + k + 1], modT3[:, k, vec:vec + 1], ALU.mult, ALU.add,
                       [PB[pbk], "A1", "modT"], [("xmodT", t)])
                if k % 4 == 3:
                    yield

        Pump([gen_p1(t) for t in range(NT)], 2).drain()
        XM_ALL = [("xmodT", t) for t in range(NT)]
        if b == 0 and "xmodT" in dbg_d:
            for k in range(8):
                dump("xmodT", xmodT[:, k, :], XM_ALL, dst=dbg_d["xmodT"][k * 128:(k + 1) * 128, :])
        S.barrier()
        A.release(m1)
        if stop_after == "1":
            return finish(nc, S, A)


        m3 = A.mark()
        oT = A.bf16(8 * SEQ).rearrange("p (k t) -> p k t", k=8)
        oT_w = A.last
        COSt = A.f32(NT_LAT * 128).rearrange("p (j c) -> p j c", c=128)
        SINt = A.f32(NT_LAT * 128).rearrange("p (j c) -> p j c", c=128)
        waq = [A.bf16(8 * 512).rearrange("p (k c) -> p k c", k=8) for _ in range(2)]
        wakv = [A.bf16(8 * 256).rearrange("p (k c) -> p k c", k=8) for _ in range(2)]
        kTa = [A.bf16(NTOK) for _ in range(2)]
        Va = [A.bf16(NT * 128).rearrange("p (t c) -> p t c", c=128) for _ in range(2)]
        qTa = [A.bf16(4 * SEQ).rearrange("p (g t) -> p g t", g=4) for _ in range(2)]
        qs = [A.f32(512) for _ in range(2)]
        qn = [A.f32(512) for _ in range(2)]
        qt1 = [A.f32(512) for _ in range(2)]
        qr = [A.bf16(512) for _ in range(2)]
        ssq4 = [A.f32(4) for _ in range(2)]
        kq = [A.f32(128) for _ in range(2)]
        kt1 = [A.f32(128) for _ in range(2)]
        kt2 = [A.f32(128) for _ in range(2)]
        kr = [A.bf16(128) for _ in range(2)]
        kjunk = A.f32(128)
        ssk = [A.f32(1) for _ in range(2)]
        Pt = [A.bf16(512) for _ in range(3)]
        rD = [A.f32(512) for _ in range(2)]
        DACC = os.environ.get("KDACC", "0") == "1"
        Pacc = [[A.f32(512) for _ in range(2)] for _ in range(2)] if DACC else None
        SC = float(128 ** -0.5)
        dma("sp", COSt.rearrange("p j c -> p (j c)"), cos_d, [], ["COS"], "cos")
        dma("sp", SINt.rearrange("p j c -> p (j c)"), sin_d, [], ["SIN"], "sin")

        def halves(ap2d, g=None):
            if g is None:
                return ap2d.rearrange("p (a f c) -> p a f c", a=2, f=2)
            return ap2d.rearrange("p (g a f c) -> p g a f c", g=g, a=2, f=2)

        def load_aw(j):
            dma("pool", waq[j], win_v[:, :, 4128 + j * 512: 4128 + (j + 1) * 512], [], [("waq", j)], "waq%d" % j)
            dma("pool", wakv[j], win_v[:, :, 5152 + j * 256: 5152 + (j + 1) * 256], [], [("wakv", j)], "wakv%d" % j)

        KYLD = int(os.environ.get("KYLD", "1"))
        KGAP = int(os.environ.get("KGAP", "4"))
        def gen_k(j, t):
            bk, s2 = 5, t % 2
            ko = s2 * 256
            for k in range(8):
                mm(bank(bk, 256, ko), xmodT[:, k, t * 128:(t + 1) * 128], wakv[j][:, k, :], k == 0, k == 7,
                   [("xmodT", t), ("wakv", j)], [PB[bk]])
            for _y in range(KYLD):
                yield
            act(kjunk, bank(bk, 128, ko), AF.Square, [PB[bk]], ["kjunk", ("ssk", s2)], accum_out=ssk[s2])
            for _y in range(KYLD):
                yield
            copy("act", Va[j][:, t, :], bank(bk, 128, ko + 128), [PB[bk]], [("Va", j, t)])
            for _y in range(KYLD):
                yield
            ts("dve", ssk[s2], ssk[s2], 1.0 / 128, EPS, ALU.mult, ALU.add, [("ssk", s2)], [("ssk", s2)])
            for _y in range(KYLD):
                yield
            act(ssk[s2], ssk[s2], AF.Sqrt, [("ssk", s2)], [("ssk", s2)])
            for _y in range(KYLD):
                yield
            recip(ssk[s2], ssk[s2], [("ssk", s2)], [("ssk", s2)])
            for _y in range(KYLD):
                yield
            if t < 2:
                stt(kr[s2], bank(bk, 128, ko), ssk[s2], kw_row, ALU.mult, ALU.mult, [PB[bk], ("ssk", s2), "kw"], [("kr", s2)])
                yield
            else:
                jt = t - 2
                stt(kq[s2], bank(bk, 128, ko), ssk[s2], kw_row, ALU.mult, ALU.mult, [PB[bk], ("ssk", s2), "kw"], [("kq", s2)])
                yield
                tt("pool", kt1[s2], kq[s2], COSt[:, jt, :], ALU.mult, [("kq", s2), "COS"], [("kt1", s2)])
                yield
                kq4, kt24, sn4 = halves(kq[s2]), halves(kt2[s2]), halves(SINt[:, jt, :])
                tt("pool", kt24[:, :, 0, :], kq4[:, :, 1, :], sn4[:, :, 0, :], ALU.mult, [("kq", s2), "SIN"], [("kt2", s2)])
                yield
                tt("dve", kt24[:, :, 1, :], kq4[:, :, 0, :], sn4[:, :, 1, :], ALU.mult, [("kq", s2), "SIN"], [("kt2", s2)])
                yield
                tt("dve", kr[s2], kt1[s2], kt2[s2], ALU.add, [("kt1", s2), ("kt2", s2)], [("kr", s2)])
                yield
            for _ in range(KGAP):
                yield
            tr(bank_bf(7, 128, 512 + s2 * 128), kr[s2], ident_b, [("kr", s2), "ident_b"], [PB[7]])
            copy("dve", kTa[j][:, t * 128:(t + 1) * 128], bank_bf(7, 128, 512 + s2 * 128), [PB[7]], [("kTa", j, t)])
            for _y in range(KYLD):
                yield

        def gen_q(j, jt):
            t = jt + 2
            bk, s2 = 6, jt % 2
            for k in range(8):
                mm(bank(bk, 512), xmodT[:, k, t * 128:(t + 1) * 128], waq[j][:, k, :], k == 0, k == 7,
                   [("xmodT", t), ("waq", j)], [PB[bk]])
            copy("act", qs[s2], bank(bk, 512), [PB[bk]], [("qs", s2)])
            for _y in range(KYLD):
                yield
            tt("dve", qt1[s2], qs[s2], qs[s2], ALU.mult, [("qs", s2)], [("qt1", s2)])
            for _y in range(KYLD):
                yield
            reduce(ssq4[s2], qt1[s2].rearrange("p (g c) -> p g c", g=4), AX.X, ALU.add, [("qt1", s2)], [("ssq4", s2)])
            for _y in range(KYLD):
                yield
            ts("dve", ssq4[s2], ssq4[s2], 1.0 / 128, EPS, ALU.mult, ALU.add, [("ssq4", s2)], [("ssq4", s2)])
            for _y in range(KYLD):
                yield
            act(ssq4[s2], ssq4[s2], AF.Sqrt, [("ssq4", s2)], [("ssq4", s2)])
            for _y in range(KYLD):
                yield
            recip(ssq4[s2], ssq4[s2], [("ssq4", s2)], [("ssq4", s2)])
            for _y in range(KYLD):
                yield
            qs3 = qs[s2].rearrange("p (g c) -> p g c", g=4)
            qn3 = qn[s2].rearrange("p (g c) -> p g c", g=4)
            tt("dve", qn3, qs3, ssq4[s2].unsqueeze(2).to_broadcast([128, 4, 128]), ALU.mult, [("qs", s2), ("ssq4", s2)], [("qn", s2)])
            for _y in range(KYLD):
                yield
            tt("pool", qn3, qn3, qw_row.unsqueeze(1).to_broadcast([128, 4, 128]), ALU.mult, [("qn", s2), "qw"], [("qn", s2)])
            for _y in range(KYLD):
                yield
            tt("pool", qt1[s2].rearrange("p (g c) -> p g c", g=4), qn3,
               COSt[:, jt, :].unsqueeze(1).to_broadcast([128, 4, 128]), ALU.mult, [("qn", s2), "COS"], [("qt1", s2)])
            for _y in range(KYLD):
                yield
            qn5, qt25, sn4 = halves(qn[s2], 4), halves(qs[s2], 4), halves(SINt[:, jt, :])
            tt("dve", qt25[:, :, :, 0, :], qn5[:, :, :, 1, :], sn4[:, :, 0, :].unsqueeze(1).to_broadcast([128, 4, 2, 32]),
               ALU.mult, [("qn", s2), "SIN"], [("qs", s2)])
            for _y in range(KYLD):
                yield
            tt("pool", qt25[:, :, :, 1, :], qn5[:, :, :, 0, :], sn4[:, :, 1, :].unsqueeze(1).to_broadcast([128, 4, 2, 32]),
               ALU.mult, [("qn", s2), "SIN"], [("qs", s2)])
            for _y in range(KYLD):
                yield
            tt("dve", qr[s2], qt1[s2], qs[s2], ALU.add, [("qt1", s2), ("qs", s2)], [("qr", s2)])
            for _y in range(KYLD):
                yield
            for _ in range(KGAP):
                yield
            for g in range(4):
                tr(bank_bf(7, 128, g * 128), qr[s2][:, g * 128:(g + 1) * 128], ident_b, [("qr", s2), "ident_b"], [PB[7]])
            copy("act", qTa[j][:, :, jt * 128:(jt + 1) * 128], bank_bf(7, 512, 0).rearrange("p (g c) -> p g c", g=4),
                 [PB[7]], [("qTa", j, jt // 4)])
            for _y in range(KYLD):
                yield

        def prep_pumps(j):
            return [Pump([gen_k(j, t) for t in range(NT)], 2), Pump([gen_q(j, jt) for jt in range(NT_LAT)], 2)]

        def core_block(j, g, qb, itn, pumps):
            bO = 2
            bD = 4
            qsl = qTa[j][:, g, qb * 512:(qb + 1) * 512]

            def s_mm(kt):
                bS = (0, 1, 3)[kt % 3]
                mm(bank(bS, 512), kTa[j][:, kt * 128:(kt + 1) * 128], qsl, True, True, [("kTa", j, kt), ("qTa", j, qb)], [PB[bS]])
                act(Pt[kt % 3], bank(bS, 512), AF.Exp, [PB[bS]], [("Pt", kt % 3)], scale=SC)

            s_mm(0)
            s_mm(1)
            for kt in range(NT):
                if kt + 2 < NT:
                    s_mm(kt + 2)
                mm(bank(bO, 512), Va[j][:, kt, :], Pt[kt % 3], kt == 0, kt == NT - 1, [("Va", j, kt), ("Pt", kt % 3)], [PB[bO]])
                if not DACC:
                    mm(bank(bD, 512), ones_b, Pt[kt % 3], kt == 0, kt == NT - 1, ["ones_b", ("Pt", kt % 3)], [PB[bD]])
                else:
                    ae, ai = ("pool", 1) if kt % 3 == 2 else ("dve", 0)
                    pa = Pacc[itn % 2][ai]
                    ka = ("Pacc", itn % 2, ai)
                    if kt == 0 or kt == 2:
                        copy(ae, pa, Pt[kt % 3], [("Pt", kt % 3)], [ka])
                    else:
                        tt(ae, pa, pa, Pt[kt % 3], ALU.add, [ka, ("Pt", kt % 3)], [ka])
                for p_ in pumps:
                    p_.step(int(os.environ.get("KPS", "2")))
            if DACC:
                mm(bank(bD, 512), ones_f, Pacc[itn % 2][0], True, False, ["ones_f", ("Pacc", itn % 2, 0)], [PB[bD]])
                mm(bank(bD, 512), ones_f, Pacc[itn % 2][1], False, True, ["ones_f", ("Pacc", itn % 2, 1)], [PB[bD]])
            r2 = itn % 2
            recip(rD[r2], bank(bD, 512), [PB[bD]], [("rD", r2)])
            tt("dve", oT[:, j * 4 + g, qb * 512:(qb + 1) * 512], bank(bO, 512), rD[r2], ALU.mult,
               [PB[bO], ("rD", r2)], [("oT", j * 4 + g)])

        load_aw(0)
        load_aw(1)
        kpump = Pump([gen_k(j_, t) for j_ in range(2) for t in range(NT)], 2)
        qpump = Pump([gen_q(j_, jt) for j_ in range(2) for jt in range(NT_LAT)], 2)
        itn = 0
        pumps = [kpump, qpump]
        for j in range(2):
            for qb in range(4):
                while kpump.completed < NT * (j + 1) or qpump.completed < NT_LAT * j + 4 * (qb + 1):
                    if kpump.completed < NT * (j + 1):
                        kpump.step(1)
                    if qpump.completed < NT_LAT * j + 4 * (qb + 1):
                        qpump.step(1)
                for g in range(4):
                    if os.environ.get("KSKIPCORE") is None:
                        core_block(j, g, qb, itn, pumps)
                    itn += 1
        for p_ in pumps:
            p_.drain()
        OT_ALL = [("oT", h) for h in range(8)]
        if b == 0 and "oT" in dbg_d:
            for h in range(8):
                dump("oT", oT[:, h, :], OT_ALL, dst=dbg_d["oT"][h * 128:(h + 1) * 128, :])
        for h in range(8):
            dma("sp", otscr_d[b, :, h * (SEQ // 2):(h + 1) * (SEQ // 2)], oT_w[:, h * (SEQ // 2):(h + 1) * (SEQ // 2)],
                [("oT", h)], [("otscr", h)], "otst")
        S.barrier()
        A.release(m3)
        if stop_after == "3":
            return finish(nc, S, A)

        hmgT = A.bf16(8 * SEQ).rearrange("p (k t) -> p k t", k=8)
        m2 = A.mark()
        wgt = A.bf16(8 * 32).rearrange("p (k c) -> p k c", k=8)
        AA = [A.f32(NT * 8).rearrange("p (t h) -> p t h", h=8) for _ in range(2)]
        FL = [A.f32(NT * 8).rearrange("p (t h) -> p t h", h=8) for _ in range(2)]
        EB = [A.f32(NT * 8).rearrange("p (t h) -> p t h", h=8) for _ in range(2)]
        wsl_flat = [A.bf16(8 * 512).rearrange("p (k c) -> p k c", k=8) for _ in range(2)]
        wsl = [w_.rearrange("p k (f c) -> p k f c", f=4) for w_ in wsl_flat]
        qT = [A.bf16(NTOK) for _ in range(2)]
        kT = [A.bf16(NTOK) for _ in range(2)]
        Kt = [A.bf16(NT * 128).rearrange("p (t c) -> p t c", c=128) for _ in range(2)]
        Vv = [A.bf16(NT * 130).rearrange("p (t c) -> p t c", c=130) for _ in range(2)]
        vT = A.bf16(NTOK)
        ogT = [A.bf16(SEQ) for _ in range(2)]
        Tst = [A.f32(136) for _ in range(2)]
        VP = [A.bf16(NT * 130).rearrange("p (t c) -> p t c", c=130) for _ in range(2)]
        CBs = [A.bf16(NT * 130).rearrange("p (t c) -> p t c", c=130) for _ in range(2)]
        W_OUT = 6
        SpS = [A.bf16(128) for _ in range(W_OUT)]
        denS = [A.f32(1) for _ in range(W_OUT)]
        ssq = A.f32(NT_LAT)
        rs16 = A.f32(NT_LAT)
        hg4 = [A.bf16(128) for _ in range(4)]
        sqj = A.f32(128)
        m2g = A.mark()
        G = A.f32(NT * 32).rearrange("p (t c) -> p t c", c=32)
        SP_ = [A.f32(NT * 8).rearrange("p (t h) -> p t h", h=8) for _ in range(2)]
        tmpA = A.f32(NT * 8).rearrange("p (t h) -> p t h", h=8)
        tri = [trif, trib]
        trik = ["trif", "trib"]

        dma("pool", wgt, win_v[:, :, 4096:4128], [], ["wgt"], "wgt")
        for hb in range(2):
            memset("pool", Vv[hb][:, :, 128:129], 1.0, [("Vv1", hb)])
        for t in range(NT):
            bk, col = (0, t * 32) if t < 16 else (1, (t - 16) * 32)
            for k in range(8):
                mm(bank(bk, 32, col), xmodT[:, k, t * 128:(t + 1) * 128], wgt[:, k, :], k == 0, k == 7,
                   [("xmodT", t), "wgt"], [PB[bk]])
        if stop_after == "2a1":
            return finish(nc, S, A)
        tt("dve", G[:, 0:16, :], bank(0).rearrange("p (t c) -> p t c", c=32),
           bmg_row.unsqueeze(1).to_broadcast([128, 16, 32]), ALU.add, [PB[0], "bmg"], ["G"])
        tt("dve", G[:, 16:18, :], bank(1, 64).rearrange("p (t c) -> p t c", c=32),
           bmg_row.unsqueeze(1).to_broadcast([128, 2, 32]), ALU.add, [PB[1], "bmg"], ["G"])
        if stop_after == "2a2":
            return finish(nc, S, A)
        for d_ in range(2):
            fo = 8 + 16 * d_
            io = 16 * d_
            act(tmpA, G[:, :, fo:fo + 8], AF.Exp, ["G"], ["tmpA"], scale=-1.0)
            act(SP_[d_], tmpA, AF.Ln, ["tmpA"], [("SP", d_)], bias=1.0)
            spf = SP_[d_].rearrange("p t h -> p (t h)")
            mm(bank(2, 144, 0), tri[d_], spf, True, True, [trik[d_], ("SP", d_)], [PB[2]])
            mm(bank(3, 144, 0), ones_f, spf, True, True, ["ones_f", ("SP", d_)], [PB[3]])
            cum3 = bank(2, 144, 0).rearrange("p (t h) -> p t h", h=8)
            tot3 = bank(3, 144, 0).rearrange("p (t h) -> p t h", h=8)
            tt("dve", tmpA, G[:, :, io:io + 8], cum3, ALU.add, ["G", PB[2]], ["tmpA"])
            if stop_after == "2a3":
                return finish(nc, S, A)
            act(AA[d_], tmpA, AF.Exp, ["tmpA"], [("AA", d_)])
            act(FL[d_], cum3, AF.Exp, [PB[2]], [("FL", d_)])
            act(EB[d_], tot3, AF.Exp, [PB[3]], [("EB", d_)], scale=-1.0)
        if b == 0:
            dump("G", G.rearrange("p t c -> p (t c)"), ["G"])
            for d_ in range(2):
                dump("AA%d" % d_, AA[d_].rearrange("p t h -> p (t h)"), [("AA", d_)])
                dump("FL%d" % d_, FL[d_].rearrange("p t h -> p (t h)"), [("FL", d_)])
                dump("EB%d" % d_, EB[d_].rearrange("p t h -> p (t h)"), [("EB", d_)])
        if stop_after == "2a":
            return finish(nc, S, A)
        S.barrier()
        A.release(m2g)
        Hs = [A.f32(NT_LAT * 128).rearrange("p (t c) -> p t c", c=128) for _ in range(2)]
        order = [list(range(NT)), [1, 0] + list(range(NT - 1, 1, -1))]
        KDIR = os.environ.get("KDIR")
        KDIR = int(KDIR) if KDIR is not None else None
        QS = float(128 ** -0.5)

        def load_head_w(h):
            sl = h % 2
            dma("pool", wsl_flat[sl], win_v[:, :, h * 512:(h + 1) * 512], [], [("wsl", sl)], "wsl%d" % sl)

        def gen_proj(h):
            sl = h % 2
            hb = h % 2
            W = wsl[sl]
            wk_ = ("wsl", sl)
            cbs = [(0, 512), (512, 512), (1024, 512), (1536, 512), (2048, 256)]
            n = 0
            for ci, (c0, cn) in enumerate(cbs):
                tl = [("xmodT", t) for t in range(c0 // 128, (c0 + cn) // 128)]
                for f_, dst, nm in ((0, qT[hb], ("qT", hb, ci)), (1, kT[hb], ("kT", hb, ci)), (2, vT, ("vT", ci))):
                    bk = 6 + n % 2
                    n += 1
                    for k in range(8):
                        mm(bank(bk, cn), W[:, k, f_, :], xmodT[:, k, c0:c0 + cn], k == 0, k == 7, tl + [wk_], [PB[bk]])
                    if f_ == 0:
                        act(dst[:, c0:c0 + cn], bank(bk, cn), AF.Copy, [PB[bk]], [nm], scale=QS)
                    elif f_ == 1:
                        copy("dve", dst[:, c0:c0 + cn], bank(bk, cn), [PB[bk]], [nm])
                    else:
                        copy("act", dst[:, c0:c0 + cn], bank(bk, cn), [PB[bk]], [nm])
                    yield
                nt_ = cn // 128
                t0 = c0 // 128
                for src, dst3, nm in ((kT[hb], Kt[hb], "Kt"), (vT, Vv[hb], "Vv")):
                    bk = 6 + n % 2
                    n += 1
                    rk = ("kT", hb, ci) if nm == "Kt" else ("vT", ci)
                    for q4 in range(nt_):
                        tr(bank_bf(bk, 128, q4 * 128), src[:, c0 + q4 * 128:c0 + (q4 + 1) * 128], ident_b, [rk, "ident_b"], [PB[bk]])
                    copy("dve" if nm == "Kt" else "act", dst3[:, t0:t0 + nt_, 0:128],
                         bank_bf(bk, nt_ * 128, 0).rearrange("p (t c) -> p t c", c=128), [PB[bk]],
                         [(nm, hb, t0 + q4) for q4 in range(nt_)])
                    yield
            for g4 in range(4):
                bk = 6 + n % 2
                n += 1
                xsl = slice(256 + g4 * 512, 256 + (g4 + 1) * 512)
                tl = [("xmodT", 2 + g4 * 4 + q4) for q4 in range(4)]
                for k in range(8):
                    mm(bank(bk, 512), W[:, k, 3, :], xmodT[:, k, xsl], k == 0, k == 7, tl + [wk_], [PB[bk]])
                act(ogT[hb][:, g4 * 512:(g4 + 1) * 512], bank(bk, 512), AF.Sigmoid, [PB[bk]], [("ogT", hb, g4)])
                yield
            if h + 2 < 8:
                load_head_w(h + 2)

        DC_REG = [[(0, 0), (1, 0)], [(2, 0), (3, 0)]]
        S_REG = [(0, 0), (0, 128), (0, 256), (0, 384), (1, 0), (1, 128)]
        X_REG = [(2, 0), (2, 136), (2, 272), (3, 0), (3, 136), (3, 272)]

        def gen_state(h, d_):
            hb = h % 2
            for i in range(NT):
                t = order[d_][i]
                ts("pool", VP[d_][:, t, 0:129], Vv[hb][:, t, 0:129], AA[d_][:, t, h:h + 1], 1.0, ALU.mult, ALU.mult,
                   [("Vv", hb, t), ("Vv1", hb), ("AA", d_)], [("VP", d_, t)])
                yield
                if i < NT - 1:
                    bC, cC = DC_REG[d_][i % 2]
                    kC = PB[bC]
                    mm(bank(bC, 129, cC), Kt[hb][:, t, :], VP[d_][:, t, 0:129], True, True, [("Kt", hb, t), ("VP", d_, t)], [kC])
                    yield
                    if i == 0:
                        copy("dve", Tst[d_][:, 0:129], bank(bC, 129, cC), [kC], [("T", d_)])
                    else:
                        tp = order[d_][i - 1]
                        stt(Tst[d_][:, 0:129], Tst[d_][:, 0:129], EB[d_][:, tp, h:h + 1], bank(bC, 129, cC), ALU.mult, ALU.add,
                            [("T", d_), ("EB", d_), kC], [("T", d_)])
                    yield
                    tn = order[d_][i + 1]
                    act(CBs[d_][:, tn, 0:129], Tst[d_][:, 0:129], AF.Identity, [("T", d_), ("EB", d_)], [("CB", d_, tn)],
                        scale=EB[d_][:, t, h:h + 1])
                    yield

        def gen_out(h, d_, t, slot):
            hb = h % 2
            ci = min(t // 4, 4)
            tsl = slice(t * 128, (t + 1) * 128)
            bS, cS = S_REG[slot]
            kS = PB[bS]
            mm(bank(bS, 128, cS), kT[hb][:, tsl], qT[hb][:, tsl], True, True, [("kT", hb, ci), ("qT", hb, ci)], [kS])
            yield
            tt("dve", SpS[slot], bank(bS, 128, cS), tri[d_], ALU.mult, [kS, trik[d_]], [("SpS", slot)])
            yield
            bX, cX = X_REG[slot]
            mm(bank(bX, 129, cX), SpS[slot], VP[d_][:, t, 0:129], True, False, [("SpS", slot), ("VP", d_, t)], [PB[bX]])
            mm(bank(bX, 129, cX), qT[hb][:, tsl], CBs[d_][:, t, 0:129], False, True, [("qT", hb, ci), ("CB", d_, t)], [PB[bX]])
            yield
            act(denS[slot], bank(bX, 1, cX + 128), AF.Abs, [PB[bX]], [("den", slot)])
            yield
            ts("dve", denS[slot], denS[slot], FL[d_][:, t, h:h + 1], None, ALU.max, None,
               [("den", slot), ("FL", d_)], [("den", slot)])
            yield
            recip(denS[slot], denS[slot], [("den", slot)], [("den", slot)])
            yield
            if d_ == 0:
                act(Hs[hb][:, t - 2, :], bank(bX, 128, cX), AF.Identity, [PB[bX], ("den", slot)], [("Hs", hb, t - 2)],
                    scale=denS[slot])
            else:
                stt(Hs[hb][:, t - 2, :], bank(bX, 128, cX), denS[slot], Hs[hb][:, t - 2, :], ALU.mult, ALU.add,
                    [PB[bX], ("den", slot), ("Hs", hb, t - 2)], [("Hs", hb, t - 2)])
            yield

        def gen_norm(h):
            hb = h % 2
            for j in range(NT_LAT):
                act(sqj, Hs[hb][:, j, :], AF.Square, [("Hs", hb, j)], ["sqj", "ssq"], accum_out=ssq[:, j:j + 1])
                yield
            ts("dve", rs16, ssq, 1.0 / 128, EPS, ALU.mult, ALU.add, ["ssq"], ["rs16"])
            act(rs16, rs16, AF.Sqrt, ["rs16"], ["rs16"])
            recip(rs16, rs16, ["rs16"], ["rs16"])
            yield
            for g4 in range(4):
                for j4 in range(4):
                    j = g4 * 4 + j4
                    stt(hg4[j4], Hs[hb][:, j, :], rs16[:, j:j + 1], mhw_row[:, h * 128:(h + 1) * 128], ALU.mult, ALU.mult,
                        [("Hs", hb, j), "rs16", "mhw"], [("hg4", j4)])
                    yield
                for j4 in range(4):
                    tr(bank_bf(5, 128, 512 + j4 * 128), hg4[j4], ident_b, [("hg4", j4), "ident_b"], [PB[5]])
                tt("dve", hmgT[:, h, g4 * 512:(g4 + 1) * 512], bank_bf(5, 512, 512), ogT[hb][:, g4 * 512:(g4 + 1) * 512], ALU.mult,
                   [PB[5], ("ogT", hb, g4)], [("hmgT", h)])
                yield

        load_head_w(0)
        load_head_w(1)
        Pump([gen_proj(0)], 1).drain()
        prev_norm = None
        for h in range(8):
            stp = Pump([gen_state(h, 0), gen_state(h, 1)], 2)
            projp = Pump([gen_proj(h + 1)], 1) if h + 1 < 8 else None
            cnt_ = [0]

            def side():
                cnt_[0] += 1
                if prev_norm is not None and cnt_[0] % 3 == 0:
                    prev_norm.step(1)
                if projp is not None and cnt_[0] % 8 == 0:
                    projp.step(1)

            for d_ in range(2):
                memset("pool", Tst[d_], 0.0, [("T", d_)])
            while not stp.done():
                stp.step(1)
                side()
            items = [(0, t) for t in range(2, NT)] + [(1, t) for t in range(2, NT)]
            if os.environ.get("KNOOUT"):
                items = []
            if os.environ.get("KNITEMS"):
                items = items[:int(os.environ["KNITEMS"])]
            slots = [None] * W_OUT
            while items or any(g_ is not None for g_ in slots):
                for k_ in range(W_OUT):
                    if slots[k_] is None and items:
                        d_, t_ = items.pop(0)
                        slots[k_] = gen_out(h, d_, t_, k_)
                    if slots[k_] is not None:
                        try:
                            next(slots[k_])
                        except StopIteration:
                            slots[k_] = None
                        side()
            if prev_norm is not None:
                prev_norm.drain()
            if projp is not None:
                projp.drain()
            prev_norm = Pump([gen_norm(h)], 1)
            if b == 0 and "Hs%d" % h in dbg_d:
                dump("Hs%d" % h, Hs[h % 2].rearrange("p t c -> p (t c)"), [("Hs", h % 2, j) for j in range(NT_LAT)])
        prev_norm.drain()
        HMG_ALL = [("hmgT", h) for h in range(8)]
        if b == 0 and "hmgT" in dbg_d:
            for h in range(8):
                dump("hmgT", hmgT[:, h, :], HMG_ALL, dst=dbg_d["hmgT"][h * 128:(h + 1) * 128, :])
        S.barrier()
        A.release(m2)
        if stop_after == "2":
            return finish(nc, S, A)


        m4 = A.mark()
        oT = A.bf16(8 * SEQ).rearrange("p (k t) -> p k t", k=8)
        oT_w = A.last
        for h in range(8):
            dma("sp", oT_w[:, h * (SEQ // 2):(h + 1) * (SEQ // 2)], otscr_d[b, :, h * (SEQ // 2):(h + 1) * (SEQ // 2)],
                [("otscr", h)], [("oT", h)], "otld")
        zT_off = A.mark()
        zT = A.bf16(8 * SEQ).rearrange("p (k t) -> p k t", k=8)
        wbm = A.bf16(8 * D).rearrange("p (k c) -> p k c", k=8)
        wba = A.bf16(8 * D).rearrange("p (k c) -> p k c", k=8)
        wbg = [A.bf16(8 * 256).rearrange("p (k c) -> p k c", k=8) for _ in range(2)]
        sg0 = A.f32(512)
        sg1 = A.f32(512)
        zt0 = A.f32(512)
        zt1 = A.f32(512)
        for k in range(8):
            dma("pool", wbm[:, k, :], wbm_d[k * 128:(k + 1) * 128, :], [], ["wbm"], "wbm")
            dma("pool", wba[:, k, :], wba_d[k * 128:(k + 1) * 128, :], [], ["wba"], "wba")
        it = 0
        for c in range(8):
            sl = c % 2
            dma("pool", wbg[sl], win_v[:, :, 5664 + c * 256: 5664 + (c + 1) * 256], [], [("wbg", sl)], "wbg%d" % sl)
            for tb in range(4):
                pb0 = 4 * (it % 2)
                it += 1
                tsl = slice(tb * 512, (tb + 1) * 512)
                xsl = slice(256 + tb * 512, 256 + (tb + 1) * 512)
                xk = [("xmodT", 2 + tb * 4 + q4) for q4 in range(4)]
                for k in range(8):
                    mm(bank(pb0, 512), wbm[:, k, c * 128:(c + 1) * 128], hmgT[:, k, tsl], k == 0, k == 7, ["wbm", ("hmgT", k)], [PB[pb0]])
                for k in range(8):
                    mm(bank(pb0 + 1, 512), wba[:, k, c * 128:(c + 1) * 128], oT[:, k, tsl], k == 0, k == 7, ["wba", ("oT", k)], [PB[pb0 + 1]])
                for k in range(8):
                    mm(bank(pb0 + 2, 512), wbg[sl][:, k, 0:128], xmodT[:, k, xsl], k == 0, k == 7, [("wbg", sl)] + xk, [PB[pb0 + 2]])
                for k in range(8):
                    mm(bank(pb0 + 3, 512), wbg[sl][:, k, 128:256], xmodT[:, k, xsl], k == 0, k == 7, [("wbg", sl)] + xk, [PB[pb0 + 3]])
                act(sg0, bank(pb0 + 2, 512), AF.Sigmoid, [PB[pb0 + 2]], ["sg0"])
                act(sg1, bank(pb0 + 3, 512), AF.Sigmoid, [PB[pb0 + 3]], ["sg1"])
                tt("dve", zt0, bank(pb0, 512), sg0, ALU.mult, [PB[pb0], "sg0"], ["zt0"])
                tt("dve", zt1, bank(pb0 + 1, 512), sg1, ALU.mult, [PB[pb0 + 1], "sg1"], ["zt1"])
                tt("pool", zT[:, c, tsl], zt0, zt1, ALU.add, ["zt0", "zt1"], [("zT", tb)])
        S.barrier()
        if stop_after == "4a":
            return finish(nc, S, A)

        A.release(mB)
        fT = A.bf16(8 * SEQ).rearrange("p (k t) -> p k t", k=8)
        m5 = A.mark()
        LG = A.f32(NT_LAT * 36).rearrange("p (t c) -> p t c", c=36)
        m4b = A.mark()
        wo = A.bf16(8 * D).rearrange("p (k c) -> p k c", k=8)
        wr = A.f32(8 * 36).rearrange("p (k c) -> p k c", k=8)
        xt2 = [A.f32(D) for _ in range(2)]
        hm = [A.f32(D) for _ in range(2)]
        hn = [A.f32(D) for _ in range(2)]
        fr = [A.f32(D).rearrange("p (k c) -> p k c", k=8) for _ in range(2)]
        junk2 = A.bf16(D)
        ss2 = [A.f32(1) for _ in range(2)]
        assert A.top <= zT_off, (A.top, zT_off)
        for k in range(8):
            dma("pool", wo[:, k, :], wo_d[k * 128:(k + 1) * 128, :], [], ["wo"], "wo")
        dma("sp", wr, wr_d.rearrange("(k p) c -> p k c", p=128), [], ["wr"], "wr")
        def gen_4b(jt):
            s2 = jt % 2
            dma("sp", xt2[s2], x_d[b, jt * 128:(jt + 1) * 128, :], [], [("xt2", s2)], "xt2%d" % s2)
            yield
            for hf in range(2):
                for k in range(8):
                    mm(bank(hf, 512), zT[:, k, jt * 128:(jt + 1) * 128], wo[:, k, hf * 512:(hf + 1) * 512], k == 0, k == 7,
                       [("zT", jt // 4), "wo"], [PB[hf]])
                hsl = slice(hf * 512, (hf + 1) * 512)
                tt("dve", hm[s2][:, hsl], bank(hf, 512), grow[b][0][:, hsl], ALU.mult, [PB[hf], ("grow", b, 0)], [("hm", s2)])
                yield
                tt("dve", hm[s2][:, hsl], hm[s2][:, hsl], xt2[s2][:, hsl], ALU.add, [("hm", s2), ("xt2", s2)], [("hm", s2)])
                yield
            dma("sp", hmid_d[b, jt * 128:(jt + 1) * 128, :], hm[s2], [("hm", s2)], [("hmid", jt)], "hmst%d" % s2)
            act(junk2, hm[s2], AF.Square, [("hm", s2)], ["junk2", ("ss2", s2)], accum_out=ss2[s2])
            yield
            ts("dve", ss2[s2], ss2[s2], 1.0 / D, EPS, ALU.mult, ALU.add, [("ss2", s2)], [("ss2", s2)])
            yield
            act(ss2[s2], ss2[s2], AF.Sqrt, [("ss2", s2)], [("ss2", s2)])
            yield
            recip(ss2[s2], ss2[s2], [("ss2", s2)], [("ss2", s2)])
            yield
            act(hn[s2], hm[s2], AF.Identity, [("hm", s2), ("ss2", s2)], [("hn", s2)], scale=ss2[s2])
            yield
            for hf in range(2):
                bt = 2 + 2 * s2 + hf
                for k4 in range(4):
                    k = hf * 4 + k4
                    tr(bank(bt, 128, k4 * 128), hn[s2][:, k * 128:(k + 1) * 128], ident_f, [("hn", s2), "ident_f"], [PB[bt]])
                yield
                for k4 in range(4):
                    k = hf * 4 + k4
                    ts("dve", fr[s2][:, k, :], bank(bt, 128, k4 * 128), A2[:, b * 8 + k:b * 8 + k + 1], modT3[:, 24 + k, b:b + 1],
                       ALU.mult, ALU.add, [PB[bt], "A2", "modT"], [("fr", s2)])
                yield
            copy("act", fT[:, :, jt * 128:(jt + 1) * 128], fr[s2], [("fr", s2)], [("fT", jt // 4)])
            yield
            bl = 6 + s2
            for k in range(8):
                mm(bank(bl, 36), fr[s2][:, k, :], wr[:, k, :], k == 0, k == 7, [("fr", s2), "wr"], [PB[bl]])
            yield
            tt("dve", LG[:, jt, :], bank(bl, 36), br_row, ALU.add, [PB[bl], "br"], ["LG"])
            yield

        Pump([gen_4b(jt) for jt in range(NT_LAT)], 2).drain()
        if b == 0 and "fT" in dbg_d:
            for k in range(8):
                dump("fT", fT[:, k, :], [("fT", q4) for q4 in range(4)], dst=dbg_d["fT"][k * 128:(k + 1) * 128, :])
        if b == 0:
            dump("LG", LG.rearrange("p t c -> p (t c)"), ["LG"])
        S.barrier()
        A.release(m4b)
        if stop_after == "4b":
            return finish(nc, S, A)

        CW = A.f32(NT_LAT * 32).rearrange("p (t e) -> p t e", e=32)
        m5b = A.mark()
        BIG = 1.0e30
        gmax = A.f32(16)
        goh = A.f32(64).rearrange("p (t g) -> p t g", g=4)
        gsh = A.f32(64).rearrange("p (t g) -> p t g", g=4)
        gsum = A.f32(16)
        pgrp = A.f32(16)
        negm = A.f32(64).rearrange("p (t g) -> p t g", g=4)
        em = A.f32(512).rearrange("p (t e) -> p t e", e=32)
        em2 = A.f32(512).rearrange("p (t e) -> p t e", e=32)
        oh1 = A.f32(512).rearrange("p (t e) -> p t e", e=32)
        oh2 = A.f32(512).rearrange("p (t e) -> p t e", e=32)
        v1 = A.f32(16)
        v2 = A.f32(16)
        w1 = A.f32(16)
        w2 = A.f32(16)
        gl = LG[:, :, 0:4]
        el4 = LG[:, :, 4:36].rearrange("p t (g e) -> p t g e", g=4)
        bc4 = lambda a: a.unsqueeze(2).to_broadcast([128, 16, 4])
        bc32 = lambda a: a.unsqueeze(2).to_broadcast([128, 16, 32])
        reduce(gmax, gl, AX.X, ALU.max, ["LG"], ["gmax"])
        tt("dve", goh, gl, bc4(gmax), ALU.is_equal, ["LG", "gmax"], ["goh"])
        tt("dve", gsh, gl, bc4(gmax), ALU.subtract, ["LG", "gmax"], ["gsh"])
        act(gsh, gsh, AF.Exp, ["gsh"], ["gsh"])
        reduce(gsum, gsh, AX.X, ALU.add, ["gsh"], ["gsum"])
        recip(pgrp, gsum, ["gsum"], ["pgrp"])
        ts("dve", negm, goh, BIG, -BIG, ALU.mult, ALU.add, ["goh"], ["negm"])
        tt("dve", em.rearrange("p t (g e) -> p t g e", g=4), el4, negm.unsqueeze(3).to_broadcast([128, 16, 4, 8]), ALU.add,
           ["LG", "negm"], ["em"])
        reduce(v1, em, AX.X, ALU.max, ["em"], ["v1"])
        tt("dve", oh1, em, bc32(v1), ALU.is_equal, ["em", "v1"], ["oh1"])
        stt(em2.rearrange("p t e -> p (t e)"), oh1.rearrange("p t e -> p (t e)"), -BIG, em.rearrange("p t e -> p (t e)"),
            ALU.mult, ALU.add, ["oh1", "em"], ["em2"])
        reduce(v2, em2, AX.X, ALU.max, ["em2"], ["v2"])
        tt("dve", oh2, em2, bc32(v2), ALU.is_equal, ["em2", "v2"], ["oh2"])
        tt("dve", w1, v1, v2, ALU.subtract, ["v1", "v2"], ["w1"])
        act(w1, w1, AF.Sigmoid, ["w1"], ["w1"])
        tt("dve", w1, w1, pgrp, ALU.mult, ["w1", "pgrp"], ["w1"])
        tt("dve", w2, pgrp, w1, ALU.subtract, ["pgrp", "w1"], ["w2"])
        tt("dve", CW, oh1, bc32(w1), ALU.mult, ["oh1", "w1"], ["CW"])
        tt("dve", oh2, oh2, bc32(w2), ALU.mult, ["oh2", "w2"], ["oh2"])
        tt("dve", CW, CW, oh2, ALU.add, ["CW", "oh2"], ["CW"])
        if b == 0:
            dump("CW", CW.rearrange("p t e -> p (t e)"), ["CW"])
        if stop_after == "5b":
            return finish(nc, S, A)

        acc = A.f32(NT_LAT * D).rearrange("p (t c) -> p t c", c=D)
        wgu = [A.bf16(8 * 512).rearrange("p (k c) -> p k c", k=8) for _ in range(2)]
        wdn = [A.bf16(2 * D).rearrange("p (k c) -> p k c", k=2) for _ in range(2)]
        sgl = [A.f32(512) for _ in range(2)]
        hT = [[A.bf16(512) for _ in range(2)] for _ in range(2)]
        hm2 = [A.f32(D) for _ in range(2)]
        ot = [A.f32(D) for _ in range(2)]
        ysc = [A.f32(512) for _ in range(2)]
        NE = NEXP if stop_after != "5c1" else 2

        def load_expert(e):
            sl = e % 2
            dma("pool", wgu[sl], wgu_d[e].rearrange("(k p) c -> p k c", p=128), [], [("wgu", sl)], "wgu%d" % sl)
            dma("pool", wdn[sl], wed_d[e].rearrange("(k p) c -> p k c", p=128), [], [("wdn", sl)], "wdn%d" % sl)

        load_expert(0)
        ity = [0]

        def moe_gu(e, tb, fc):
            sl = e % 2
            tsl = slice(tb * 512, (tb + 1) * 512)
            for k in range(8):
                mm(bank(fc, 512), wgu[sl][:, k, fc * 128:(fc + 1) * 128], fT[:, k, tsl], k == 0, k == 7,
                   [("wgu", sl), ("fT", tb)], [PB[fc]])
            for k in range(8):
                mm(bank(2 + fc, 512), wgu[sl][:, k, 256 + fc * 128:256 + (fc + 1) * 128], fT[:, k, tsl], k == 0, k == 7,
                   [("wgu", sl), ("fT", tb)], [PB[2 + fc]])
            act(sgl[fc], bank(fc, 512), AF.Silu, [PB[fc]], [("sgl", fc)])
            tt("dve", hT[tb % 2][fc], bank(2 + fc, 512), sgl[fc], ALU.mult, [PB[2 + fc], ("sgl", fc)], [("hT", tb % 2, fc)])

        def moe_down(e, tb):
            sl = e % 2
            for q4 in range(4):
                jt = tb * 4 + q4
                by = 4 + 2 * (ity[0] % 2)
                ity[0] += 1
                for hf in range(2):
                    for fc in range(2):
                        mm(bank(by + hf, 512), hT[tb % 2][fc][:, q4 * 128:(q4 + 1) * 128], wdn[sl][:, fc, hf * 512:(hf + 1) * 512],
                           fc == 0, fc == 1, [("hT", tb % 2, fc), ("wdn", sl)], [PB[by + hf]])
                    hsl = slice(hf * 512, (hf + 1) * 512)
                    if e == 0:
                        ts("dve", acc[:, jt, hsl], bank(by + hf, 512), CW[:, jt, e:e + 1], None, ALU.mult, None,
                           [PB[by + hf], "CW"], [("acc", jt, hf)])
                    elif hf == 1:
                        sy = ysc[ity[0] % 2]
                        act(sy, bank(by + hf, 512), AF.Identity, [PB[by + hf], "CW"], [("ysc", ity[0] % 2)], scale=CW[:, jt, e:e + 1])
                        tt("pool", acc[:, jt, hsl], acc[:, jt, hsl], sy, ALU.add, [("ysc", ity[0] % 2), ("acc", jt, hf)], [("acc", jt, hf)])
                    else:
                        stt(acc[:, jt, hsl], bank(by + hf, 512), CW[:, jt, e:e + 1], acc[:, jt, hsl], ALU.mult, ALU.add,
                            [PB[by + hf], "CW", ("acc", jt, hf)], [("acc", jt, hf)])

        pend = None
        for e in range(NE):
            for tb in range(4):
                moe_gu(e, tb, 0)
                if pend is not None:
                    moe_down(*pend)
                if tb == 0 and e + 1 < NE:
                    load_expert(e + 1)
                moe_gu(e, tb, 1)
                pend = (e, tb)
        moe_down(*pend)
        for jt in range(NT_LAT):
            s2 = jt % 2
            dma("sp", hm2[s2], hmid_d[b, jt * 128:(jt + 1) * 128, :], [("hmid", jt)], [("hm2", s2)], "hm2%d" % s2)
            tt("pool", ot[s2], acc[:, jt, :], grow[b][1], ALU.mult, [("acc", jt, 0), ("acc", jt, 1), ("grow", b, 1)], [("ot", s2)])
            tt("dve", ot[s2], ot[s2], hm2[s2], ALU.add, [("ot", s2), ("hm2", s2)], [("ot", s2)])
            dma("sp", out_d[b, jt * 128:(jt + 1) * 128, :], ot[s2], [("ot", s2)], [("out", jt)], "ost%d" % s2)
        S.barrier()
        A.release(mB)

    return finish(nc, S, A)


def finish(nc, S, A):
    print('SBUF arena peak words', A.peak, 'of', A.words)
    S.barrier()
    S.add("sp", lambda e: e.nop(), reads=(), writes=())
    S.emit(None)
    return nc


def _consts():
    ident = np.eye(128, dtype=np.float32)
    r = np.arange(128)
    trif = (r[:, None] <= r[None, :]).astype(np.float32)
    trib = (r[:, None] >= r[None, :]).astype(np.float32)
    half = 64
    inv = (10000.0 ** (-np.arange(0, half, 2, dtype=np.float32) / half)).astype(np.float32)
    tok = np.arange(SEQ)
    row = (tok // 64).astype(np.float32)
    col = (tok % 64).astype(np.float32)
    ang_r = row[:, None] * inv[None, :]
    ang_c = col[:, None] * inv[None, :]
    cr, sr, cc, sc = np.cos(ang_r), np.sin(ang_r), np.cos(ang_c), np.sin(ang_c)
    cos = np.concatenate([cr, cr, cc, cc], axis=1).astype(np.float32)
    sin = np.concatenate([-sr, sr, -sc, sc], axis=1).astype(np.float32)
    cos_t = cos.reshape(NT_LAT, 128, 128).transpose(1, 0, 2).reshape(128, NT_LAT * 128)
    sin_t = sin.reshape(NT_LAT, 128, 128).transpose(1, 0, 2).reshape(128, NT_LAT * 128)
    return ident, trif, trib, np.ascontiguousarray(cos_t), np.ascontiguousarray(sin_t)


def _win_perm():
    idx = []
    for h in range(8):
        for f_ in range(4):
            idx += list(range(f_ * 1024 + h * 128, f_ * 1024 + (h + 1) * 128))
    idx += list(range(4096, 4128))
    idx += list(range(4128, 5152))
    for j in range(2):
        idx += list(range(5152 + j * 128, 5152 + (j + 1) * 128))
        idx += list(range(5408 + j * 128, 5408 + (j + 1) * 128))
    for c in range(8):
        idx += list(range(5664 + c * 128, 5664 + (c + 1) * 128))
        idx += list(range(6688 + c * 128, 6688 + (c + 1) * 128))
    assert len(idx) == INW and len(set(idx)) == INW
    return np.asarray(idx)


def make_in_maps(inputs, NB, cores):
    f = lambda a: np.ascontiguousarray(np.asarray(a, dtype=np.float32))
    ident, trif, trib, cos_t, sin_t = _consts()

    def pk(v):
        return np.ascontiguousarray(np.asarray(v, np.float32).reshape(8, 128).T)

    shared = {
        "w_ada": f(inputs["w_ada"][0]),
        "b_adaT": np.ascontiguousarray(np.asarray(inputs["b_ada"][0], np.float32).reshape(48, 128).T),
        "n1T": pk(inputs["norm1_w"][0]), "n2T": pk(inputs["norm2_w"][0]),
        "w_in": np.ascontiguousarray(np.asarray(inputs["w_in"][0], np.float32)[:, _win_perm()]),
        "b_mgate": f(inputs["b_mgate"][0]).reshape(1, 32),
        "q_norm_w": f(inputs["q_norm_w"][0]).reshape(1, 128),
        "k_norm_w": f(inputs["k_norm_w"][0]).reshape(1, 128),
        "mh_norm_w": f(inputs["mh_norm_w"][0]).reshape(1, D),
        "w_bm": f(inputs["w_branch_m"][0]), "w_ba": f(inputs["w_branch_a"][0]), "w_o": f(inputs["w_out"][0]),
        "w_r": np.ascontiguousarray(np.concatenate([inputs["w_rg"][0], inputs["w_re"][0]], axis=1).astype(np.float32)),
        "b_r": np.ascontiguousarray(np.concatenate([inputs["b_rg"][0], inputs["b_re"][0]]).astype(np.float32).reshape(1, 36)),
        "w_egu": np.ascontiguousarray(np.concatenate([np.asarray(inputs["w_e_gate"][0], np.float32),
                                                      np.asarray(inputs["w_e_up"][0], np.float32)], axis=2)),
        "w_ed": f(inputs["w_e_down"][0]),
        "c_ident": ident, "c_trif": trif, "c_trib": trib, "c_cos": cos_t, "c_sin": sin_t,
    }
    maps = []
    for c in cores:
        bs = slice(c * NB, (c + 1) * NB)
        cv = np.concatenate([np.asarray(inputs["c"], np.float32)[bs], np.asarray(inputs["c_ctx"], np.float32)[None, :]], axis=0)
        NV = NB + 1
        cT = cv.reshape(NV, 8, 128).transpose(2, 1, 0).reshape(128, 8 * NV)
        m = dict(shared)
        m["x"] = f(inputs["x"][bs])
        m["ctxx"] = f(inputs["ctx"][bs])
        m["cT"] = np.ascontiguousarray(cT)
        maps.append(m)
    return maps


_NC_CACHE = {}


def kernel(**inputs):
    NB = 2
    if "full" not in _NC_CACHE:
        _NC_CACHE["full"] = build_program(NB=NB)
    nc = _NC_CACHE["full"]
    maps = make_in_maps(inputs, NB, list(range(N_CORES)))
    res = run_bass_kernel_spmd(nc, maps, core_ids=list(range(N_CORES)))
    out = np.concatenate([np.asarray(r["out"], dtype=np.float32) for r in res.results], axis=0)
    return out
```

```python
import os
import numpy as np
import concourse.bass as bass
import concourse.mybir as mybir
from concourse.bass_utils import run_bass_kernel_spmd

F32 = mybir.dt.float32
BF16 = mybir.dt.bfloat16
AF = mybir.ActivationFunctionType
ALU = mybir.AluOpType
AX = mybir.AxisListType

D = 1024
SEQ = 2048
CTX = 256
NT_LAT = 16
NT = 18
NTOK = NT * 128
INW = 7712
EPS = 1e-6
NEXP = 32
DEXP = 256
N_CORES = 8

ENGS = ("pe", "act", "dve", "pool", "sp")


class _Ins:
    __slots__ = ("eng", "idx", "fn", "deps", "dma", "flag")

    def __init__(self, eng, idx, fn, deps, dma):
        self.eng, self.idx, self.fn, self.deps, self.dma, self.flag = eng, idx, fn, deps, dma, False


class Sched:
    def __init__(self, nc):
        self.nc = nc
        self.ins = {e: [] for e in ENGS}
        self.res = {}
        self.dma_cnt = {}
        self.pending = {e: set() for e in ENGS}
        self.order = []
        self.swq = []
        self.SW_LIMIT = 2600

    def add(self, eng, fn, reads=(), writes=(), dma=None, ndesc=0):
        deps = set(self.pending[eng])
        self.pending[eng] = set()
        if dma is not None and eng == "pool":
            tot = sum(n for _, n in self.swq) + ndesc
            while self.swq and tot > self.SW_LIMIT:
                tk, n = self.swq.pop(0)
                deps.add(tk)
                tot -= n
        raw = set()
        for r in reads:
            st = self.res.get(r)
            if st is not None and st[0] is not None:
                deps.add(st[0])
                raw.add(st[0])
            if st is not None and isinstance(r, str) and r.startswith("pb"):
                deps.update(st[1].values())
        for w in writes:
            st = self.res.get(w)
            if st is not None:
                if st[0] is not None:
                    deps.add(st[0])
                deps.update(st[1].values())
        deps = {(d if d[0] == "E" else ("D", d[1], self.dma_cnt[d[1]])) for d in deps}
        idx = len(self.ins[eng])
        self.order.append((eng, idx))
        if dma is None:
            deps = {d for d in deps if not (d[0] == "E" and d[1] == eng and (eng == "pe" or d not in raw))}
            tok = ("E", eng, idx)
        else:
            c = self.dma_cnt.get(dma, 0) + 1
            self.dma_cnt[dma] = c
            tok = ("D", dma, c)
        if dma is not None and eng == "pool":
            self.swq.append((tok, ndesc))
        ins = _Ins(eng, idx, fn, deps, dma)
        self.ins[eng].append(ins)
        for w in writes:
            self.res[w] = [tok, {}]
        for r in reads:
            st = self.res.setdefault(r, [None, {}])
            st[1][(tok[0], tok[1])] = tok
        return tok

    def barrier(self):
        toks = set()
        for e in ENGS:
            if self.ins[e]:
                last = self.ins[e][-1]
                if last.dma is None:
                    toks.add(("E", e, last.idx))
                else:
                    for j in range(len(self.ins[e]) - 1, -1, -1):
                        if self.ins[e][j].dma is None:
                            toks.add(("E", e, j))
                            break
        for k, c in self.dma_cnt.items():
            toks.add(("D", k, c))
        for e in ENGS:
            self.pending[e] |= toks

    def emit(self, block):
        nc = self.nc
        for e in ENGS:
            for ins in self.ins[e]:
                for d in ins.deps:
                    if d[0] == "E":
                        self.ins[d[1]][d[2]].flag = True
        cnt = {}
        for e in ENGS:
            c = 0
            arr = []
            for ins in self.ins[e]:
                if ins.dma is None and ins.flag:
                    c += 1
                arr.append(c)
            cnt[e] = arr
        esem = {e: nc.alloc_semaphore("s_" + e) for e in ENGS}
        dsem = {k: nc.alloc_semaphore("d_%d" % i) for i, k in enumerate(self.dma_cnt)}
        self.n_waits = 0

        if block is None:
            engobjs = {"pe": nc.tensor, "act": nc.scalar, "dve": nc.vector, "pool": nc.gpsimd, "sp": nc.sync}
            seen_all = {e: {} for e in ENGS}
            for (e, idx) in self.order:
                ins = self.ins[e][idx]
                engobj = engobjs[e]
                seen = seen_all[e]
                need = {}
                for d in ins.deps:
                    if d[0] == "E":
                        key, val = ("E", d[1]), cnt[d[1]][d[2]]
                    else:
                        key, val = ("D", d[1]), 16 * d[2]
                    if val > need.get(key, 0):
                        need[key] = val
                for key, val in need.items():
                    if seen.get(key, 0) < val:
                        sem = esem[key[1]] if key[0] == "E" else dsem[key[1]]
                        engobj.wait_ge(sem, val)
                        seen[key] = val
                        self.n_waits += 1
                inst = ins.fn(engobj)
                if ins.dma is not None:
                    inst.then_inc(dsem[ins.dma], 16)
                elif ins.flag:
                    inst.then_inc(esem[e], 1)
            return

        def run(e, engobj):
            seen = {}
            for ins in self.ins[e]:
                need = {}
                for d in ins.deps:
                    if d[0] == "E":
                        key, val = ("E", d[1]), cnt[d[1]][d[2]]
                    else:
                        key, val = ("D", d[1]), 16 * d[2]
                    if val > need.get(key, 0):
                        need[key] = val
                for key, val in need.items():
                    if seen.get(key, 0) < val:
                        sem = esem[key[1]] if key[0] == "E" else dsem[key[1]]
                        engobj.wait_ge(sem, val)
                        seen[key] = val
                        self.n_waits += 1
                inst = ins.fn(engobj)
                if ins.dma is not None:
                    inst.then_inc(dsem[ins.dma], 16)
                elif ins.flag:
                    inst.then_inc(esem[e], 1)

        block.tensor(lambda t: run("pe", t))
        block.scalar(lambda a: run("act", a))
        block.vector(lambda v: run("dve", v))
        block.gpsimd(lambda g: run("pool", g))
        block.sync(lambda s: run("sp", s))


class Pump:
    def __init__(self, gens, width):
        self.queue = list(gens)
        self.width = width
        self.active = []
        self.rr = 0
        self.completed = 0

    def done(self):
        return not self.queue and not self.active

    def step(self, n=1):
        for _ in range(n):
            while len(self.active) < self.width and self.queue:
                self.active.append(self.queue.pop(0))
            if not self.active:
                return
            self.rr %= len(self.active)
            g = self.active[self.rr]
            try:
                next(g)
                self.rr += 1
            except StopIteration:
                self.active.pop(self.rr)
                self.completed += 1

    def drain(self):
        while not self.done():
            self.step(1)


class Arena:
    def __init__(self, nc, words):
        self.t = nc.alloc_sbuf_tensor("arena", [128, words], F32)
        self.words = words
        self.top = 0
        self.peak = 0

    def mark(self):
        return self.top

    def release(self, m):
        self.top = m

    def alloc(self, nbytes):
        w = (nbytes + 3) // 4
        w = (w + 7) // 8 * 8
        off = self.top
        self.top += w
        self.peak = max(self.peak, self.top)
        assert self.top <= self.words, "SBUF arena overflow %d > %d" % (self.top, self.words)
        return off, w

    def f32(self, n):
        off, w = self.alloc(4 * n)
        return self.t[:, off:off + n]

    def bf16(self, n):
        off, w = self.alloc(2 * n)
        self.last = self.t[:, off:off + w]
        return self.t[:, off:off + w].bitcast(BF16)[:, 0:n]


def build_program(NB=2, stop_after=None, dbg=()):
    nc = bass.Bass("TRN2", target_bir_lowering=False)
    NV = NB + 1

    def din(name, shape, dt=F32):
        return nc.dram_tensor(name, list(shape), dt, kind="ExternalInput").ap()

    x_d = din("x", [NB, SEQ, D])
    ctx_d = din("ctxx", [NB, CTX, D])
    cT_d = din("cT", [128, 8 * NV])
    wada_d = din("w_ada", [D, 6 * D])
    bada_d = din("b_adaT", [128, 48])
    n1_d = din("n1T", [128, 8])
    n2_d = din("n2T", [128, 8])
    win_d = din("w_in", [D, INW])
    bmg_d = din("b_mgate", [1, 32])
    qw_d = din("q_norm_w", [1, 128])
    kw_d = din("k_norm_w", [1, 128])
    mhw_d = din("mh_norm_w", [1, D])
    wbm_d = din("w_bm", [D, D])
    wba_d = din("w_ba", [D, D])
    wo_d = din("w_o", [D, D])
    wr_d = din("w_r", [D, 36])
    br_d = din("b_r", [1, 36])
    wgu_d = din("w_egu", [NEXP, D, 2 * DEXP])
    wed_d = din("w_ed", [NEXP, DEXP, D])
    ident_d = din("c_ident", [128, 128])
    trif_d = din("c_trif", [128, 128])
    trib_d = din("c_trib", [128, 128])
    cos_d = din("c_cos", [128, NT_LAT * 128])
    sin_d = din("c_sin", [128, NT_LAT * 128])
    out_d = nc.dram_tensor("out", [NB, SEQ, D], F32, kind="ExternalOutput").ap()
    hmid_d = nc.dram_tensor("hmid_scr", [NB, SEQ, D], F32, kind="Internal").ap()
    otscr_d = nc.dram_tensor("ot_scr", [NB, 128, 4 * SEQ], F32, kind="Internal").ap()
    dbg_d = {}
    for name, shape in dbg:
        dbg_d[name] = nc.dram_tensor("dbg_" + name, list(shape), F32, kind="ExternalOutput").ap()

    S = Sched(nc)
    A = Arena(nc, 53180)
    psum = nc.alloc_psum_tensor("psum", [128, 4096], F32)

    def bank(i, n=512, off=0):
        return psum[:, i * 512 + off: i * 512 + off + n]

    def bank_bf(i, n=1024, off=0):
        return psum[:, i * 512:(i + 1) * 512].bitcast(BF16)[:, off:off + n]

    PB = ["pb%d" % i for i in range(8)]

    win_v = win_d.rearrange("(k p) c -> p k c", p=128)

    def dma(eng, out, in_, reads, writes, sem):
        def nd(ap):
            sh = list(ap.shape)
            n = 1
            for v_ in sh[:-1]:
                n *= v_
            return n
        ndesc = max(nd(out), nd(in_))
        S.add(eng, lambda e: e.dma_start(out=out, in_=in_), reads=reads, writes=writes, dma=sem, ndesc=ndesc)

    def mm(out, lhsT, rhs, start, stop, reads, writes):
        S.add("pe", lambda e: e.matmul(out, lhsT=lhsT, rhs=rhs, start=start, stop=stop), reads=reads, writes=writes)

    def tr(out, in_, ident, reads, writes):
        S.add("pe", lambda e: e.transpose(out, in_, ident), reads=reads, writes=writes)

    def act(out, in_, func, reads, writes, bias=None, scale=None, accum_out=None):
        kw = {}
        if bias is not None:
            kw["bias"] = bias
        if scale is not None:
            kw["scale"] = scale
        if accum_out is not None:
            kw["accum_out"] = accum_out
        S.add("act", lambda e: e.activation(out=out, in_=in_, func=func, **kw), reads=reads, writes=writes)

    def ts(eng, out, in0, s1, s2, op0, op1, reads, writes):
        if op1 is None:
            S.add(eng, lambda e: e.tensor_scalar(out=out, in0=in0, scalar1=s1, scalar2=None, op0=op0), reads=reads, writes=writes)
        else:
            S.add(eng, lambda e: e.tensor_scalar(out=out, in0=in0, scalar1=s1, scalar2=s2, op0=op0, op1=op1), reads=reads, writes=writes)

    def tt(eng, out, in0, in1, op, reads, writes):
        S.add(eng, lambda e: e.tensor_tensor(out=out, in0=in0, in1=in1, op=op), reads=reads, writes=writes)

    def stt(out, in0, scalar, in1, op0, op1, reads, writes):
        S.add("dve", lambda e: e.scalar_tensor_tensor(out=out, in0=in0, scalar=scalar, in1=in1, op0=op0, op1=op1), reads=reads, writes=writes)

    def recip(out, in_, reads, writes):
        S.add("dve", lambda e: e.reciprocal(out=out, in_=in_), reads=reads, writes=writes)

    def copy(eng, out, in_, reads, writes):
        if eng == "act":
            S.add("act", lambda e: e.activation(out=out, in_=in_, func=AF.Copy), reads=reads, writes=writes)
        else:
            S.add(eng, lambda e: e.tensor_copy(out=out, in_=in_), reads=reads, writes=writes)

    def memset(eng, ap, val, writes):
        S.add(eng, lambda e: e.memset(ap, val), writes=writes)

    def reduce(out, in_, axis, op, reads, writes):
        S.add("dve", lambda e: e.tensor_reduce(out=out, in_=in_, axis=axis, op=op), reads=reads, writes=writes)

    def dump(name, src_ap, reads, dst=None):
        if name in dbg_d:
            d = dbg_d[name] if dst is None else dst
            dma("pool", d, src_ap, reads, [("dbg", name)], "dbg_" + name)

    ident_f = A.f32(128)
    trif = A.f32(128)
    trib = A.f32(128)
    ones_f = A.f32(128)
    ident_b = A.bf16(128)
    ones_b = A.bf16(128)
    bmg_row = A.f32(32)
    qw_row = A.f32(128)
    kw_row = A.f32(128)
    mhw_row = A.f32(D)
    br_row = A.f32(36)
    n1T = A.f32(8)
    n2T = A.f32(8)
    badaT = A.f32(48)
    modT = A.f32(48 * NV)
    modT3 = modT.rearrange("p (j v) -> p j v", v=NV)
    A1 = A.f32(8 * NV)
    A2 = A.f32(8 * NV)
    grow1 = [A.f32(D) for _ in range(2)]
    grow = [grow1 for _ in range(NB)]

    dma("sp", ident_f, ident_d, [], ["ident_f"], "c0")
    dma("sp", trif, trif_d, [], ["trif"], "c0")
    dma("sp", trib, trib_d, [], ["trib"], "c0")
    dma("pool", ident_b, ident_d, [], ["ident_b"], "c1")
    dma("sp", bmg_row, bmg_d.partition_broadcast(128), [], ["bmg"], "c0")
    dma("sp", qw_row, qw_d.partition_broadcast(128), [], ["qw"], "c0")
    dma("sp", kw_row, kw_d.partition_broadcast(128), [], ["kw"], "c0")
    dma("sp", mhw_row, mhw_d.partition_broadcast(128), [], ["mhw"], "c0")
    dma("sp", br_row, br_d.partition_broadcast(128), [], ["br"], "c0")
    dma("sp", n1T, n1_d, [], ["n1T"], "c0")
    dma("sp", n2T, n2_d, [], ["n2T"], "c0")
    dma("sp", badaT, bada_d, [], ["badaT"], "c0")
    memset("dve", ones_f, 1.0, ["ones_f"])
    memset("dve", ones_b, 1.0, ["ones_b"])

    mA = A.mark()
    cT = A.f32(8 * NV)
    scT = A.f32(8 * NV)
    wa = [A.f32(8 * 1024).rearrange("p (k c) -> p k c", k=8) for _ in range(2)]
    diag = A.f32(D)
    dma("sp", cT, cT_d, [], ["cT"], "c0")
    act(scT, cT, AF.Silu, ["cT"], ["scT"])
    wada_v = wada_d.rearrange("(k p) c -> p k c", p=128)
    for blk in range(6):
        sl = blk % 2
        for k in range(8):
            dma(("sp", "act")[k % 2] if os.environ.get("KADA2", "1") == "1" else "sp", wa[sl][:, k, :],
                wada_v[:, k, blk * 1024:(blk + 1) * 1024], [], [("wa", sl)], "wa%d" % sl)
        for j in range(8):
            col = (blk * 8 + j) * NV
            for k in range(8):
                if os.environ.get("KSKIPA") and not (k == 0 or k == 7):
                    continue
                mm(bank(0, NV, col), wa[sl][:, k, j * 128:(j + 1) * 128], scT[:, k * NV:(k + 1) * NV],
                   k == 0, k == 7, [("wa", sl), "scT"], [PB[0]])
    tt("dve", modT3, bank(0, 48 * NV).rearrange("p (j v) -> p j v", v=NV),
       badaT.unsqueeze(2).to_broadcast([128, 48, NV]), ALU.add, [PB[0], "badaT"], ["modT"])
    for v in range(NV):
        stt(A1[:, v * 8:(v + 1) * 8], modT3[:, 8:16, v], 1.0, n1T, ALU.add, ALU.mult, ["modT", "n1T"], ["A1"])
        stt(A2[:, v * 8:(v + 1) * 8], modT3[:, 32:40, v], 1.0, n2T, ALU.add, ALU.mult, ["modT", "n2T"], ["A2"])
    if "modT" in dbg_d:
        dump("modT", modT, ["modT"])
    S.barrier()
    A.release(mA)
    if stop_after == "A":
        return finish(nc, S, A)

    for b in range(NB):
        mB = A.mark()
        xmodT = A.bf16(8 * NTOK).rearrange("p (k t) -> p k t", k=8)

        m1 = A.mark()
        diag = A.f32(D)
        for gi, base in enumerate((16, 40)):
            for k in range(8):
                ts("dve", diag[:, k * 128:(k + 1) * 128], ident_f, modT3[:, base + k, b:b + 1], None, ALU.mult, None,
                   ["ident_f", "modT"], ["diag"])
            for hf in range(2):
                mm(bank(hf), ones_f, diag[:, hf * 512:(hf + 1) * 512], True, True, ["ones_f", "diag"], [PB[hf]])
                copy("act", grow[b][gi][:, hf * 512:(hf + 1) * 512], bank(hf), [PB[hf]], [("grow", b, gi)])
        xt = [A.f32(D) for _ in range(3)]
        junk = A.bf16(D)
        xn = [A.bf16(D) for _ in range(2)]
        ss = [A.f32(1) for _ in range(2)]
        sv = [A.f32(1) for _ in range(2)]
        def gen_p1(t):
            s3, s2 = t % 3, t % 2
            src = ctx_d[b, t * 128:(t + 1) * 128, :] if t < 2 else x_d[b, (t - 2) * 128:(t - 1) * 128, :]
            vec = NB if t < 2 else b
            dma("sp", xt[s3], src, [], [("xt", s3)], "xt%d" % s3)
            yield
            act(junk, xt[s3], AF.Square, [("xt", s3)], ["junk", ("ss", s2)], accum_out=ss[s2])
            yield
            ts("dve", sv[s2], ss[s2], 1.0 / D, EPS, ALU.mult, ALU.add, [("ss", s2)], [("sv", s2)])
            yield
            act(sv[s2], sv[s2], AF.Sqrt, [("sv", s2)], [("sv", s2)])
            yield
            recip(sv[s2], sv[s2], [("sv", s2)], [("sv", s2)])
            yield
            ts("pool", xn[s2], xt[s3], sv[s2], 1.0, ALU.mult, ALU.mult, [("xt", s3), ("sv", s2)], [("xn", s2)])
            yield
            pbk = 2 + s2
            for k in range(8):
                tr(bank_bf(pbk, 128, k * 128), xn[s2][:, k * 128:(k + 1) * 128], ident_b, [("xn", s2), "ident_b"], [PB[pbk]])
            yield
            yield
            for k in range(8):
                o = xmodT[:, k, t * 128:(t + 1) * 128]
                i_ = bank_bf(pbk, 128, k * 128)
                if t % 2 == 0:
                    act(o, i_, AF.Identity, [PB[pbk], "A1", "modT"], [("xmodT", t)],
                        bias=modT3[:, k, vec:vec + 1], scale=A1[:, vec * 8 + k:vec * 8 + k + 1])
                else:
                    ts("dve", o, i_, A1[:, vec * 8 + k:vec * 8 + k + 1], modT3[:, k, vec:vec + 1], ALU.mult, ALU.add,
                       [PB[pbk], "A1", "modT"], [("xmodT", t)])
                if k % 4 == 3:
                    yield

        Pump([gen_p1(t) for t in range(NT)], 2).drain()
        XM_ALL = [("xmodT", t) for t in range(NT)]
        if b == 0 and "xmodT" in dbg_d:
            for k in range(8):
                dump("xmodT", xmodT[:, k, :], XM_ALL, dst=dbg_d["xmodT"][k * 128:(k + 1) * 128, :])
        S.barrier()
        A.release(m1)
        if stop_after == "1":
            return finish(nc, S, A)


        m3 = A.mark()
        oT = A.bf16(8 * SEQ).rearrange("p (k t) -> p k t", k=8)
        oT_w = A.last
        COSt = A.f32(NT_LAT * 128).rearrange("p (j c) -> p j c", c=128)
        SINt = A.f32(NT_LAT * 128).rearrange("p (j c) -> p j c", c=128)
        waq = [A.bf16(8 * 512).rearrange("p (k c) -> p k c", k=8) for _ in range(2)]
        wakv = [A.bf16(8 * 256).rearrange("p (k c) -> p k c", k=8) for _ in range(2)]
        kTa = [A.bf16(NTOK) for _ in range(2)]
        Va = [A.bf16(NT * 128).rearrange("p (t c) -> p t c", c=128) for _ in range(2)]
        qTa = [A.bf16(4 * SEQ).rearrange("p (g t) -> p g t", g=4) for _ in range(2)]
        qs = [A.f32(512) for _ in range(2)]
        qn = [A.f32(512) for _ in range(2)]
        qt1 = [A.f32(512) for _ in range(2)]
        qr = [A.bf16(512) for _ in range(2)]
        ssq4 = [A.f32(4) for _ in range(2)]
        kq = [A.f32(128) for _ in range(2)]
        kt1 = [A.f32(128) for _ in range(2)]
        kt2 = [A.f32(128) for _ in range(2)]
        kr = [A.bf16(128) for _ in range(2)]
        kjunk = A.f32(128)
        ssk = [A.f32(1) for _ in range(2)]
        Pt = [A.bf16(512) for _ in range(3)]
        rD = [A.f32(512) for _ in range(2)]
        DACC = os.environ.get("KDACC", "0") == "1"
        Pacc = [[A.f32(512) for _ in range(2)] for _ in range(2)] if DACC else None
        SC = float(128 ** -0.5)
        dma("sp", COSt.rearrange("p j c -> p (j c)"), cos_d, [], ["COS"], "cos")
        dma("sp", SINt.rearrange("p j c -> p (j c)"), sin_d, [], ["SIN"], "sin")

        def halves(ap2d, g=None):
            if g is None:
                return ap2d.rearrange("p (a f c) -> p a f c", a=2, f=2)
            return ap2d.rearrange("p (g a f c) -> p g a f c", g=g, a=2, f=2)

        def load_aw(j):
            dma("pool", waq[j], win_v[:, :, 4128 + j * 512: 4128 + (j + 1) * 512], [], [("waq", j)], "waq%d" % j)
            dma("pool", wakv[j], win_v[:, :, 5152 + j * 256: 5152 + (j + 1) * 256], [], [("wakv", j)], "wakv%d" % j)

        KYLD = int(os.environ.get("KYLD", "1"))
        KGAP = int(os.environ.get("KGAP", "4"))
        def gen_k(j, t):
            bk, s2 = 5, t % 2
            ko = s2 * 256
            for k in range(8):
                mm(bank(bk, 256, ko), xmodT[:, k, t * 128:(t + 1) * 128], wakv[j][:, k, :], k == 0, k == 7,
                   [("xmodT", t), ("wakv", j)], [PB[bk]])
            for _y in range(KYLD):
                yield
            act(kjunk, bank(bk, 128, ko), AF.Square, [PB[bk]], ["kjunk", ("ssk", s2)], accum_out=ssk[s2])
            for _y in range(KYLD):
                yield
            copy("act", Va[j][:, t, :], bank(bk, 128, ko + 128), [PB[bk]], [("Va", j, t)])
            for _y in range(KYLD):
                yield
            ts("dve", ssk[s2], ssk[s2], 1.0 / 128, EPS, ALU.mult, ALU.add, [("ssk", s2)], [("ssk", s2)])
            for _y in range(KYLD):
                yield
            act(ssk[s2], ssk[s2], AF.Sqrt, [("ssk", s2)], [("ssk", s2)])
            for _y in range(KYLD):
                yield
            recip(ssk[s2], ssk[s2], [("ssk", s2)], [("ssk", s2)])
            for _y in range(KYLD):
                yield
            if t < 2:
                stt(kr[s2], bank(bk, 128, ko), ssk[s2], kw_row, ALU.mult, ALU.mult, [PB[bk], ("ssk", s2), "kw"], [("kr", s2)])
                yield
            else:
                jt = t - 2
                stt(kq[s2], bank(bk, 128, ko), ssk[s2], kw_row, ALU.mult, ALU.mult, [PB[bk], ("ssk", s2), "kw"], [("kq", s2)])
                yield
                tt("pool", kt1[s2], kq[s2], COSt[:, jt, :], ALU.mult, [("kq", s2), "COS"], [("kt1", s2)])
                yield
                kq4, kt24, sn4 = halves(kq[s2]), halves(kt2[s2]), halves(SINt[:, jt, :])
                tt("pool", kt24[:, :, 0, :], kq4[:, :, 1, :], sn4[:, :, 0, :], ALU.mult, [("kq", s2), "SIN"], [("kt2", s2)])
                yield
                tt("dve", kt24[:, :, 1, :], kq4[:, :, 0, :], sn4[:, :, 1, :], ALU.mult, [("kq", s2), "SIN"], [("kt2", s2)])
                yield
                tt("dve", kr[s2], kt1[s2], kt2[s2], ALU.add, [("kt1", s2), ("kt2", s2)], [("kr", s2)])
                yield
            for _ in range(KGAP):
                yield
            tr(bank_bf(7, 128, 512 + s2 * 128), kr[s2], ident_b, [("kr", s2), "ident_b"], [PB[7]])
            copy("dve", kTa[j][:, t * 128:(t + 1) * 128], bank_bf(7, 128, 512 + s2 * 128), [PB[7]], [("kTa", j, t)])
            for _y in range(KYLD):
                yield

        def gen_q(j, jt):
            t = jt + 2
            bk, s2 = 6, jt % 2
            for k in range(8):
                mm(bank(bk, 512), xmodT[:, k, t * 128:(t + 1) * 128], waq[j][:, k, :], k == 0, k == 7,
                   [("xmodT", t), ("waq", j)], [PB[bk]])
            copy("act", qs[s2], bank(bk, 512), [PB[bk]], [("qs", s2)])
            for _y in range(KYLD):
                yield
            tt("dve", qt1[s2], qs[s2], qs[s2], ALU.mult, [("qs", s2)], [("qt1", s2)])
            for _y in range(KYLD):
                yield
            reduce(ssq4[s2], qt1[s2].rearrange("p (g c) -> p g c", g=4), AX.X, ALU.add, [("qt1", s2)], [("ssq4", s2)])
            for _y in range(KYLD):
                yield
            ts("dve", ssq4[s2], ssq4[s2], 1.0 / 128, EPS, ALU.mult, ALU.add, [("ssq4", s2)], [("ssq4", s2)])
            for _y in range(KYLD):
                yield
            act(ssq4[s2], ssq4[s2], AF.Sqrt, [("ssq4", s2)], [("ssq4", s2)])
            for _y in range(KYLD):
                yield
            recip(ssq4[s2], ssq4[s2], [("ssq4", s2)], [("ssq4", s2)])
            for _y in range(KYLD):
                yield
            qs3 = qs[s2].rearrange("p (g c) -> p g c", g=4)
            qn3 = qn[s2].rearrange("p (g c) -> p g c", g=4)
            tt("dve", qn3, qs3, ssq4[s2].unsqueeze(2).to_broadcast([128, 4, 128]), ALU.mult, [("qs", s2), ("ssq4", s2)], [("qn", s2)])
            for _y in range(KYLD):
                yield
            tt("pool", qn3, qn3, qw_row.unsqueeze(1).to_broadcast([128, 4, 128]), ALU.mult, [("qn", s2), "qw"], [("qn", s2)])
            for _y in range(KYLD):
                yield
            tt("pool", qt1[s2].rearrange("p (g c) -> p g c", g=4), qn3,
               COSt[:, jt, :].unsqueeze(1).to_broadcast([128, 4, 128]), ALU.mult, [("qn", s2), "COS"], [("qt1", s2)])
            for _y in range(KYLD):
                yield
            qn5, qt25, sn4 = halves(qn[s2], 4), halves(qs[s2], 4), halves(SINt[:, jt, :])
            tt("dve", qt25[:, :, :, 0, :], qn5[:, :, :, 1, :], sn4[:, :, 0, :].unsqueeze(1).to_broadcast([128, 4, 2, 32]),
               ALU.mult, [("qn", s2), "SIN"], [("qs", s2)])
            for _y in range(KYLD):
                yield
            tt("pool", qt25[:, :, :, 1, :], qn5[:, :, :, 0, :], sn4[:, :, 1, :].unsqueeze(1).to_broadcast([128, 4, 2, 32]),
               ALU.mult, [("qn", s2), "SIN"], [("qs", s2)])
            for _y in range(KYLD):
                yield
            tt("dve", qr[s2], qt1[s2], qs[s2], ALU.add, [("qt1", s2), ("qs", s2)], [("qr", s2)])
            for _y in range(KYLD):
                yield
            for _ in range(KGAP):
                yield
            for g in range(4):
                tr(bank_bf(7, 128, g * 128), qr[s2][:, g * 128:(g + 1) * 128], ident_b, [("qr", s2), "ident_b"], [PB[7]])
            copy("act", qTa[j][:, :, jt * 128:(jt + 1) * 128], bank_bf(7, 512, 0).rearrange("p (g c) -> p g c", g=4),
                 [PB[7]], [("qTa", j, jt // 4)])
            for _y in range(KYLD):
                yield

        def prep_pumps(j):
            return [Pump([gen_k(j, t) for t in range(NT)], 2), Pump([gen_q(j, jt) for jt in range(NT_LAT)], 2)]

        def core_block(j, g, qb, itn, pumps):
            bO = 2
            bD = 4
            qsl = qTa[j][:, g, qb * 512:(qb + 1) * 512]

            def s_mm(kt):
                bS = (0, 1, 3)[kt % 3]
                mm(bank(bS, 512), kTa[j][:, kt * 128:(kt + 1) * 128], qsl, True, True, [("kTa", j, kt), ("qTa", j, qb)], [PB[bS]])
                act(Pt[kt % 3], bank(bS, 512), AF.Exp, [PB[bS]], [("Pt", kt % 3)], scale=SC)

            s_mm(0)
            s_mm(1)
            for kt in range(NT):
                if kt + 2 < NT:
                    s_mm(kt + 2)
                mm(bank(bO, 512), Va[j][:, kt, :], Pt[kt % 3], kt == 0, kt == NT - 1, [("Va", j, kt), ("Pt", kt % 3)], [PB[bO]])
                if not DACC:
                    mm(bank(bD, 512), ones_b, Pt[kt % 3], kt == 0, kt == NT - 1, ["ones_b", ("Pt", kt % 3)], [PB[bD]])
                else:
                    ae, ai = ("pool", 1) if kt % 3 == 2 else ("dve", 0)
                    pa = Pacc[itn % 2][ai]
                    ka = ("Pacc", itn % 2, ai)
                    if kt == 0 or kt == 2:
                        copy(ae, pa, Pt[kt % 3], [("Pt", kt % 3)], [ka])
                    else:
                        tt(ae, pa, pa, Pt[kt % 3], ALU.add, [ka, ("Pt", kt % 3)], [ka])
                for p_ in pumps:
                    p_.step(int(os.environ.get("KPS", "2")))
            if DACC:
                mm(bank(bD, 512), ones_f, Pacc[itn % 2][0], True, False, ["ones_f", ("Pacc", itn % 2, 0)], [PB[bD]])
                mm(bank(bD, 512), ones_f, Pacc[itn % 2][1], False, True, ["ones_f", ("Pacc", itn % 2, 1)], [PB[bD]])
            r2 = itn % 2
            recip(rD[r2], bank(bD, 512), [PB[bD]], [("rD", r2)])
            tt("dve", oT[:, j * 4 + g, qb * 512:(qb + 1) * 512], bank(bO, 512), rD[r2], ALU.mult,
               [PB[bO], ("rD", r2)], [("oT", j * 4 + g)])

        load_aw(0)
        load_aw(1)
        kpump = Pump([gen_k(j_, t) for j_ in range(2) for t in range(NT)], 2)
        qpump = Pump([gen_q(j_, jt) for j_ in range(2) for jt in range(NT_LAT)], 2)
        itn = 0
        pumps = [kpump, qpump]
        for j in range(2):
            for qb in range(4):
                while kpump.completed < NT * (j + 1) or qpump.completed < NT_LAT * j + 4 * (qb + 1):
                    if kpump.completed < NT * (j + 1):
                        kpump.step(1)
                    if qpump.completed < NT_LAT * j + 4 * (qb + 1):
                        qpump.step(1)
                for g in range(4):
                    if os.environ.get("KSKIPCORE") is None:
                        core_block(j, g, qb, itn, pumps)
                    itn += 1
        for p_ in pumps:
            p_.drain()
        OT_ALL = [("oT", h) for h in range(8)]
        if b == 0 and "oT" in dbg_d:
            for h in range(8):
                dump("oT", oT[:, h, :], OT_ALL, dst=dbg_d["oT"][h * 128:(h + 1) * 128, :])
        for h in range(8):
            dma("sp", otscr_d[b, :, h * (SEQ // 2):(h + 1) * (SEQ // 2)], oT_w[:, h * (SEQ // 2):(h + 1) * (SEQ // 2)],
                [("oT", h)], [("otscr", h)], "otst")
        S.barrier()
        A.release(m3)
        if stop_after == "3":
            return finish(nc, S, A)

        hmgT = A.bf16(8 * SEQ).rearrange("p (k t) -> p k t", k=8)
        m2 = A.mark()
        wgt = A.bf16(8 * 32).rearrange("p (k c) -> p k c", k=8)
        AA = [A.f32(NT * 8).rearrange("p (t h) -> p t h", h=8) for _ in range(2)]
        FL = [A.f32(NT * 8).rearrange("p (t h) -> p t h", h=8) for _ in range(2)]
        EB = [A.f32(NT * 8).rearrange("p (t h) -> p t h", h=8) for _ in range(2)]
        wsl_flat = [A.bf16(8 * 512).rearrange("p (k c) -> p k c", k=8) for _ in range(2)]
        wsl = [w_.rearrange("p k (f c) -> p k f c", f=4) for w_ in wsl_flat]
        qT = [A.bf16(NTOK) for _ in range(2)]
        kT = [A.bf16(NTOK) for _ in range(2)]
        Kt = [A.bf16(NT * 128).rearrange("p (t c) -> p t c", c=128) for _ in range(2)]
        Vv = [A.bf16(NT * 130).rearrange("p (t c) -> p t c", c=130) for _ in range(2)]
        vT = A.bf16(NTOK)
        ogT = [A.bf16(SEQ) for _ in range(2)]
        Tst = [A.f32(136) for _ in range(2)]
        VP = [A.bf16(NT * 130).rearrange("p (t c) -> p t c", c=130) for _ in range(2)]
        CBs = [A.bf16(NT * 130).rearrange("p (t c) -> p t c", c=130) for _ in range(2)]
        W_OUT = 6
        SpS = [A.bf16(128) for _ in range(W_OUT)]
        denS = [A.f32(1) for _ in range(W_OUT)]
        ssq = A.f32(NT_LAT)
        rs16 = A.f32(NT_LAT)
        hg4 = [A.bf16(128) for _ in range(4)]
        sqj = A.f32(128)
        m2g = A.mark()
        G = A.f32(NT * 32).rearrange("p (t c) -> p t c", c=32)
        SP_ = [A.f32(NT * 8).rearrange("p (t h) -> p t h", h=8) for _ in range(2)]
        tmpA = A.f32(NT * 8).rearrange("p (t h) -> p t h", h=8)
        tri = [trif, trib]
        trik = ["trif", "trib"]

        dma("pool", wgt, win_v[:, :, 4096:4128], [], ["wgt"], "wgt")
        for hb in range(2):
            memset("pool", Vv[hb][:, :, 128:129], 1.0, [("Vv1", hb)])
        for t in range(NT):
            bk, col = (0, t * 32) if t < 16 else (1, (t - 16) * 32)
            for k in range(8):
                mm(bank(bk, 32, col), xmodT[:, k, t * 128:(t + 1) * 128], wgt[:, k, :], k == 0, k == 7,
                   [("xmodT", t), "wgt"], [PB[bk]])
        if stop_after == "2a1":
            return finish(nc, S, A)
        tt("dve", G[:, 0:16, :], bank(0).rearrange("p (t c) -> p t c", c=32),
           bmg_row.unsqueeze(1).to_broadcast([128, 16, 32]), ALU.add, [PB[0], "bmg"], ["G"])
        tt("dve", G[:, 16:18, :], bank(1, 64).rearrange("p (t c) -> p t c", c=32),
           bmg_row.unsqueeze(1).to_broadcast([128, 2, 32]), ALU.add, [PB[1], "bmg"], ["G"])
        if stop_after == "2a2":
            return finish(nc, S, A)
        for d_ in range(2):
            fo = 8 + 16 * d_
            io = 16 * d_
            act(tmpA, G[:, :, fo:fo + 8], AF.Exp, ["G"], ["tmpA"], scale=-1.0)
            act(SP_[d_], tmpA, AF.Ln, ["tmpA"], [("SP", d_)], bias=1.0)
            spf = SP_[d_].rearrange("p t h -> p (t h)")
            mm(bank(2, 144, 0), tri[d_], spf, True, True, [trik[d_], ("SP", d_)], [PB[2]])
            mm(bank(3, 144, 0), ones_f, spf, True, True, ["ones_f", ("SP", d_)], [PB[3]])
            cum3 = bank(2, 144, 0).rearrange("p (t h) -> p t h", h=8)
            tot3 = bank(3, 144, 0).rearrange("p (t h) -> p t h", h=8)
            tt("dve", tmpA, G[:, :, io:io + 8], cum3, ALU.add, ["G", PB[2]], ["tmpA"])
            if stop_after == "2a3":
                return finish(nc, S, A)
            act(AA[d_], tmpA, AF.Exp, ["tmpA"], [("AA", d_)])
            act(FL[d_], cum3, AF.Exp, [PB[2]], [("FL", d_)])
            act(EB[d_], tot3, AF.Exp, [PB[3]], [("EB", d_)], scale=-1.0)
        if b == 0:
            dump("G", G.rearrange("p t c -> p (t c)"), ["G"])
            for d_ in range(2):
                dump("AA%d" % d_, AA[d_].rearrange("p t h -> p (t h)"), [("AA", d_)])
                dump("FL%d" % d_, FL[d_].rearrange("p t h -> p (t h)"), [("FL", d_)])
                dump("EB%d" % d_, EB[d_].rearrange("p t h -> p (t h)"), [("EB", d_)])
        if stop_after == "2a":
            return finish(nc, S, A)
        S.barrier()
        A.release(m2g)
        Hs = [A.f32(NT_LAT * 128).rearrange("p (t c) -> p t c", c=128) for _ in range(2)]
        order = [list(range(NT)), [1, 0] + list(range(NT - 1, 1, -1))]
        KDIR = os.environ.get("KDIR")
        KDIR = int(KDIR) if KDIR is not None else None
        QS = float(128 ** -0.5)

        def load_head_w(h):
            sl = h % 2
            dma("pool", wsl_flat[sl], win_v[:, :, h * 512:(h + 1) * 512], [], [("wsl", sl)], "wsl%d" % sl)

        def gen_proj(h):
            sl = h % 2
            hb = h % 2
            W = wsl[sl]
            wk_ = ("wsl", sl)
            cbs = [(0, 512), (512, 512), (1024, 512), (1536, 512), (2048, 256)]
            n = 0
            for ci, (c0, cn) in enumerate(cbs):
                tl = [("xmodT", t) for t in range(c0 // 128, (c0 + cn) // 128)]
                for f_, dst, nm in ((0, qT[hb], ("qT", hb, ci)), (1, kT[hb], ("kT", hb, ci)), (2, vT, ("vT", ci))):
                    bk = 6 + n % 2
                    n += 1
                    for k in range(8):
                        mm(bank(bk, cn), W[:, k, f_, :], xmodT[:, k, c0:c0 + cn], k == 0, k == 7, tl + [wk_], [PB[bk]])
                    if f_ == 0:
                        act(dst[:, c0:c0 + cn], bank(bk, cn), AF.Copy, [PB[bk]], [nm], scale=QS)
                    elif f_ == 1:
                        copy("dve", dst[:, c0:c0 + cn], bank(bk, cn), [PB[bk]], [nm])
                    else:
                        copy("act", dst[:, c0:c0 + cn], bank(bk, cn), [PB[bk]], [nm])
                    yield
                nt_ = cn // 128
                t0 = c0 // 128
                for src, dst3, nm in ((kT[hb], Kt[hb], "Kt"), (vT, Vv[hb], "Vv")):
                    bk = 6 + n % 2
                    n += 1
                    rk = ("kT", hb, ci) if nm == "Kt" else ("vT", ci)
                    for q4 in range(nt_):
                        tr(bank_bf(bk, 128, q4 * 128), src[:, c0 + q4 * 128:c0 + (q4 + 1) * 128], ident_b, [rk, "ident_b"], [PB[bk]])
                    copy("dve" if nm == "Kt" else "act", dst3[:, t0:t0 + nt_, 0:128],
                         bank_bf(bk, nt_ * 128, 0).rearrange("p (t c) -> p t c", c=128), [PB[bk]],
                         [(nm, hb, t0 + q4) for q4 in range(nt_)])
                    yield
            for g4 in range(4):
                bk = 6 + n % 2
                n += 1
                xsl = slice(256 + g4 * 512, 256 + (g4 + 1) * 512)
                tl = [("xmodT", 2 + g4 * 4 + q4) for q4 in range(4)]
                for k in range(8):
                    mm(bank(bk, 512), W[:, k, 3, :], xmodT[:, k, xsl], k == 0, k == 7, tl + [wk_], [PB[bk]])
                act(ogT[hb][:, g4 * 512:(g4 + 1) * 512], bank(bk, 512), AF.Sigmoid, [PB[bk]], [("ogT", hb, g4)])
                yield
            if h + 2 < 8:
                load_head_w(h + 2)

        DC_REG = [[(0, 0), (1, 0)], [(2, 0), (3, 0)]]
        S_REG = [(0, 0), (0, 128), (0, 256), (0, 384), (1, 0), (1, 128)]
        X_REG = [(2, 0), (2, 136), (2, 272), (3, 0), (3, 136), (3, 272)]

        def gen_state(h, d_):
            hb = h % 2
            for i in range(NT):
                t = order[d_][i]
                ts("pool", VP[d_][:, t, 0:129], Vv[hb][:, t, 0:129], AA[d_][:, t, h:h + 1], 1.0, ALU.mult, ALU.mult,
                   [("Vv", hb, t), ("Vv1", hb), ("AA", d_)], [("VP", d_, t)])
                yield
                if i < NT - 1:
                    bC, cC = DC_REG[d_][i % 2]
                    kC = PB[bC]
                    mm(bank(bC, 129, cC), Kt[hb][:, t, :], VP[d_][:, t, 0:129], True, True, [("Kt", hb, t), ("VP", d_, t)], [kC])
                    yield
                    if i == 0:
                        copy("dve", Tst[d_][:, 0:129], bank(bC, 129, cC), [kC], [("T", d_)])
                    else:
                        tp = order[d_][i - 1]
                        stt(Tst[d_][:, 0:129], Tst[d_][:, 0:129], EB[d_][:, tp, h:h + 1], bank(bC, 129, cC), ALU.mult, ALU.add,
                            [("T", d_), ("EB", d_), kC], [("T", d_)])
                    yield
                    tn = order[d_][i + 1]
                    act(CBs[d_][:, tn, 0:129], Tst[d_][:, 0:129], AF.Identity, [("T", d_), ("EB", d_)], [("CB", d_, tn)],
                        scale=EB[d_][:, t, h:h + 1])
                    yield

        def gen_out(h, d_, t, slot):
            hb = h % 2
            ci = min(t // 4, 4)
            tsl = slice(t * 128, (t + 1) * 128)
            bS, cS = S_REG[slot]
            kS = PB[bS]
            mm(bank(bS, 128, cS), kT[hb][:, tsl], qT[hb][:, tsl], True, True, [("kT", hb, ci), ("qT", hb, ci)], [kS])
            yield
            tt("dve", SpS[slot], bank(bS, 128, cS), tri[d_], ALU.mult, [kS, trik[d_]], [("SpS", slot)])
            yield
            bX, cX = X_REG[slot]
            mm(bank(bX, 129, cX), SpS[slot], VP[d_][:, t, 0:129], True, False, [("SpS", slot), ("VP", d_, t)], [PB[bX]])
            mm(bank(bX, 129, cX), qT[hb][:, tsl], CBs[d_][:, t, 0:129], False, True, [("qT", hb, ci), ("CB", d_, t)], [PB[bX]])
            yield
            act(denS[slot], bank(bX, 1, cX + 128), AF.Abs, [PB[bX]], [("den", slot)])
            yield
            ts("dve", denS[slot], denS[slot], FL[d_][:, t, h:h + 1], None, ALU.max, None,
               [("den", slot), ("FL", d_)], [("den", slot)])
            yield
            recip(denS[slot], denS[slot], [("den", slot)], [("den", slot)])
            yield
            if d_ == 0:
                act(Hs[hb][:, t - 2, :], bank(bX, 128, cX), AF.Identity, [PB[bX], ("den", slot)], [("Hs", hb, t - 2)],
                    scale=denS[slot])
            else:
                stt(Hs[hb][:, t - 2, :], bank(bX, 128, cX), denS[slot], Hs[hb][:, t - 2, :], ALU.mult, ALU.add,
                    [PB[bX], ("den", slot), ("Hs", hb, t - 2)], [("Hs", hb, t - 2)])
            yield

        def gen_norm(h):
            hb = h % 2
            for j in range(NT_LAT):
                act(sqj, Hs[hb][:, j, :], AF.Square, [("Hs", hb, j)], ["sqj", "ssq"], accum_out=ssq[:, j:j + 1])
                yield
            ts("dve", rs16, ssq, 1.0 / 128, EPS, ALU.mult, ALU.add, ["ssq"], ["rs16"])
            act(rs16, rs16, AF.Sqrt, ["rs16"], ["rs16"])
            recip(rs16, rs16, ["rs16"], ["rs16"])
            yield
            for g4 in range(4):
                for j4 in range(4):
                    j = g4 * 4 + j4
                    stt(hg4[j4], Hs[hb][:, j, :], rs16[:, j:j + 1], mhw_row[:, h * 128:(h + 1) * 128], ALU.mult, ALU.mult,
                        [("Hs", hb, j), "rs16", "mhw"], [("hg4", j4)])
                    yield
                for j4 in range(4):
                    tr(bank_bf(5, 128, 512 + j4 * 128), hg4[j4], ident_b, [("hg4", j4), "ident_b"], [PB[5]])
                tt("dve", hmgT[:, h, g4 * 512:(g4 + 1) * 512], bank_bf(5, 512, 512), ogT[hb][:, g4 * 512:(g4 + 1) * 512], ALU.mult,
                   [PB[5], ("ogT", hb, g4)], [("hmgT", h)])
                yield

        load_head_w(0)
        load_head_w(1)
        Pump([gen_proj(0)], 1).drain()
        prev_norm = None
        for h in range(8):
            stp = Pump([gen_state(h, 0), gen_state(h, 1)], 2)
            projp = Pump([gen_proj(h + 1)], 1) if h + 1 < 8 else None
            cnt_ = [0]

            def side():
                cnt_[0] += 1
                if prev_norm is not None and cnt_[0] % 3 == 0:
                    prev_norm.step(1)
                if projp is not None and cnt_[0] % 8 == 0:
                    projp.step(1)

            for d_ in range(2):
                memset("pool", Tst[d_], 0.0, [("T", d_)])
            while not stp.done():
                stp.step(1)
                side()
            items = [(0, t) for t in range(2, NT)] + [(1, t) for t in range(2, NT)]
            if os.environ.get("KNOOUT"):
                items = []
            if os.environ.get("KNITEMS"):
                items = items[:int(os.environ["KNITEMS"])]
            slots = [None] * W_OUT
            while items or any(g_ is not None for g_ in slots):
                for k_ in range(W_OUT):
                    if slots[k_] is None and items:
                        d_, t_ = items.pop(0)
                        slots[k_] = gen_out(h, d_, t_, k_)
                    if slots[k_] is not None:
                        try:
                            next(slots[k_])
                        except StopIteration:
                            slots[k_] = None
                        side()
            if prev_norm is not None:
                prev_norm.drain()
            if projp is not None:
                projp.drain()
            prev_norm = Pump([gen_norm(h)], 1)
            if b == 0 and "Hs%d" % h in dbg_d:
                dump("Hs%d" % h, Hs[h % 2].rearrange("p t c -> p (t c)"), [("Hs", h % 2, j) for j in range(NT_LAT)])
        prev_norm.drain()
        HMG_ALL = [("hmgT", h) for h in range(8)]
        if b == 0 and "hmgT" in dbg_d:
            for h in range(8):
                dump("hmgT", hmgT[:, h, :], HMG_ALL, dst=dbg_d["hmgT"][h * 128:(h + 1) * 128, :])
        S.barrier()
        A.release(m2)
        if stop_after == "2":
            return finish(nc, S, A)


        m4 = A.mark()
        oT = A.bf16(8 * SEQ).rearrange("p (k t) -> p k t", k=8)
        oT_w = A.last
        for h in range(8):
            dma("sp", oT_w[:, h * (SEQ // 2):(h + 1) * (SEQ // 2)], otscr_d[b, :, h * (SEQ // 2):(h + 1) * (SEQ // 2)],
                [("otscr", h)], [("oT", h)], "otld")
        zT_off = A.mark()
        zT = A.bf16(8 * SEQ).rearrange("p (k t) -> p k t", k=8)
        wbm = A.bf16(8 * D).rearrange("p (k c) -> p k c", k=8)
        wba = A.bf16(8 * D).rearrange("p (k c) -> p k c", k=8)
        wbg = [A.bf16(8 * 256).rearrange("p (k c) -> p k c", k=8) for _ in range(2)]
        sg0 = A.f32(512)
        sg1 = A.f32(512)
        zt0 = A.f32(512)
        zt1 = A.f32(512)
        for k in range(8):
            dma("pool", wbm[:, k, :], wbm_d[k * 128:(k + 1) * 128, :], [], ["wbm"], "wbm")
            dma("pool", wba[:, k, :], wba_d[k * 128:(k + 1) * 128, :], [], ["wba"], "wba")
        it = 0
        for c in range(8):
            sl = c % 2
            dma("pool", wbg[sl], win_v[:, :, 5664 + c * 256: 5664 + (c + 1) * 256], [], [("wbg", sl)], "wbg%d" % sl)
            for tb in range(4):
                pb0 = 4 * (it % 2)
                it += 1
                tsl = slice(tb * 512, (tb + 1) * 512)
                xsl = slice(256 + tb * 512, 256 + (tb + 1) * 512)
                xk = [("xmodT", 2 + tb * 4 + q4) for q4 in range(4)]
                for k in range(8):
                    mm(bank(pb0, 512), wbm[:, k, c * 128:(c + 1) * 128], hmgT[:, k, tsl], k == 0, k == 7, ["wbm", ("hmgT", k)], [PB[pb0]])
                for k in range(8):
                    mm(bank(pb0 + 1, 512), wba[:, k, c * 128:(c + 1) * 128], oT[:, k, tsl], k == 0, k == 7, ["wba", ("oT", k)], [PB[pb0 + 1]])
                for k in range(8):
                    mm(bank(pb0 + 2, 512), wbg[sl][:, k, 0:128], xmodT[:, k, xsl], k == 0, k == 7, [("wbg", sl)] + xk, [PB[pb0 + 2]])
                for k in range(8):
                    mm(bank(pb0 + 3, 512), wbg[sl][:, k, 128:256], xmodT[:, k, xsl], k == 0, k == 7, [("wbg", sl)] + xk, [PB[pb0 + 3]])
                act(sg0, bank(pb0 + 2, 512), AF.Sigmoid, [PB[pb0 + 2]], ["sg0"])
                act(sg1, bank(pb0 + 3, 512), AF.Sigmoid, [PB[pb0 + 3]], ["sg1"])
                tt("dve", zt0, bank(pb0, 512), sg0, ALU.mult, [PB[pb0], "sg0"], ["zt0"])
                tt("dve", zt1, bank(pb0 + 1, 512), sg1, ALU.mult, [PB[pb0 + 1], "sg1"], ["zt1"])
                tt("pool", zT[:, c, tsl], zt0, zt1, ALU.add, ["zt0", "zt1"], [("zT", tb)])
        S.barrier()
        if stop_after == "4a":
            return finish(nc, S, A)

        A.release(mB)
        fT = A.bf16(8 * SEQ).rearrange("p (k t) -> p k t", k=8)
        m5 = A.mark()
        LG = A.f32(NT_LAT * 36).rearrange("p (t c) -> p t c", c=36)
        m4b = A.mark()
        wo = A.bf16(8 * D).rearrange("p (k c) -> p k c", k=8)
        wr = A.f32(8 * 36).rearrange("p (k c) -> p k c", k=8)
        xt2 = [A.f32(D) for _ in range(2)]
        hm = [A.f32(D) for _ in range(2)]
        hn = [A.f32(D) for _ in range(2)]
        fr = [A.f32(D).rearrange("p (k c) -> p k c", k=8) for _ in range(2)]
        junk2 = A.bf16(D)
        ss2 = [A.f32(1) for _ in range(2)]
        assert A.top <= zT_off, (A.top, zT_off)
        for k in range(8):
            dma("pool", wo[:, k, :], wo_d[k * 128:(k + 1) * 128, :], [], ["wo"], "wo")
        dma("sp", wr, wr_d.rearrange("(k p) c -> p k c", p=128), [], ["wr"], "wr")
        def gen_4b(jt):
            s2 = jt % 2
            dma("sp", xt2[s2], x_d[b, jt * 128:(jt + 1) * 128, :], [], [("xt2", s2)], "xt2%d" % s2)
            yield
            for hf in range(2):
                for k in range(8):
                    mm(bank(hf, 512), zT[:, k, jt * 128:(jt + 1) * 128], wo[:, k, hf * 512:(hf + 1) * 512], k == 0, k == 7,
                       [("zT", jt // 4), "wo"], [PB[hf]])
                hsl = slice(hf * 512, (hf + 1) * 512)
                tt("dve", hm[s2][:, hsl], bank(hf, 512), grow[b][0][:, hsl], ALU.mult, [PB[hf], ("grow", b, 0)], [("hm", s2)])
                yield
                tt("dve", hm[s2][:, hsl], hm[s2][:, hsl], xt2[s2][:, hsl], ALU.add, [("hm", s2), ("xt2", s2)], [("hm", s2)])
                yield
            dma("sp", hmid_d[b, jt * 128:(jt + 1) * 128, :], hm[s2], [("hm", s2)], [("hmid", jt)], "hmst%d" % s2)
            act(junk2, hm[s2], AF.Square, [("hm", s2)], ["junk2", ("ss2", s2)], accum_out=ss2[s2])
            yield
            ts("dve", ss2[s2], ss2[s2], 1.0 / D, EPS, ALU.mult, ALU.add, [("ss2", s2)], [("ss2", s2)])
            yield
            act(ss2[s2], ss2[s2], AF.Sqrt, [("ss2", s2)], [("ss2", s2)])
            yield
            recip(ss2[s2], ss2[s2], [("ss2", s2)], [("ss2", s2)])
            yield
            act(hn[s2], hm[s2], AF.Identity, [("hm", s2), ("ss2", s2)], [("hn", s2)], scale=ss2[s2])
            yield
            for hf in range(2):
                bt = 2 + 2 * s2 + hf
                for k4 in range(4):
                    k = hf * 4 + k4
                    tr(bank(bt, 128, k4 * 128), hn[s2][:, k * 128:(k + 1) * 128], ident_f, [("hn", s2), "ident_f"], [PB[bt]])
                yield
                for k4 in range(4):
                    k = hf * 4 + k4
                    ts("dve", fr[s2][:, k, :], bank(bt, 128, k4 * 128), A2[:, b * 8 + k:b * 8 + k + 1], modT3[:, 24 + k, b:b + 1],
                       ALU.mult, ALU.add, [PB[bt], "A2", "modT"], [("fr", s2)])
                yield
            copy("act", fT[:, :, jt * 128:(jt + 1) * 128], fr[s2], [("fr", s2)], [("fT", jt // 4)])
            yield
            bl = 6 + s2
            for k in range(8):
                mm(bank(bl, 36), fr[s2][:, k, :], wr[:, k, :], k == 0, k == 7, [("fr", s2), "wr"], [PB[bl]])
            yield
            tt("dve", LG[:, jt, :], bank(bl, 36), br_row, ALU.add, [PB[bl], "br"], ["LG"])
            yield

        Pump([gen_4b(jt) for jt in range(NT_LAT)], 2).drain()
        if b == 0 and "fT" in dbg_d:
            for k in range(8):
                dump("fT", fT[:, k, :], [("fT", q4) for q4 in range(4)], dst=dbg_d["fT"][k * 128:(k + 1) * 128, :])
        if b == 0:
            dump("LG", LG.rearrange("p t c -> p (t c)"), ["LG"])
        S.barrier()
        A.release(m4b)
        if stop_after == "4b":
            return finish(nc, S, A)

        CW = A.f32(NT_LAT * 32).rearrange("p (t e) -> p t e", e=32)
        m5b = A.mark()
        BIG = 1.0e30
        gmax = A.f32(16)
        goh = A.f32(64).rearrange("p (t g) -> p t g", g=4)
        gsh = A.f32(64).rearrange("p (t g) -> p t g", g=4)
        gsum = A.f32(16)
        pgrp = A.f32(16)
        negm = A.f32(64).rearrange("p (t g) -> p t g", g=4)
        em = A.f32(512).rearrange("p (t e) -> p t e", e=32)
        em2 = A.f32(512).rearrange("p (t e) -> p t e", e=32)
        oh1 = A.f32(512).rearrange("p (t e) -> p t e", e=32)
        oh2 = A.f32(512).rearrange("p (t e) -> p t e", e=32)
        v1 = A.f32(16)
        v2 = A.f32(16)
        w1 = A.f32(16)
        w2 = A.f32(16)
        gl = LG[:, :, 0:4]
        el4 = LG[:, :, 4:36].rearrange("p t (g e) -> p t g e", g=4)
        bc4 = lambda a: a.unsqueeze(2).to_broadcast([128, 16, 4])
        bc32 = lambda a: a.unsqueeze(2).to_broadcast([128, 16, 32])
        reduce(gmax, gl, AX.X, ALU.max, ["LG"], ["gmax"])
        tt("dve", goh, gl, bc4(gmax), ALU.is_equal, ["LG", "gmax"], ["goh"])
        tt("dve", gsh, gl, bc4(gmax), ALU.subtract, ["LG", "gmax"], ["gsh"])
        act(gsh, gsh, AF.Exp, ["gsh"], ["gsh"])
        reduce(gsum, gsh, AX.X, ALU.add, ["gsh"], ["gsum"])
        recip(pgrp, gsum, ["gsum"], ["pgrp"])
        ts("dve", negm, goh, BIG, -BIG, ALU.mult, ALU.add, ["goh"], ["negm"])
        tt("dve", em.rearrange("p t (g e) -> p t g e", g=4), el4, negm.unsqueeze(3).to_broadcast([128, 16, 4, 8]), ALU.add,
           ["LG", "negm"], ["em"])
        reduce(v1, em, AX.X, ALU.max, ["em"], ["v1"])
        tt("dve", oh1, em, bc32(v1), ALU.is_equal, ["em", "v1"], ["oh1"])
        stt(em2.rearrange("p t e -> p (t e)"), oh1.rearrange("p t e -> p (t e)"), -BIG, em.rearrange("p t e -> p (t e)"),
            ALU.mult, ALU.add, ["oh1", "em"], ["em2"])
        reduce(v2, em2, AX.X, ALU.max, ["em2"], ["v2"])
        tt("dve", oh2, em2, bc32(v2), ALU.is_equal, ["em2", "v2"], ["oh2"])
        tt("dve", w1, v1, v2, ALU.subtract, ["v1", "v2"], ["w1"])
        act(w1, w1, AF.Sigmoid, ["w1"], ["w1"])
        tt("dve", w1, w1, pgrp, ALU.mult, ["w1", "pgrp"], ["w1"])
        tt("dve", w2, pgrp, w1, ALU.subtract, ["pgrp", "w1"], ["w2"])
        tt("dve", CW, oh1, bc32(w1), ALU.mult, ["oh1", "w1"], ["CW"])
        tt("dve", oh2, oh2, bc32(w2), ALU.mult, ["oh2", "w2"], ["oh2"])
        tt("dve", CW, CW, oh2, ALU.add, ["CW", "oh2"], ["CW"])
        if b == 0:
            dump("CW", CW.rearrange("p t e -> p (t e)"), ["CW"])
        if stop_after == "5b":
            return finish(nc, S, A)

        acc = A.f32(NT_LAT * D).rearrange("p (t c) -> p t c", c=D)
        wgu = [A.bf16(8 * 512).rearrange("p (k c) -> p k c", k=8) for _ in range(2)]
        wdn = [A.bf16(2 * D).rearrange("p (k c) -> p k c", k=2) for _ in range(2)]
        sgl = [A.f32(512) for _ in range(2)]
        hT = [[A.bf16(512) for _ in range(2)] for _ in range(2)]
        hm2 = [A.f32(D) for _ in range(2)]
        ot = [A.f32(D) for _ in range(2)]
        ysc = [A.f32(512) for _ in range(2)]
        NE = NEXP if stop_after != "5c1" else 2

        def load_expert(e):
            sl = e % 2
            dma("pool", wgu[sl], wgu_d[e].rearrange("(k p) c -> p k c", p=128), [], [("wgu", sl)], "wgu%d" % sl)
            dma("pool", wdn[sl], wed_d[e].rearrange("(k p) c -> p k c", p=128), [], [("wdn", sl)], "wdn%d" % sl)

        load_expert(0)
        ity = [0]

        def moe_gu(e, tb, fc):
            sl = e % 2
            tsl = slice(tb * 512, (tb + 1) * 512)
            for k in range(8):
                mm(bank(fc, 512), wgu[sl][:, k, fc * 128:(fc + 1) * 128], fT[:, k, tsl], k == 0, k == 7,
                   [("wgu", sl), ("fT", tb)], [PB[fc]])
            for k in range(8):
                mm(bank(2 + fc, 512), wgu[sl][:, k, 256 + fc * 128:256 + (fc + 1) * 128], fT[:, k, tsl], k == 0, k == 7,
                   [("wgu", sl), ("fT", tb)], [PB[2 + fc]])
            act(sgl[fc], bank(fc, 512), AF.Silu, [PB[fc]], [("sgl", fc)])
            tt("dve", hT[tb % 2][fc], bank(2 + fc, 512), sgl[fc], ALU.mult, [PB[2 + fc], ("sgl", fc)], [("hT", tb % 2, fc)])

        def moe_down(e, tb):
            sl = e % 2
            for q4 in range(4):
                jt = tb * 4 + q4
                by = 4 + 2 * (ity[0] % 2)
                ity[0] += 1
                for hf in range(2):
                    for fc in range(2):
                        mm(bank(by + hf, 512), hT[tb % 2][fc][:, q4 * 128:(q4 + 1) * 128], wdn[sl][:, fc, hf * 512:(hf + 1) * 512],
                           fc == 0, fc == 1, [("hT", tb % 2, fc), ("wdn", sl)], [PB[by + hf]])
                    hsl = slice(hf * 512, (hf + 1) * 512)
                    if e == 0:
                        ts("dve", acc[:, jt, hsl], bank(by + hf, 512), CW[:, jt, e:e + 1], None, ALU.mult, None,
                           [PB[by + hf], "CW"], [("acc", jt, hf)])
                    elif hf == 1:
                        sy = ysc[ity[0] % 2]
                        act(sy, bank(by + hf, 512), AF.Identity, [PB[by + hf], "CW"], [("ysc", ity[0] % 2)], scale=CW[:, jt, e:e + 1])
                        tt("pool", acc[:, jt, hsl], acc[:, jt, hsl], sy, ALU.add, [("ysc", ity[0] % 2), ("acc", jt, hf)], [("acc", jt, hf)])
                    else:
                        stt(acc[:, jt, hsl], bank(by + hf, 512), CW[:, jt, e:e + 1], acc[:, jt, hsl], ALU.mult, ALU.add,
                            [PB[by + hf], "CW", ("acc", jt, hf)], [("acc", jt, hf)])

        pend = None
        for e in range(NE):
            for tb in range(4):
                moe_gu(e, tb, 0)
                if pend is not None:
                    moe_down(*pend)
                if tb == 0 and e + 1 < NE:
                    load_expert(e + 1)
                moe_gu(e, tb, 1)
                pend = (e, tb)
        moe_down(*pend)
        for jt in range(NT_LAT):
            s2 = jt % 2
            dma("sp", hm2[s2], hmid_d[b, jt * 128:(jt + 1) * 128, :], [("hmid", jt)], [("hm2", s2)], "hm2%d" % s2)
            tt("dve" if jt % 4 != 3 else "pool", ot[s2], acc[:, jt, :], grow[b][1], ALU.mult,
               [("acc", jt, 0), ("acc", jt, 1), ("grow", b, 1)], [("ot", s2)])
            tt("dve", ot[s2], ot[s2], hm2[s2], ALU.add, [("ot", s2), ("hm2", s2)], [("ot", s2)])
            dma("sp", out_d[b, jt * 128:(jt + 1) * 128, :], ot[s2], [("ot", s2)], [("out", jt)], "ost%d" % s2)
        S.barrier()
        A.release(mB)

    return finish(nc, S, A)


def finish(nc, S, A):
    print('SBUF arena peak words', A.peak, 'of', A.words)
    S.barrier()
    S.add("sp", lambda e: e.nop(), reads=(), writes=())
    S.emit(None)
    return nc


def _consts():
    ident = np.eye(128, dtype=np.float32)
    r = np.arange(128)
    trif = (r[:, None] <= r[None, :]).astype(np.float32)
    trib = (r[:, None] >= r[None, :]).astype(np.float32)
    half = 64
    inv = (10000.0 ** (-np.arange(0, half, 2, dtype=np.float32) / half)).astype(np.float32)
    tok = np.arange(SEQ)
    row = (tok // 64).astype(np.float32)
    col = (tok % 64).astype(np.float32)
    ang_r = row[:, None] * inv[None, :]
    ang_c = col[:, None] * inv[None, :]
    cr, sr, cc, sc = np.cos(ang_r), np.sin(ang_r), np.cos(ang_c), np.sin(ang_c)
    cos = np.concatenate([cr, cr, cc, cc], axis=1).astype(np.float32)
    sin = np.concatenate([-sr, sr, -sc, sc], axis=1).astype(np.float32)
    cos_t = cos.reshape(NT_LAT, 128, 128).transpose(1, 0, 2).reshape(128, NT_LAT * 128)
    sin_t = sin.reshape(NT_LAT, 128, 128).transpose(1, 0, 2).reshape(128, NT_LAT * 128)
    return ident, trif, trib, np.ascontiguousarray(cos_t), np.ascontiguousarray(sin_t)


def _win_perm():
    idx = []
    for h in range(8):
        for f_ in range(4):
            idx += list(range(f_ * 1024 + h * 128, f_ * 1024 + (h + 1) * 128))
    idx += list(range(4096, 4128))
    idx += list(range(4128, 5152))
    for j in range(2):
        idx += list(range(5152 + j * 128, 5152 + (j + 1) * 128))
        idx += list(range(5408 + j * 128, 5408 + (j + 1) * 128))
    for c in range(8):
        idx += list(range(5664 + c * 128, 5664 + (c + 1) * 128))
        idx += list(range(6688 + c * 128, 6688 + (c + 1) * 128))
    assert len(idx) == INW and len(set(idx)) == INW
    return np.asarray(idx)


def make_in_maps(inputs, NB, cores):
    f = lambda a: np.ascontiguousarray(np.asarray(a, dtype=np.float32))
    ident, trif, trib, cos_t, sin_t = _consts()

    def pk(v):
        return np.ascontiguousarray(np.asarray(v, np.float32).reshape(8, 128).T)

    shared = {
        "w_ada": f(inputs["w_ada"][0]),
        "b_adaT": np.ascontiguousarray(np.asarray(inputs["b_ada"][0], np.float32).reshape(48, 128).T),
        "n1T": pk(inputs["norm1_w"][0]), "n2T": pk(inputs["norm2_w"][0]),
        "w_in": np.ascontiguousarray(np.asarray(inputs["w_in"][0], np.float32)[:, _win_perm()]),
        "b_mgate": f(inputs["b_mgate"][0]).reshape(1, 32),
        "q_norm_w": f(inputs["q_norm_w"][0]).reshape(1, 128),
        "k_norm_w": f(inputs["k_norm_w"][0]).reshape(1, 128),
        "mh_norm_w": f(inputs["mh_norm_w"][0]).reshape(1, D),
        "w_bm": f(inputs["w_branch_m"][0]), "w_ba": f(inputs["w_branch_a"][0]), "w_o": f(inputs["w_out"][0]),
        "w_r": np.ascontiguousarray(np.concatenate([inputs["w_rg"][0], inputs["w_re"][0]], axis=1).astype(np.float32)),
        "b_r": np.ascontiguousarray(np.concatenate([inputs["b_rg"][0], inputs["b_re"][0]]).astype(np.float32).reshape(1, 36)),
        "w_egu": np.ascontiguousarray(np.concatenate([np.asarray(inputs["w_e_gate"][0], np.float32),
                                                      np.asarray(inputs["w_e_up"][0], np.float32)], axis=2)),
        "w_ed": f(inputs["w_e_down"][0]),
        "c_ident": ident, "c_trif": trif, "c_trib": trib, "c_cos": cos_t, "c_sin": sin_t,
    }
    maps = []
    for c in cores:
        bs = slice(c * NB, (c + 1) * NB)
        cv = np.concatenate([np.asarray(inputs["c"], np.float32)[bs], np.asarray(inputs["c_ctx"], np.float32)[None, :]], axis=0)
        NV = NB + 1
        cT = cv.reshape(NV, 8, 128).transpose(2, 1, 0).reshape(128, 8 * NV)
        m = dict(shared)
        m["x"] = f(inputs["x"][bs])
        m["ctxx"] = f(inputs["ctx"][bs])
        m["cT"] = np.ascontiguousarray(cT)
        maps.append(m)
    return maps


_NC_CACHE = {}


def kernel(**inputs):
    NB = 2
    if "full" not in _NC_CACHE:
        _NC_CACHE["full"] = build_program(NB=NB)
    nc = _NC_CACHE["full"]
    maps = make_in_maps(inputs, NB, list(range(N_CORES)))
    res = run_bass_kernel_spmd(nc, maps, core_ids=list(range(N_CORES)))
    out = np.concatenate([np.asarray(r["out"], dtype=np.float32) for r in res.results], axis=0)
    return out
```
